# Optimizing a Trainium2 kernel written in Bass

```python
import jax
import jax.numpy as jnp
from jax import lax
import numpy as np

D_MODEL = 2048
BATCH = 1
SEQ = 8192
DEPTH = 1

CHUNK = 64
N_HEADS = 16
HEAD_DIM = 128
KV_LATENT = 256
IDX_HEADS = 16
IDX_DIM = 64
TOPK_KEYS_MAX = 256
Q_BLOCK = 128
ATTN_SCALE = HEAD_DIM ** -0.5
LRU_WIDTH = 2048
LRU_BLOCKS = 16
LRU_BLOCK_DIM = LRU_WIDTH // LRU_BLOCKS
CONV_WIDTH = 4
RG_C = 8.0
N_EXPERTS = 64
TOP_K = 8
N_GROUPS = 8
TOPK_GROUPS = 4
EXPERT_FF = 512
SHARED_FF = 512
ROUTE_SCALE = 2.5
EXPERT_BLOCK = 128
LN_EPS = 1e-5
RMS_EPS = 1e-6

Q_DIM = N_HEADS * HEAD_DIM
IDX_Q_DIM = IDX_HEADS * IDX_DIM
PROJ_WIDTHS = (Q_DIM, KV_LATENT, IDX_Q_DIM, IDX_DIM, IDX_HEADS, LRU_WIDTH, LRU_WIDTH, D_MODEL, D_MODEL)
PROJ_DIM = sum(PROJ_WIDTHS)
PROJ_SPLITS = tuple(sum(PROJ_WIDTHS[:i + 1]) for i in range(len(PROJ_WIDTHS) - 1))

kernel_name = 'hybrid_dsa_rglru_moe_deepnorm'


def layer_norm(x, g, b):
    xf = x.astype(jnp.float32)
    mu = jnp.mean(xf, -1, keepdims=True)
    var = jnp.mean(jnp.square(xf - mu), -1, keepdims=True)
    y = (xf - mu) * lax.rsqrt(var + LN_EPS)
    return (y * g.astype(jnp.float32) + b.astype(jnp.float32)).astype(x.dtype)


def rms_norm(x, g):
    xf = x.astype(jnp.float32)
    y = xf * lax.rsqrt(jnp.mean(xf * xf, -1, keepdims=True) + RMS_EPS)
    return (y * g.astype(jnp.float32)).astype(x.dtype)


def swiglu(x, w_gate, w_up, w_down):
    return (jax.nn.silu(x @ w_gate) * (x @ w_up)) @ w_down


def dsa_attention(q, c, q_idx, k_idx, w_idx, w_uk, w_uv):
    B, T = q.shape[0], q.shape[1]
    n_sel = min(TOPK_KEYS_MAX, T // 4)
    n_blocks = T // Q_BLOCK
    q_lat = jnp.einsum('bthd,hdc->bthc', q, w_uk)
    key_pos = jnp.arange(T, dtype=jnp.int32)
    w_idx = w_idx * (IDX_HEADS ** -0.5)
    idx_scale = IDX_DIM ** -0.5

    def to_blocks(a):
        return jnp.moveaxis(a.reshape((B, n_blocks, Q_BLOCK) + a.shape[2:]), 1, 0)

    def query_block(args):
        qi, wi, ql, blk = args
        t = blk * Q_BLOCK + jnp.arange(Q_BLOCK, dtype=jnp.int32)
        limit = (t // CHUNK + 1) * CHUNK
        admissible = key_pos[None, :] < limit[:, None]
        logits = jnp.einsum('bqhd,bsd->bqhs', qi, k_idx) * idx_scale
        index_score = jnp.einsum('bqh,bqhs->bqs', wi, jax.nn.relu(logits)).astype(jnp.float32)
        index_score = jnp.where(admissible[None], index_score, -jnp.inf)
        _, sel = lax.top_k(index_score, n_sel)
        valid = sel < limit[None, :, None]
        c_sel = jax.vmap(lambda cb, ib: cb[ib])(c, sel)
        s = jnp.einsum('bqhc,bqkc->bqhk', ql, c_sel).astype(jnp.float32) * ATTN_SCALE
        s = jnp.where(valid[:, :, None, :], s, -jnp.inf)
        p = jax.nn.softmax(s, axis=-1).astype(c.dtype)
        return jnp.einsum('bqhk,bqkc->bqhc', p, c_sel)

    blocks = jnp.arange(n_blocks, dtype=jnp.int32)
    o_lat = lax.map(query_block, (to_blocks(q_idx), to_blocks(w_idx), to_blocks(q_lat), blocks))
    o_lat = jnp.moveaxis(o_lat, 0, 1).reshape(B, T, N_HEADS, KV_LATENT)
    o = jnp.einsum('bthc,hcd->bthd', o_lat, w_uv)
    return o.reshape(B, T, Q_DIM)


def rg_lru_branch(xr, yg, conv_w, conv_b, w_a, b_a, w_x, b_x, lam):
    B, T, C = xr.shape
    xc = lax.conv_general_dilated(
        xr, conv_w[:, None, :].astype(xr.dtype), window_strides=(1,),
        padding=[(CONV_WIDTH - 1, 0)], dimension_numbers=('NWC', 'WIO', 'NWC'),
        feature_group_count=C) + conv_b
    xb = xc.reshape(B, T, LRU_BLOCKS, LRU_BLOCK_DIM)
    r = jax.nn.sigmoid(jnp.einsum('btnd,nde->btne', xb, w_a).reshape(B, T, C) + b_a)
    i = jax.nn.sigmoid(jnp.einsum('btnd,nde->btne', xb, w_x).reshape(B, T, C) + b_x)
    log_a = -RG_C * r.astype(jnp.float32) * jax.nn.softplus(-lam.astype(jnp.float32))
    a = jnp.exp(log_a)
    u = jnp.sqrt(-jnp.expm1(2.0 * log_a)) * (i * xc).astype(jnp.float32)

    def combine(left, right):
        a1, b1 = left
        a2, b2 = right
        return a1 * a2, a2 * b1 + b2

    _, h = lax.associative_scan(combine, (a, u), axis=1)
    return h.astype(xr.dtype) * jax.nn.gelu(yg)


def routed_moe(h, w_router, router_bias, w_gate_e, w_up_e, w_down_e, w_gate_s, w_up_s, w_down_s):
    B, T, D = h.shape
    N = B * T
    hf = h.reshape(N, D)
    scores = jax.nn.sigmoid((hf @ w_router).astype(jnp.float32))
    biased = scores + router_bias.astype(jnp.float32)
    per_group = N_EXPERTS // N_GROUPS
    group_score = jnp.sum(lax.top_k(biased.reshape(N, N_GROUPS, per_group), 2)[0], -1)
    _, group_idx = lax.top_k(group_score, TOPK_GROUPS)
    group_mask = jnp.zeros((N, N_GROUPS), jnp.bool_).at[jnp.arange(N)[:, None], group_idx].set(True)
    expert_mask = jnp.repeat(group_mask, per_group, axis=1)
    _, expert_idx = lax.top_k(jnp.where(expert_mask, biased, -jnp.inf), TOP_K)
    gate = jnp.take_along_axis(scores, expert_idx, axis=1)
    gate = gate / jnp.sum(gate, -1, keepdims=True) * ROUTE_SCALE
    A = N * TOP_K
    e_flat = expert_idx.reshape(A)
    tok_flat = jnp.repeat(jnp.arange(N, dtype=jnp.int32), TOP_K)
    order = jnp.argsort(e_flat)
    e_sorted = e_flat[order]
    counts = jnp.bincount(e_flat, length=N_EXPERTS)
    padded = (counts + EXPERT_BLOCK - 1) // EXPERT_BLOCK * EXPERT_BLOCK
    pad_end = jnp.cumsum(padded)
    pad_start = pad_end - padded
    cnt_start = jnp.cumsum(counts) - counts
    dest = pad_start[e_sorted] + jnp.arange(A) - cnt_start[e_sorted]
    n_blk = -(-A // EXPERT_BLOCK) + N_EXPERTS
    rows = n_blk * EXPERT_BLOCK
    tok_buf = jnp.zeros((rows,), jnp.int32).at[dest].set(tok_flat[order])
    gate_buf = jnp.zeros((rows,), h.dtype).at[dest].set(gate.reshape(A)[order].astype(h.dtype))
    blk_expert = jnp.minimum(
        jnp.searchsorted(pad_end, jnp.arange(n_blk) * EXPERT_BLOCK, side='right'), N_EXPERTS - 1)

    def expert_rows(args):
        tok, g, e = args
        y = swiglu(hf[tok], w_gate_e[e], w_up_e[e], w_down_e[e])
        return y * g[:, None]

    ys = lax.map(expert_rows, (tok_buf.reshape(n_blk, EXPERT_BLOCK),
                               gate_buf.reshape(n_blk, EXPERT_BLOCK), blk_expert))
    routed = jnp.zeros_like(hf).at[tok_buf].add(ys.reshape(rows, D))
    shared = swiglu(hf, w_gate_s, w_up_s, w_down_s)
    return (routed + shared).reshape(B, T, D)


def setup_inputs(seed: int = 0) -> dict:
    key = jax.random.key(seed)
    ks = jax.random.split(key, 32)
    f32 = jnp.float32
    L = DEPTH
    beta = (8.0 * DEPTH) ** -0.25

    def normal(k, shape, scale):
        return jax.random.normal(k, shape, f32) * scale

    def gain(k, shape):
        return 1.0 + 0.02 * jax.random.normal(k, shape, f32)

    def bias(k, shape):
        return 0.02 * jax.random.normal(k, shape, f32)

    a_c = jax.random.uniform(ks[13], (L, LRU_WIDTH), f32, 0.9, 0.999)
    a_base = a_c ** (1.0 / RG_C)
    rg_lambda = jnp.log(a_base) - jnp.log1p(-a_base)
    return {
        'x': normal(ks[0], (BATCH, SEQ, D_MODEL), 1.0),
        'ln_in_g': gain(ks[1], (D_MODEL,)),
        'ln_in_b': bias(ks[2], (D_MODEL,)),
        'w_in': normal(ks[3], (L, D_MODEL, PROJ_DIM), D_MODEL ** -0.5),
        'kv_norm_g': gain(ks[4], (L, KV_LATENT)),
        'w_uk': normal(ks[5], (L, N_HEADS, HEAD_DIM, KV_LATENT), HEAD_DIM ** -0.5),
        'w_uv': normal(ks[6], (L, N_HEADS, KV_LATENT, HEAD_DIM), beta * KV_LATENT ** -0.5),
        'conv_w': normal(ks[7], (L, CONV_WIDTH, LRU_WIDTH), CONV_WIDTH ** -0.5),
        'conv_b': bias(ks[8], (L, LRU_WIDTH)),
        'w_rg_a': normal(ks[9], (L, LRU_BLOCKS, LRU_BLOCK_DIM, LRU_BLOCK_DIM), LRU_BLOCK_DIM ** -0.5),
        'b_rg_a': bias(ks[10], (L, LRU_WIDTH)),
        'w_rg_x': normal(ks[11], (L, LRU_BLOCKS, LRU_BLOCK_DIM, LRU_BLOCK_DIM), LRU_BLOCK_DIM ** -0.5),
        'b_rg_x': bias(ks[12], (L, LRU_WIDTH)),
        'rg_lambda': rg_lambda,
        'w_branch_a': normal(ks[14], (L, Q_DIM, D_MODEL), beta * Q_DIM ** -0.5),
        'w_branch_b': normal(ks[15], (L, LRU_WIDTH, D_MODEL), beta * LRU_WIDTH ** -0.5),
        'w_out': normal(ks[16], (L, D_MODEL, D_MODEL), beta * D_MODEL ** -0.5),
        'ln1_g': gain(ks[17], (L, D_MODEL)),
        'ln1_b': bias(ks[18], (L, D_MODEL)),
        'w_router': normal(ks[19], (L, D_MODEL, N_EXPERTS), D_MODEL ** -0.5),
        'router_bias': normal(ks[20], (L, N_EXPERTS), 0.01),
        'w_gate_e': normal(ks[21], (L, N_EXPERTS, D_MODEL, EXPERT_FF), D_MODEL ** -0.5),
        'w_up_e': normal(ks[22], (L, N_EXPERTS, D_MODEL, EXPERT_FF), beta * D_MODEL ** -0.5),
        'w_down_e': normal(ks[23], (L, N_EXPERTS, EXPERT_FF, D_MODEL), beta * EXPERT_FF ** -0.5),
        'w_gate_s': normal(ks[24], (L, D_MODEL, SHARED_FF), D_MODEL ** -0.5),
        'w_up_s': normal(ks[25], (L, D_MODEL, SHARED_FF), beta * D_MODEL ** -0.5),
        'w_down_s': normal(ks[26], (L, SHARED_FF, D_MODEL), beta * SHARED_FF ** -0.5),
        'ln2_g': gain(ks[27], (L, D_MODEL)),
        'ln2_b': bias(ks[28], (L, D_MODEL)),
    }


def reference(x, ln_in_g, ln_in_b, w_in, kv_norm_g, w_uk, w_uv, conv_w, conv_b, w_rg_a, b_rg_a,
              w_rg_x, b_rg_x, rg_lambda, w_branch_a, w_branch_b, w_out, ln1_g, ln1_b, w_router,
              router_bias, w_gate_e, w_up_e, w_down_e, w_gate_s, w_up_s, w_down_s, ln2_g, ln2_b):
    alpha = (2.0 * DEPTH) ** 0.25
    B, T, _ = x.shape
    h = layer_norm(x, ln_in_g, ln_in_b)
    for l in range(DEPTH):
        z = h @ w_in[l]
        q, c, qi, ki, wi, xr, yg, ga, gb = jnp.split(z, PROJ_SPLITS, axis=-1)
        c = rms_norm(c, kv_norm_g[l])
        attn = dsa_attention(q.reshape(B, T, N_HEADS, HEAD_DIM), c,
                             qi.reshape(B, T, IDX_HEADS, IDX_DIM), ki, wi, w_uk[l], w_uv[l])
        lru = rg_lru_branch(xr, yg, conv_w[l], conv_b[l], w_rg_a[l], b_rg_a[l],
                            w_rg_x[l], b_rg_x[l], rg_lambda[l])
        merged = (jax.nn.sigmoid(ga) * (attn @ w_branch_a[l])
                  + jax.nn.sigmoid(gb) * (lru @ w_branch_b[l]))
        h = layer_norm(alpha * h + merged @ w_out[l], ln1_g[l], ln1_b[l])
        ffn = routed_moe(h, w_router[l], router_bias[l], w_gate_e[l], w_up_e[l], w_down_e[l],
                         w_gate_s[l], w_up_s[l], w_down_s[l])
        h = layer_norm(alpha * h + ffn, ln2_g[l], ln2_b[l])
    return h
```

```python
import os
from contextlib import ExitStack

import numpy as np
import concourse.bass as bass
import concourse.mybir as mybir
from concourse.bass_utils import run_bass_kernel_spmd

F32 = mybir.dt.float32
BF16 = mybir.dt.bfloat16
U32 = mybir.dt.uint32
I32 = mybir.dt.int32
ALU = mybir.AluOpType
AF = mybir.ActivationFunctionType
AX = mybir.AxisListType

NCORES = 8
D = 2048
T = 8192
NT = 64
NOWN = 8
KC = 16
PROJ = 11600
C_Q, C_C, C_QI, C_KI, C_WI, C_XR, C_YG, C_GA, C_GB = 0, 2048, 2304, 3328, 3392, 3408, 5456, 7504, 9552
LN_EPS = 1e-5
RMS_EPS = 1e-6
ALPHA = 2.0 ** 0.25
ATTN_SCALE = 128 ** -0.5
BIG = 1.0e30
NSEL = 256
NBIS = 14
CAP = 256
NEXP = 64


class Buf:
    __slots__ = ("name", "lw", "rd", "wo")

    def __init__(self, name, wo=False):
        self.name = name
        self.lw = None
        self.rd = []
        self.wo = wo


class Ins:
    __slots__ = ("eng", "fn", "deps", "dma", "sig", "ticket", "dsem", "dval", "idx")


class Prog:
    ENGS = ("pe", "act", "dve", "pool", "sp")

    def __init__(self, nc, es):
        self.nc = nc
        self.es = es
        self.lists = {e: [] for e in self.ENGS}
        self.n = 0
        self.psem = {e: es.enter_context(nc.semaphore("prog_" + e)) for e in ("pe", "act", "dve", "pool")}
        self.bar_pos = {}
        self.free_sems = []
        self.dma_sems = {}
        self.nsem = 0

    def _dsem(self, key):
        if key not in self.dma_sems:
            if self.free_sems:
                self.dma_sems[key] = self.free_sems.pop()
            else:
                self.dma_sems[key] = [self.es.enter_context(self.nc.semaphore("dma%d" % self.nsem)), 0]
                self.nsem += 1
        return self.dma_sems[key]

    def barrier(self):
        lasts = []
        for e in self.ENGS:
            comp = [i for i in self.lists[e] if i.dma is None and i.fn is not None]
            if comp:
                lasts.append(comp[-1])
        dmas = [i for e in self.ENGS for i in self.lists[e][self.bar_pos.get(e, 0):] if i.dma is not None]
        for e in self.ENGS:
            self.bar_pos[e] = len(self.lists[e])
        for e in self.ENGS:
            self.op(e, None, extra=lasts + dmas)
        self.free_sems.extend(self.dma_sems.values())
        self.dma_sems = {}

    def op(self, eng, fn, r=(), w=(), dma=None, extra=()):
        ins = Ins()
        ins.eng, ins.fn, ins.dma, ins.sig, ins.ticket = eng, fn, dma, False, 0
        ins.idx = self.n
        self.n += 1
        deps = list(extra)
        for b in r:
            if b.lw is not None:
                deps.append(b.lw)
        for b in w:
            if b.wo:
                continue
            if b.lw is not None:
                deps.append(b.lw)
            deps.extend(b.rd)
        seen = set()
        ins.deps = []
        for d in deps:
            if d.idx in seen or d is ins:
                continue
            seen.add(d.idx)
            if d.eng == "pe" and eng == "pe" and d.dma is None and dma is None:
                continue
            ins.deps.append(d)
            d.sig = True
        for b in r:
            b.rd.append(ins)
        for b in w:
            b.lw = ins
            b.rd = []
        if dma is not None:
            s = self._dsem(dma)
            s[1] += 16
            ins.dsem, ins.dval = s[0], s[1]
        self.lists[eng].append(ins)
        return ins

    def emit(self):
        nc = self.nc
        for e in ("pe", "act", "dve", "pool"):
            t = 0
            for ins in self.lists[e]:
                if ins.dma is None and ins.sig:
                    t += 1
                    ins.ticket = t

        def run(ename, eng):
            waited = {}
            for ins in self.lists[ename]:
                for d in ins.deps:
                    if d.dma is not None:
                        sem, val = d.dsem, d.dval
                    else:
                        sem, val = self.psem[d.eng], d.ticket
                    k = id(sem)
                    if waited.get(k, 0) >= val:
                        continue
                    waited[k] = val
                    eng.wait_ge(sem, val)
                if ins.fn is None:
                    continue
                res = ins.fn(eng)
                if ins.dma is not None:
                    res.then_inc(ins.dsem, 16)
                elif ins.sig:
                    res.then_inc(self.psem[ename], 1)

        with nc.Block() as block:
            @block.sync
            def _(e):
                run("sp", e)

            @block.scalar
            def _(e):
                run("act", e)

            @block.vector
            def _(e):
                run("dve", e)

            @block.gpsimd
            def _(e):
                run("pool", e)

            @block.tensor
            def _(e):
                run("pe", e)


class Ring:
    def __init__(self, name, tiles):
        self.tiles = tiles
        self.bufs = [Buf("%s%d" % (name, i)) for i in range(len(tiles))]
        self.i = 0
        self.name = name

    def next(self):
        k = self.i % len(self.tiles)
        self.i += 1
        return self.tiles[k], self.bufs[k], (self.name, k)


def build_program(stop_after=None, debug=False):
    nc = bass.Bass("TRN2", target_bir_lowering=False)
    es = ExitStack()
    with es:
        P = Prog(nc, es)

        def dram_in(name, shape, dt=F32):
            return nc.dram_tensor(name, list(shape), dt, kind="ExternalInput").ap()

        def dram_scr(name, shape, dt):
            return nc.dram_tensor(name, list(shape), dt, kind="Internal").ap()

        def sb(name, shape, dt, stack=None):
            return (stack or es).enter_context(nc.sbuf_tensor(name, list(shape), dt))

        def ps(name, shape, dt, stack=None):
            return (stack or es).enter_context(nc.psum_tensor(name, list(shape), dt))

        SHAPES = {
            "xw": [T, D], "valid7": [1, 896], "ln_in_g": [D], "ln_in_b": [D], "w_in": [D, PROJ],
            "kv_norm_g": [256], "w_uk": [16, 128, 256], "w_uv": [16, 256, 128], "conv_w": [4, D],
            "conv_b": [D], "w_rg_a": [16, 128, 128], "b_rg_a": [D], "w_rg_x": [16, 128, 128],
            "b_rg_x": [D], "rg_lambda": [D], "w_branch_a": [D, D], "w_branch_b": [D, D], "w_out": [D, D],
            "ln1_g": [D], "ln1_b": [D], "w_router": [D, NEXP], "router_bias": [NEXP],
            "w_gate_e": [NEXP, D, 512], "w_up_e": [NEXP, D, 512], "w_down_e": [NEXP, 512, D],
            "w_gate_s": [D, 512], "w_up_s": [D, 512], "w_down_s": [512, D], "ln2_g": [D], "ln2_b": [D],
        }
        declared = {}

        def IN(name):
            if name not in declared:
                declared[name] = dram_in(name, SHAPES[name])
            return declared[name]

        if not debug:
            for nm in SHAPES:
                IN(nm)
        xw = IN("xw"); ln_in_g = IN("ln_in_g"); ln_in_b = IN("ln_in_b"); w_in = IN("w_in")
        kv_norm_g = IN("kv_norm_g"); conv_w = IN("conv_w"); conv_b = IN("conv_b")
        b_rg_a = IN("b_rg_a"); b_rg_x = IN("b_rg_x"); rg_lambda = IN("rg_lambda")
        out = nc.dram_tensor("out", [NOWN * 128, D], F32, kind="ExternalOutput").ap()
        dbg = {}
        B_dbg = Buf("dbg", wo=True)

        def dbg_out(name, shape, dt=F32):
            t = nc.dram_tensor("dbg_" + name, list(shape), dt, kind="ExternalOutput").ap()
            dbg[name] = t
            return t

        hT_all = dram_scr("hT_all", [16, 128, KC, 512], BF16)
        h_own = dram_scr("h_own", [NOWN, 128, D], F32)
        lruh = dram_scr("lruh", [NOWN, 128, KC, 128], F32)
        B_hT = [Buf("hT_all%d" % b) for b in range(16)]
        B_hown = [Buf("h_own%d" % j) for j in range(NOWN)]
        B_lruh = [[Buf("lruh%d_%d" % (j, p)) for p in range(2)] for j in range(NOWN)]

        ident_f = sb("ident_f", [128, 128], F32)
        ident_b = sb("ident_b", [128, 128], BF16)
        ones_f = sb("ones_f", [128, 128], F32)
        B_const = Buf("const")
        eps_ln = sb("eps_ln", [128, 1], F32)
        eps_rms = sb("eps_rms", [128, 1], F32)
        P.op("pool", lambda e: e.memset(eps_ln[:], LN_EPS), w=[B_const])
        P.op("pool", lambda e: e.memset(eps_rms[:], RMS_EPS), w=[B_const])

        P.op("pool", lambda e: e.memset(ones_f[:], 1.0), w=[B_const])
        P.op("pool", lambda e: e.affine_select(out=ident_f[:], in_=ones_f[:], pattern=[[-1, 128]],
                                               compare_op=ALU.is_equal, fill=0.0, base=0,
                                               channel_multiplier=1), r=[B_const], w=[B_const])
        P.op("pool", lambda e: e.tensor_copy(out=ident_b[:], in_=ident_f[:]), r=[B_const], w=[B_const])

        PAR = [ln_in_g, ln_in_b, conv_w[0], conv_w[1], conv_w[2], conv_w[3], conv_b, b_rg_a, b_rg_x, rg_lambda]
        PI = {n: i for i, n in enumerate(["lng", "lnb", "cw0", "cw1", "cw2", "cw3", "cb", "ba", "bx", "lam"])}
        prow = sb("prow", [128, 2, 128], F32)
        pcol = sb("pcol", [128, 256], F32)
        B_prow = Buf("prow"); B_pcol = Buf("pcol")
        P.op("pool", lambda e: e.memset(prow[:], 0.0), w=[B_prow])
        for i, par in enumerate(PAR):
            r0 = i * 16
            g, rr = divmod(r0, 128)
            P.op("sp", (lambda e, par=par, g=g, rr=rr: e.dma_start(
                out=prow[rr:rr + 16, g, :], in_=par.rearrange("(k p) -> k p", p=128))),
                r=[], w=[B_prow], dma=("prow", i))
        with ExitStack() as s0:
            pst = ps("pst0", [128, 256], F32, s0)
            B_pst = Buf("pst0")
            for g in range(2):
                P.op("pe", lambda e, g=g: e.transpose(out=pst[:, g * 128:(g + 1) * 128], in_=prow[:, g, :],
                                                     identity=ident_f[:]), r=[B_prow, B_const], w=[B_pst])
            P.op("dve", lambda e: e.tensor_copy(out=pcol[:], in_=pst[:]), r=[B_pst], w=[B_pcol])

        def pc(name, k):
            i = PI[name] * 16 + k
            return pcol[:, i:i + 1]

        cT_d = dram_scr("cT_d", [128, 2, T], BF16)
        c_d = dram_scr("c_d", [128, NT, 257], BF16)
        ki_d = dram_scr("ki_d", [128, T], BF16)
        B_cTd = Buf("cT_d"); B_cd = Buf("c_d"); B_kid = Buf("ki_d")
        B_cT = [Buf("cT%d" % b) for b in range(16)]
        B_c = [Buf("c%d" % b) for b in range(16)]
        B_ki = [Buf("ki%d" % b) for b in range(16)]
        B_c1 = Buf("c_ones")

        with ExitStack() as s1:
            cT_all = sb("cT_all", [128, 2, T], BF16, s1)
            c_all = sb("c_all", [128, NT, 257], BF16, s1)
            kiT_all = sb("kiT_all", [128, T], BF16, s1)
            P.op("pool", lambda e: e.memset(c_all[:, :, 256:257], 1.0), w=[B_c1])
            wc = sb("wc", [128, KC, 256], BF16, s1)
            wki = sb("wki", [128, KC, 128], BF16, s1)
            gkv = sb("gkv", [128, 2], F32, s1)
            grow = sb("grow", [128, D], F32, s1)
            brow = sb("brow", [128, D], F32, s1)
            B_wc = Buf("wc"); B_wki = Buf("wki"); B_gkv = Buf("gkv"); B_grow = Buf("grow")
            w_in_v = w_in.rearrange("(k p) n -> p k n", p=128)
            P.op("pool", lambda e: e.dma_start(out=wc[:], in_=w_in_v[:, :, C_C:C_C + 256]), w=[B_wc], dma=("wc", 0))
            P.op("pool", lambda e: e.dma_start(out=wki[:, :, 0:64], in_=w_in_v[:, :, C_KI:C_KI + 64]), w=[B_wki], dma=("wki", 0))
            P.op("pool", lambda e: e.dma_start(out=wki[:, :, 64:128], in_=w_in_v[:, :, C_KI:C_KI + 64]), w=[B_wki], dma=("wki", 0))
            P.op("sp", lambda e: e.dma_start(out=grow[:], in_=ln_in_g.partition_broadcast(128)), w=[B_grow], dma=("grow", 0))
            P.op("sp", lambda e: e.dma_start(out=brow[:], in_=ln_in_b.partition_broadcast(128)), w=[B_grow], dma=("grow", 0))
            gkv_row = sb("gkv_row", [2, 128], F32, s1)
            B_gkvr = Buf("gkvr")
            P.op("sp", lambda e: e.dma_start(out=gkv_row[:], in_=kv_norm_g.rearrange("(k p) -> k p", p=128)),
                 w=[B_gkvr], dma=("gkvr", 0))
            pst1 = ps("pst1", [128, 2], F32, s1)
            B_pst1 = B_pst
            P.op("pe", lambda e: e.transpose(out=pst1[:, 0:2], in_=gkv_row[:, :], identity=ident_f[0:2, 0:2]),
                 r=[B_gkvr, B_const], w=[B_pst1])
            P.op("dve", lambda e: e.tensor_copy(out=gkv[:], in_=pst1[:]), r=[B_pst1], w=[B_gkv])

            xt_ring = Ring("xt", [sb("xt%d" % i, [128, D], F32, s1) for i in range(3)])
            nt_ring = Ring("nt", [sb("nt%d" % i, [128, D], F32, s1) for i in range(3)])
            hT_ring = Ring("hTb", [sb("hTb%d" % i, [128, KC, 512], BF16, s1) for i in range(2)])
            st_ring = Ring("stat", [sb("stat%d" % i, [128, 40], F32, s1) for i in range(4)])
            tp_ring = Ring("tp", [ps("tp%d" % i, [128, 512], F32, s1) for i in range(2)])
            cps = [ps("cps%d" % i, [128, 512], F32, s1) for i in range(2)]
            B_cps = [Buf("cps%d" % i) for i in range(2)]
            ssps = ps("ssps", [128, 512], F32, s1); B_ssps = Buf("ssps")
            kips = ps("kips", [128, 512], F32, s1); B_kips = Buf("kips")
            ctp = ps("ctp", [128, 4, 256], BF16, s1); B_ctp = Buf("ctp")
            sq_ring = Ring("sq", [sb("sq%d" % i, [128, 512], F32, s1) for i in range(2)])
            rs = sb("rs", [128, 512], F32, s1); B_rs = Buf("rs")
            hown_ring = Ring("hown", [sb("hown%d" % i, [128, D], F32, s1) for i in range(1)])

            blkbuf = {}
            tilebuf = {}

            def p1_s0(tile_i):
                xt, B_xt, kx = xt_ring.next()
                P.op("sp", lambda e, xt=xt, tile_i=tile_i: e.dma_start(
                    out=xt[:], in_=xw[tile_i * 128:(tile_i + 1) * 128, :]), w=[B_xt], dma=kx)
                st, B_st, _ = st_ring.next()
                for q4 in range(4):
                    P.op("dve", lambda e, st=st, xt=xt, q4=q4: e.bn_stats(
                        out=st[:, q4 * 6:(q4 + 1) * 6], in_=xt[:, q4 * 512:(q4 + 1) * 512]), r=[B_xt], w=[B_st])
                P.op("dve", lambda e, st=st: e.bn_aggr(out=st[:, 24:26], in_=st[:, 0:24]), r=[B_st], w=[B_st])
                P.op("act", lambda e, st=st: e.activation(out=st[:, 26:27], in_=st[:, 25:26], func=AF.Sqrt,
                                                          bias=eps_ln[:, 0:1], scale=1.0), r=[B_st, B_const], w=[B_st])
                tilebuf[tile_i] = (xt, B_xt, st, B_st)

            def p1_s0b(tile_i):
                xt, B_xt, st, B_st = tilebuf[tile_i]
                P.op("dve", lambda e, st=st: e.reciprocal(out=st[:, 27:28], in_=st[:, 26:27]), r=[B_st], w=[B_st])
                ntile, B_nt, _ = nt_ring.next()
                P.op("dve", lambda e, st=st, xt=xt, ntile=ntile: e.tensor_scalar(
                    out=ntile[:], in0=xt[:], scalar1=st[:, 24:25], scalar2=st[:, 27:28],
                    op0=ALU.subtract, op1=ALU.mult), r=[B_xt, B_st], w=[B_nt])
                tilebuf[tile_i] = (ntile, B_nt)
                if tile_i % 8 == 7:
                    j = tile_i // 8
                    ho, B_ho, kh = hown_ring.next()
                    P.op("pool", lambda e, ho=ho, ntile=ntile: e.tensor_tensor(
                        out=ho[:], in0=ntile[:], in1=grow[:], op=ALU.mult), r=[B_nt, B_grow], w=[B_ho])
                    P.op("pool", lambda e, ho=ho: e.tensor_tensor(
                        out=ho[:], in0=ho[:], in1=brow[:], op=ALU.add), r=[B_ho, B_grow], w=[B_ho])
                    P.op("sp", lambda e, ho=ho, j=j: e.dma_start(out=h_own[j], in_=ho[:]),
                         r=[B_ho], w=[B_hown[j]], dma=("h_own", j % 4))

            def p1_s1(tile_i):
                blk, tl = divmod(tile_i, 4)
                if tl == 0:
                    blkbuf[blk] = hT_ring.next()
                hTb, B_hTb, _ = blkbuf[blk]
                ntile, B_nt = tilebuf.pop(tile_i)
                for k4 in range(4):
                    tp, B_tp, _ = tp_ring.next()
                    for kk in range(4):
                        k = k4 * 4 + kk
                        P.op("pe", lambda e, tp=tp, ntile=ntile, k=k, kk=kk: e.transpose(
                            out=tp[:, kk * 128:(kk + 1) * 128], in_=ntile[:, k * 128:(k + 1) * 128],
                            identity=ident_f[:]), r=[B_nt, B_const], w=[B_tp])
                    for kk in range(4):
                        k = k4 * 4 + kk
                        P.op("act", lambda e, tp=tp, hTb=hTb, k=k, kk=kk, tl=tl: e.activation(
                            out=hTb[:, k, tl * 128:(tl + 1) * 128], in_=tp[:, kk * 128:(kk + 1) * 128],
                            func=AF.Identity, bias=pc("lnb", k), scale=pc("lng", k)),
                            r=[B_tp, B_pcol], w=[B_hTb])

            def p1_b0(blk):
                hTb, B_hTb, _ = blkbuf[blk]
                P.op("sp", lambda e, hTb=hTb, blk=blk: e.dma_start(out=hT_all[blk], in_=hTb[:]),
                     r=[B_hTb], w=[B_hT[blk]], dma=("hT_all", blk % 4))
                cols = slice(blk * 512, (blk + 1) * 512)
                for ch in range(2):
                    for k in range(KC):
                        P.op("pe", lambda e, ch=ch, k=k, hTb=hTb: e.matmul(
                            cps[ch][:], lhsT=wc[:, k, ch * 128:(ch + 1) * 128], rhs=hTb[:, k, :],
                            start=(k == 0), stop=(k == KC - 1)), r=[B_wc, B_hTb], w=[B_cps[ch]])
                for k in range(KC):
                    P.op("pe", lambda e, k=k, hTb=hTb: e.matmul(
                        kips[:], lhsT=wki[:, k, :], rhs=hTb[:, k, :], start=(k == 0), stop=(k == KC - 1)),
                        r=[B_wki, B_hTb], w=[B_kips])
                P.op("act", lambda e, cols=cols: e.copy(out=kiT_all[:, cols], in_=kips[:]), r=[B_kips], w=[B_ki[blk]])
                for ch in range(2):
                    sq, B_sq, _ = sq_ring.next()
                    P.op("act", lambda e, sq=sq, ch=ch: e.activation(out=sq[:], in_=cps[ch][:], func=AF.Square),
                         r=[B_cps[ch]], w=[B_sq])
                    P.op("pe", lambda e, sq=sq, ch=ch: e.matmul(ssps[:], lhsT=ones_f[:], rhs=sq[:],
                                                                start=(ch == 0), stop=(ch == 1)),
                         r=[B_sq, B_const], w=[B_ssps])
                P.op("act", lambda e: e.activation(out=rs[:], in_=ssps[:], func=AF.Sqrt, bias=eps_rms[:, 0:1],
                                                   scale=1.0 / 256.0), r=[B_ssps, B_const], w=[B_rs])

            def p1_b1(blk):
                cols = slice(blk * 512, (blk + 1) * 512)
                P.op("dve", lambda e: e.reciprocal(out=rs[:], in_=rs[:]), r=[B_rs], w=[B_rs])
                for ch in range(2):
                    P.op("dve", lambda e, ch=ch, cols=cols: e.scalar_tensor_tensor(
                        out=cT_all[:, ch, cols], in0=cps[ch][:], scalar=gkv[:, ch:ch + 1], in1=rs[:],
                        op0=ALU.mult, op1=ALU.mult), r=[B_cps[ch], B_gkv, B_rs], w=[B_cT[blk]])

            def p1_b2(blk):
                for tl in range(4):
                    for ch in range(2):
                        c0 = blk * 512 + tl * 128
                        P.op("pe", lambda e, tl=tl, ch=ch, c0=c0: e.transpose(
                            out=ctp[:, tl, ch * 128:(ch + 1) * 128], in_=cT_all[:, ch, c0:c0 + 128],
                            identity=ident_b[:]), r=[B_cT[blk], B_const], w=[B_ctp])
                P.op("act", lambda e, blk=blk: e.copy(out=c_all[:, blk * 4:(blk + 1) * 4, 0:256], in_=ctp[:]),
                     r=[B_ctp], w=[B_c[blk]])

            for step in range(NT + 7):
                if step < NT:
                    p1_s0(step)
                if 0 <= step - 1 < NT:
                    p1_s0b(step - 1)
                t1_ = step - 3
                if 0 <= t1_ < NT:
                    p1_s1(t1_)
                    if t1_ % 4 == 3:
                        p1_b0(t1_ // 4)
                t2_ = step - 4
                if 0 <= t2_ < NT and t2_ % 4 == 3:
                    p1_b1(t2_ // 4)
                t3_ = step - 5
                if 0 <= t3_ < NT and t3_ % 4 == 3:
                    p1_b2(t3_ // 4)

            P.op("sp", lambda e: e.dma_start(out=cT_d, in_=cT_all[:]), r=B_cT, w=[B_cTd], dma=("cT_d", 0))
            P.op("sp", lambda e: e.dma_start(out=c_d, in_=c_all[:]), r=B_c + [B_c1], w=[B_cd], dma=("c_d", 0))
            P.op("sp", lambda e: e.dma_start(out=ki_d, in_=kiT_all[:]), r=B_ki, w=[B_kid], dma=("ki_d", 0))

        P.barrier()

        def finish_debug():
            P.op("sp", None, r=[B_dbg])
            P.emit()
            return nc, dbg, list(declared)

        if debug and stop_after == "1a":
            d1 = dbg_out("cT", [128, 2, T], BF16)
            d2 = dbg_out("c", [128, NT, 257], BF16)
            d3 = dbg_out("kiT", [128, T], BF16)
            d4 = dbg_out("h_own", [NOWN, 128, D], F32)
            d5 = dbg_out("hT_all", [16, 128, KC, 512], BF16)
            P.op("sp", lambda e: e.dma_start(out=d1, in_=cT_d), r=[B_cTd], w=[B_dbg], dma=("dbg", 0))
            P.op("sp", lambda e: e.dma_start(out=d2, in_=c_d), r=[B_cd], w=[B_dbg], dma=("dbg", 0))
            P.op("sp", lambda e: e.dma_start(out=d3, in_=ki_d), r=[B_kid], w=[B_dbg], dma=("dbg", 0))
            P.op("sp", lambda e: e.dma_start(out=d4.rearrange("j p d -> p j d"), in_=h_own.rearrange("j p d -> p j d")),
                 r=B_hown, w=[B_dbg], dma=("dbg", 0))
            P.op("sp", lambda e: e.dma_start(out=d5.rearrange("b p k t -> p b (k t)"),
                                             in_=hT_all.rearrange("b p k t -> p b (k t)")),
                 r=B_hT, w=[B_dbg], dma=("dbg", 0))
            return finish_debug()

        w_rg_a = IN("w_rg_a"); w_rg_x = IN("w_rg_x"); valid7 = IN("valid7")
        with ExitStack() as s2:
            hstate = sb("hstate", [128, KC], F32, s2)
            vrow = sb("vrow", [128, 896], F32, s2)
            lp = sb("lp", [128, 8, KC], F32, s2)
            B_hst = [Buf("hst%d" % c) for c in range(KC)]
            B_vrow = Buf("vrow"); B_lp = Buf("lp")
            P.op("pool", lambda e: e.memset(hstate[:], 0.0), w=B_hst)
            P.op("sp", lambda e: e.dma_start(out=vrow[:], in_=valid7[0].partition_broadcast(128)), w=[B_vrow], dma=("vrow", 0))
            lam = pcol[:, PI["lam"] * 16:PI["lam"] * 16 + 16]
            X, SER, LN1P, MSK, SP_, CH, HBA, HBX = range(8)
            P.op("act", lambda e: e.activation(out=lp[:, X, :], in_=lam, func=AF.Exp, scale=-1.0), r=[B_pcol], w=[B_lp])
            P.op("act", lambda e: e.activation(out=lp[:, LN1P, :], in_=lp[:, X, :], func=AF.Ln, bias=ones_f[:, 0:1], scale=1.0),
                 r=[B_lp, B_const], w=[B_lp])
            P.op("dve", lambda e: e.tensor_scalar(out=lp[:, SER, :], in0=lp[:, X, :], scalar1=-0.25, scalar2=1.0 / 3.0,
                                                  op0=ALU.mult, op1=ALU.add), r=[B_lp], w=[B_lp])
            P.op("dve", lambda e: e.tensor_tensor(out=lp[:, SER, :], in0=lp[:, SER, :], in1=lp[:, X, :], op=ALU.mult), r=[B_lp], w=[B_lp])
            P.op("dve", lambda e: e.tensor_scalar(out=lp[:, SER, :], in0=lp[:, SER, :], scalar1=-0.5, scalar2=None,
                                                  op0=ALU.add), r=[B_lp], w=[B_lp])
            P.op("dve", lambda e: e.tensor_tensor(out=lp[:, SER, :], in0=lp[:, SER, :], in1=lp[:, X, :], op=ALU.mult), r=[B_lp], w=[B_lp])
            P.op("dve", lambda e: e.tensor_scalar(out=lp[:, SER, :], in0=lp[:, SER, :], scalar1=1.0, scalar2=None,
                                                  op0=ALU.add), r=[B_lp], w=[B_lp])
            P.op("dve", lambda e: e.tensor_tensor(out=lp[:, SER, :], in0=lp[:, SER, :], in1=lp[:, X, :], op=ALU.mult), r=[B_lp], w=[B_lp])
            P.op("dve", lambda e: e.tensor_scalar(out=lp[:, MSK, :], in0=lp[:, X, :], scalar1=0.05, scalar2=None,
                                                  op0=ALU.is_lt), r=[B_lp], w=[B_lp])
            P.op("dve", lambda e: e.tensor_tensor(out=lp[:, SP_, :], in0=lp[:, SER, :], in1=lp[:, LN1P, :], op=ALU.subtract), r=[B_lp], w=[B_lp])
            P.op("dve", lambda e: e.tensor_tensor(out=lp[:, SP_, :], in0=lp[:, SP_, :], in1=lp[:, MSK, :], op=ALU.mult), r=[B_lp], w=[B_lp])
            P.op("dve", lambda e: e.tensor_tensor(out=lp[:, SP_, :], in0=lp[:, SP_, :], in1=lp[:, LN1P, :], op=ALU.add), r=[B_lp], w=[B_lp])
            P.op("dve", lambda e: e.tensor_scalar(out=lp[:, CH, :], in0=lp[:, SP_, :], scalar1=-4.0, scalar2=None, op0=ALU.mult), r=[B_lp], w=[B_lp])
            P.op("dve", lambda e: e.tensor_scalar(out=lp[:, HBA, :], in0=pcol[:, PI["ba"] * 16:PI["ba"] * 16 + 16], scalar1=0.5,
                                                  scalar2=None, op0=ALU.mult), r=[B_pcol], w=[B_lp])
            P.op("dve", lambda e: e.tensor_scalar(out=lp[:, HBX, :], in0=pcol[:, PI["bx"] * 16:PI["bx"] * 16 + 16], scalar1=0.5,
                                                  scalar2=None, op0=ALU.mult), r=[B_pcol], w=[B_lp])

            wxr = sb("wxr", [128, KC, 1024], BF16, s2); B_wxr = Buf("wxr")
            wga = sb("wga", [128, 8, 128], BF16, s2); wgx = sb("wgx", [128, 8, 128], BF16, s2); B_wg = Buf("wg")
            diag = sb("diag", [128, 32, 128], BF16, s2); B_diag = Buf("diag")
            hT_ring = Ring("hTb2", [sb("hTc%d" % i, [128, KC, 512], BF16, s2) for i in range(2)])
            xrb = sb("xrb", [128, 8, 515], BF16, s2); B_xrb = [Buf("xrb%d" % i) for i in range(8)]
            xcb = sb("xcb", [128, 8, 512], BF16, s2); B_xc = [Buf("xc%d" % i) for i in range(8)]
            trb = sb("trb", [128, 8, 512], F32, s2); B_tr = [Buf("tr%d" % i) for i in range(8)]
            tib = sb("tib", [128, 8, 512], BF16, s2); B_ti = [Buf("ti%d" % i) for i in range(8)]
            ab = sb("ab", [128, 8, 512], F32, s2); B_a = [Buf("a%d" % i) for i in range(8)]
            sbf = sb("sbf", [128, 8, 512], BF16, s2); B_s = [Buf("s%d" % i) for i in range(8)]
            hb = sb("hb", [128, 8, 512], F32, s2); B_hb = [Buf("hb%d" % i) for i in range(8)]
            xps_ring = Ring("xps", [ps("xps%d" % i, [128, 512], F32, s2) for i in range(2)])
            cv_ring = Ring("cvps", [ps("cvps%d" % i, [128, 512], F32, s2) for i in range(2)])
            ga_ring = Ring("gaps", [ps("gaps%d" % i, [128, 512], F32, s2) for i in range(2)])
            gx_ring = Ring("gxps", [ps("gxps%d" % i, [128, 512], F32, s2) for i in range(2)])
            w_rg_a_v = w_rg_a.rearrange("n d e -> d n e")
            w_rg_x_v = w_rg_x.rearrange("n d e -> d n e")

            for PS in range(2):
                P.op("pool", lambda e, PS=PS: e.dma_start(out=wxr[:], in_=w_in_v[:, :, C_XR + PS * 1024:C_XR + (PS + 1) * 1024]),
                     w=[B_wxr], dma=("wxr", 0))
                P.op("pool", lambda e, PS=PS: e.dma_start(out=wga[:], in_=w_rg_a_v[:, PS * 8:(PS + 1) * 8, :]), w=[B_wg], dma=("wg", 0))
                P.op("pool", lambda e, PS=PS: e.dma_start(out=wgx[:], in_=w_rg_x_v[:, PS * 8:(PS + 1) * 8, :]), w=[B_wg], dma=("wg", 0))
                for cl in range(8):
                    c = PS * 8 + cl
                    for k in range(4):
                        P.op("dve", lambda e, cl=cl, k=k, c=c: e.tensor_scalar(
                            out=diag[:, cl * 4 + k, :], in0=ident_b[:], scalar1=pc("cw%d" % k, c), scalar2=None, op0=ALU.mult),
                            r=[B_const, B_pcol], w=[B_diag])
                    P.op("pool", lambda e, cl=cl: e.memset(xrb[:, cl, 0:3], 0.0), w=[B_xrb[cl]])
                blkb = {}
                itb = {}

                def A0(it, PS=PS):
                    blk, cl = divmod(it, 8)
                    if cl == 0:
                        hTb, B_hTb, kh = hT_ring.next()
                        P.op("sp", lambda e, hTb=hTb, blk=blk: e.dma_start(out=hTb[:], in_=hT_all[blk]), r=[B_hT[blk]], w=[B_hTb], dma=kh)
                        blkb[blk] = (hTb, B_hTb)
                    hTb, B_hTb = blkb[blk]
                    xps, B_xps, _ = xps_ring.next()
                    for k in range(KC):
                        P.op("pe", lambda e, xps=xps, k=k, cl=cl, hTb=hTb: e.matmul(
                            xps[:], lhsT=wxr[:, k, cl * 128:(cl + 1) * 128], rhs=hTb[:, k, :], start=(k == 0), stop=(k == KC - 1)),
                            r=[B_wxr, B_hTb], w=[B_xps])
                    if blk == 0:
                        P.op("dve", lambda e, xps=xps, cl=cl: e.tensor_tensor(out=xrb[:, cl, 3:515], in0=xps[:], in1=vrow[:, 0:512], op=ALU.mult),
                             r=[B_xps, B_vrow], w=[B_xrb[cl]])
                    elif blk == 1:
                        P.op("dve", lambda e, xps=xps, cl=cl: e.tensor_tensor(out=xrb[:, cl, 3:387], in0=xps[:, 0:384], in1=vrow[:, 512:896], op=ALU.mult),
                             r=[B_xps, B_vrow], w=[B_xrb[cl]])
                        P.op("dve", lambda e, xps=xps, cl=cl: e.tensor_copy(out=xrb[:, cl, 387:515], in_=xps[:, 384:512]),
                             r=[B_xps], w=[B_xrb[cl]])
                    else:
                        P.op("dve", lambda e, xps=xps, cl=cl: e.tensor_copy(out=xrb[:, cl, 3:515], in_=xps[:]), r=[B_xps], w=[B_xrb[cl]])

                def A1(it, PS=PS):
                    blk, cl = divmod(it, 8)
                    c = PS * 8 + cl
                    cvp, B_cvp, _ = cv_ring.next()
                    for k in range(4):
                        P.op("pe", lambda e, cvp=cvp, k=k, cl=cl: e.matmul(
                            cvp[:], lhsT=diag[:, cl * 4 + k, :], rhs=xrb[:, cl, k:k + 512], start=(k == 0), stop=(k == 3)),
                            r=[B_diag, B_xrb[cl]], w=[B_cvp])
                    P.op("act", lambda e, cvp=cvp, cl=cl, c=c: e.activation(out=xcb[:, cl, :], in_=cvp[:], func=AF.Identity,
                                                                         bias=pc("cb", c), scale=1.0), r=[B_cvp, B_pcol], w=[B_xc[cl]])
                    P.op("pool", lambda e, cl=cl: e.tensor_copy(out=xrb[:, cl, 0:3], in_=xrb[:, cl, 512:515]), r=[B_xrb[cl]], w=[B_xrb[cl]])

                def A2(it, PS=PS):
                    blk, cl = divmod(it, 8)
                    c = PS * 8 + cl
                    gap, B_gap, _ = ga_ring.next()
                    gxp, B_gxp, _ = gx_ring.next()
                    P.op("pe", lambda e, gap=gap, cl=cl: e.matmul(gap[:], lhsT=wga[:, cl, :], rhs=xcb[:, cl, :], start=True, stop=True),
                         r=[B_wg, B_xc[cl]], w=[B_gap])
                    P.op("pe", lambda e, gxp=gxp, cl=cl: e.matmul(gxp[:], lhsT=wgx[:, cl, :], rhs=xcb[:, cl, :], start=True, stop=True),
                         r=[B_wg, B_xc[cl]], w=[B_gxp])
                    P.op("act", lambda e, gap=gap, cl=cl, c=c: e.activation(out=trb[:, cl, :], in_=gap[:], func=AF.Tanh,
                                                                         bias=lp[:, HBA, c:c + 1], scale=0.5), r=[B_gap, B_lp], w=[B_tr[cl]])
                    P.op("act", lambda e, gxp=gxp, cl=cl, c=c: e.activation(out=tib[:, cl, :], in_=gxp[:], func=AF.Tanh,
                                                                         bias=lp[:, HBX, c:c + 1], scale=0.5), r=[B_gxp, B_lp], w=[B_ti[cl]])
                    P.op("act", lambda e, cl=cl, c=c: e.activation(out=ab[:, cl, :], in_=trb[:, cl, :], func=AF.Exp,
                                                                 bias=lp[:, CH, c:c + 1], scale=lp[:, CH, c:c + 1]), r=[B_tr[cl], B_lp], w=[B_a[cl]])

                def BST(blk, PS=PS):
                    for cl in range(8):
                        P.op("pool", lambda e, cl=cl: e.tensor_tensor(out=trb[:, cl, :], in0=ab[:, cl, :], in1=ab[:, cl, :], op=ALU.mult),
                             r=[B_a[cl]], w=[B_tr[cl]])
                    for cl in range(8):
                        P.op("act", lambda e, cl=cl: e.activation(out=sbf[:, cl, :], in_=trb[:, cl, :], func=AF.Sqrt, bias=ones_f[:, 0:1], scale=-1.0),
                             r=[B_tr[cl], B_const], w=[B_s[cl]])
                    for cl in range(8):
                        P.op("dve", lambda e, cl=cl: e.tensor_scalar(out=tib[:, cl, :], in0=tib[:, cl, :], scalar1=1.0, scalar2=0.5,
                                                                     op0=ALU.add, op1=ALU.mult), r=[B_ti[cl]], w=[B_ti[cl]])
                    for cl in range(8):
                        c = PS * 8 + cl
                        P.op("dve", lambda e, cl=cl: e.tensor_tensor(out=sbf[:, cl, :], in0=tib[:, cl, :], in1=sbf[:, cl, :], op=ALU.mult),
                             r=[B_ti[cl], B_s[cl]], w=[B_s[cl]])
                        P.op("pool", lambda e, cl=cl: e.tensor_tensor(out=tib[:, cl, :], in0=sbf[:, cl, :], in1=xcb[:, cl, :], op=ALU.mult),
                             r=[B_s[cl], B_xc[cl]], w=[B_ti[cl]])
                        if blk == 0:
                            P.op("pool", lambda e, cl=cl: e.tensor_tensor(out=tib[:, cl, :], in0=tib[:, cl, :], in1=vrow[:, 0:512], op=ALU.mult),
                                 r=[B_ti[cl], B_vrow], w=[B_ti[cl]])
                        elif blk == 1:
                            P.op("pool", lambda e, cl=cl: e.tensor_tensor(out=tib[:, cl, 0:384], in0=tib[:, cl, 0:384], in1=vrow[:, 512:896], op=ALU.mult),
                                 r=[B_ti[cl], B_vrow], w=[B_ti[cl]])
                        P.op("dve", lambda e, cl=cl, c=c: e.tensor_tensor_scan(out=hb[:, cl, :], data0=ab[:, cl, :], data1=tib[:, cl, :],
                                                                            initial=hstate[:, c:c + 1], op0=ALU.mult, op1=ALU.add),
                             r=[B_a[cl], B_ti[cl], B_hst[c]], w=[B_hb[cl]])
                        P.op("dve", lambda e, cl=cl, c=c: e.tensor_copy(out=hstate[:, c:c + 1], in_=hb[:, cl, 511:512]), r=[B_hb[cl]], w=[B_hst[c]])
                    if blk % 2 == 1:
                        j = blk // 2
                        P.op("sp", lambda e, j=j, PS=PS: e.dma_start(out=lruh[j][:, PS * 8:(PS + 1) * 8, :], in_=hb[:, :, 384:512]),
                             r=B_hb, w=[B_lruh[j][PS]], dma=("lruh", (2 * j + PS) % 4))

                NIT = 128
                SK1, SK2 = 3, 4
                for step in range(NIT + SK2):
                    t2_ = step - SK2
                    if 0 <= t2_ < NIT:
                        A2(t2_)
                        if t2_ % 8 == 7:
                            BST(t2_ // 8)
                    if 0 <= step - SK1 < NIT:
                        A1(step - SK1)
                    if step < NIT:
                        A0(step)

        P.barrier()
        if debug and stop_after == "1b":
            d1 = dbg_out("lruh", [NOWN, 128, KC, 128], F32)
            P.op("sp", lambda e: e.dma_start(out=d1.rearrange("j p k t -> p j (k t)"), in_=lruh.rearrange("j p k t -> p j (k t)")),
                 r=[b for bb in B_lruh for b in bb], w=[B_dbg], dma=("dbg", 0))
            return finish_debug()

        w_uk = IN("w_uk")
        qlat_d = dram_scr("qlat_d", [NOWN, 128, 16, 2, 128], BF16)
        qi_d = dram_scr("qi_d", [128, 8, 1024], BF16)
        lruT_d = dram_scr("lruT_d", [128, KC, 1024], BF16)
        gaT_d = dram_scr("gaT_d", [128, KC, 1024], BF16)
        gbT_d = dram_scr("gbT_d", [128, KC, 1024], BF16)
        B_qlat = [[Buf("qlat%d_%d" % (h, th)) for th in range(2)] for h in range(32)]
        B_qid = Buf("qi_d")
        B_lruT = [[Buf("lruT%d_%d" % (m, th)) for th in range(2)] for m in range(KC)]
        B_gaT = [[Buf("gaT%d_%d" % (m, th)) for th in range(2)] for m in range(KC)]
        B_gbT = [[Buf("gbT%d_%d" % (m, th)) for th in range(2)] for m in range(KC)]
        wsc = sb("wsc", [128, NOWN, 16], F32); B_wsc = Buf("wsc")
        with ExitStack() as s3:
            hT_own = sb("hT_own", [128, KC, 1024], BF16, s3); B_hTo = Buf("hT_own")
            for j in range(NOWN):
                P.op("sp", lambda e, j=j: e.dma_start(out=hT_own[:, :, j * 128:(j + 1) * 128], in_=hT_all[2 * j + 1][:, :, 384:512]),
                     r=[B_hT[2 * j + 1]], w=[B_hTo], dma=("hT_own", 0))
            qT_sb = sb("qT_sb", [128, 16, 1024], BF16, s3); B_qT = [Buf("qT%d" % h) for h in range(16)]
            qiT_sb = sb("qiT_sb", [128, 8, 1024], BF16, s3); B_qiT = Buf("qiT")
            wuk = sb("wuk", [128, 16, 256], BF16, s3); B_wuk = Buf("wuk")
            wwi = sb("wwi", [128, KC, 16], BF16, s3); B_wwi = Buf("wwi")
            P.op("pool", lambda e: e.dma_start(out=wuk[:], in_=w_uk.rearrange("h d c -> d h c")), w=[B_wuk], dma=("wuk", 0))
            P.op("pool", lambda e: e.dma_start(out=wwi[:], in_=w_in_v[:, :, C_WI:C_WI + 16]), w=[B_wwi], dma=("wwi", 0))
            w_ring = Ring("wr2", [sb("wr2_%d" % i, [128, KC, 512], BF16, s3) for i in range(3)])
            pp_ring = Ring("pp2", [ps("pp2_%d" % i, [128, 512], F32, s3) for i in range(4)])
            stg_ring = Ring("stg2", [sb("stg2_%d" % i, [128, 512], BF16, s3) for i in range(4)])
            sq_ring2 = Ring("sq2", [sb("sq2_%d" % i, [128, 512], F32, s3) for i in range(2)])
            tt_ring = Ring("tt2", [sb("tt2_%d" % i, [128, 512], F32, s3) for i in range(2)])
            lh_ring = Ring("lh2", [sb("lh2_%d" % i, [128, 512], F32, s3) for i in range(2)])
            wips = ps("wips", [128, 16], F32, s3); B_wips = Buf("wips")
            for j in range(NOWN):
                for k in range(KC):
                    P.op("pe", lambda e, j=j, k=k: e.matmul(wips[:], lhsT=hT_own[:, k, j * 128:(j + 1) * 128], rhs=wwi[:, k, :],
                                                           start=(k == 0), stop=(k == KC - 1)), r=[B_hTo, B_wwi], w=[B_wips])
                P.op("dve", lambda e, j=j: e.tensor_scalar(out=wsc[:, j, :], in0=wips[:], scalar1=1.0 / 32.0, scalar2=None, op0=ALU.mult),
                     r=[B_wips], w=[B_wsc])
            units = ([("q", C_Q + 512 * u, u) for u in range(4)] + [("qi", C_QI + 512 * u, u) for u in range(2)] +
                     [("yg", C_YG + 512 * u, u) for u in range(4)] + [("ga", C_GA + 512 * u, u) for u in range(4)] +
                     [("gb", C_GB + 512 * u, u) for u in range(4)])
            ecount = 0
            for (kind, col0, u) in units:
                wt, B_wt, kw = w_ring.next()
                P.op("pool", lambda e, wt=wt, col0=col0: e.dma_start(out=wt[:], in_=w_in_v[:, :, col0:col0 + 512]), w=[B_wt], dma=kw)
                for mm in range(4):
                    m = u * 4 + mm
                    for th in range(2):
                        tsl = slice(th * 512, (th + 1) * 512)
                        pp, B_pp, _ = pp_ring.next()
                        for k in range(KC):
                            P.op("pe", lambda e, pp=pp, wt=wt, k=k, mm=mm, tsl=tsl: e.matmul(
                                pp[:], lhsT=wt[:, k, mm * 128:(mm + 1) * 128], rhs=hT_own[:, k, tsl], start=(k == 0), stop=(k == KC - 1)),
                                r=[B_wt, B_hTo], w=[B_pp])
                        if kind in ("q", "qi"):
                            dst = qT_sb[:, m, tsl] if kind == "q" else qiT_sb[:, m, tsl]
                            Bd = B_qT[m] if kind == "q" else B_qiT
                            ecount += 1
                            if ecount % 2 == 0:
                                P.op("act", lambda e, dst=dst, pp=pp: e.copy(out=dst, in_=pp[:]), r=[B_pp], w=[Bd])
                            else:
                                P.op("dve", lambda e, dst=dst, pp=pp: e.tensor_copy(out=dst, in_=pp[:]), r=[B_pp], w=[Bd])
                        elif kind == "yg":
                            sq, B_sq, _ = sq_ring2.next()
                            tt, B_tt, _ = tt_ring.next()
                            lh, B_lh, klh = lh_ring.next()
                            stg, B_stg, _ = stg_ring.next()
                            P.op("sp", lambda e, lh=lh, m=m, th=th: e.dma_start(
                                out=lh[:].rearrange("p (j t) -> p j t", j=4),
                                in_=lruh[4 * th:4 * th + 4, :, m, :].rearrange("j p t -> p j t")),
                                r=[B_lruh[jj][m // 8] for jj in range(4 * th, 4 * th + 4)], w=[B_lh], dma=klh)
                            P.op("act", lambda e, sq=sq, pp=pp: e.activation(out=sq[:], in_=pp[:], func=AF.Square), r=[B_pp], w=[B_sq])
                            P.op("dve", lambda e, sq=sq: e.tensor_scalar(out=sq[:], in0=sq[:], scalar1=0.044715, scalar2=1.0,
                                                                         op0=ALU.mult, op1=ALU.add), r=[B_sq], w=[B_sq])
                            P.op("dve", lambda e, sq=sq, pp=pp: e.tensor_tensor(out=sq[:], in0=sq[:], in1=pp[:], op=ALU.mult), r=[B_sq, B_pp], w=[B_sq])
                            P.op("act", lambda e, sq=sq, tt=tt: e.activation(out=tt[:], in_=sq[:], func=AF.Tanh, scale=0.7978845608028654),
                                 r=[B_sq], w=[B_tt])
                            P.op("dve", lambda e, tt=tt, pp=pp: e.scalar_tensor_tensor(out=tt[:], in0=tt[:], scalar=1.0, in1=pp[:],
                                                                                  op0=ALU.add, op1=ALU.mult), r=[B_tt, B_pp], w=[B_tt])
                            P.op("dve", lambda e, tt=tt, lh=lh, stg=stg: e.scalar_tensor_tensor(out=stg[:], in0=tt[:], scalar=0.5, in1=lh[:],
                                                                                           op0=ALU.mult, op1=ALU.mult), r=[B_tt, B_lh], w=[B_stg])
                            P.op("sp", lambda e, stg=stg, m=m, tsl=tsl: e.dma_start(out=lruT_d[:, m, tsl], in_=stg[:]),
                                 r=[B_stg], w=[B_lruT[m][th]], dma=("lruT_d", (2 * m + th) % 4))
                        else:
                            tt, B_tt, _ = tt_ring.next()
                            stg, B_stg, _ = stg_ring.next()
                            dd = gaT_d if kind == "ga" else gbT_d
                            Bd = (B_gaT if kind == "ga" else B_gbT)[m][th]
                            P.op("act", lambda e, tt=tt, pp=pp: e.activation(out=tt[:], in_=pp[:], func=AF.Tanh, scale=0.5), r=[B_pp], w=[B_tt])
                            P.op("dve", lambda e, tt=tt, stg=stg: e.tensor_scalar(out=stg[:], in0=tt[:], scalar1=0.5, scalar2=0.5,
                                                                                  op0=ALU.mult, op1=ALU.add), r=[B_tt], w=[B_stg])
                            P.op("sp", lambda e, stg=stg, dd=dd, m=m, tsl=tsl: e.dma_start(out=dd[:, m, tsl], in_=stg[:]),
                                 r=[B_stg], w=[Bd], dma=(kind + "T_d", (2 * m + th) % 4))
                if kind == "q":
                    for mm in range(4):
                        h = u * 4 + mm
                        for ch in range(2):
                            for th in range(2):
                                tsl = slice(th * 512, (th + 1) * 512)
                                pp, B_pp, _ = pp_ring.next()
                                stg, B_stg, _ = stg_ring.next()
                                P.op("pe", lambda e, pp=pp, h=h, ch=ch, tsl=tsl: e.matmul(
                                    pp[:], lhsT=wuk[:, h, ch * 128:(ch + 1) * 128], rhs=qT_sb[:, h, tsl], start=True, stop=True),
                                    r=[B_wuk, B_qT[h]], w=[B_pp])
                                P.op("act", lambda e, pp=pp, stg=stg: e.activation(out=stg[:], in_=pp[:], func=AF.Copy, scale=ATTN_SCALE),
                                     r=[B_pp], w=[B_stg])
                                P.op("sp", lambda e, stg=stg, h=h, ch=ch, th=th: e.dma_start(
                                    out=qlat_d[4 * th:4 * th + 4, :, h, ch, :].rearrange("j p q -> p j q"),
                                    in_=stg[:].rearrange("p (j q) -> p j q", j=4)),
                                    r=[B_stg], w=[B_qlat[h * 2 + ch][th]], dma=("qlat_d", (h * 4 + ch * 2 + th) % 4))
            P.op("sp", lambda e: e.dma_start(out=qi_d, in_=qiT_sb[:]), r=[B_qiT], w=[B_qid], dma=("qi_d", 0))

        P.barrier()
        if debug and stop_after == "2":
            d1 = dbg_out("qlat", [NOWN, 128, 16, 2, 128], BF16)
            d2 = dbg_out("qi", [128, 8, 1024], BF16)
            d3 = dbg_out("lruT", [128, KC, 1024], BF16)
            d4 = dbg_out("gaT", [128, KC, 1024], BF16)
            d5 = dbg_out("wsc", [128, NOWN, 16], F32)
            P.op("sp", lambda e: e.dma_start(out=d1.rearrange("j p h c q -> p j (h c q)"), in_=qlat_d.rearrange("j p h c q -> p j (h c q)")),
                 r=[b for bb in B_qlat for b in bb], w=[B_dbg], dma=("dbg", 0))
            P.op("sp", lambda e: e.dma_start(out=d2, in_=qi_d), r=[B_qid], w=[B_dbg], dma=("dbg", 0))
            P.op("sp", lambda e: e.dma_start(out=d3, in_=lruT_d), r=[b for bb in B_lruT for b in bb], w=[B_dbg], dma=("dbg", 0))
            P.op("sp", lambda e: e.dma_start(out=d4, in_=gaT_d), r=[b for bb in B_gaT for b in bb], w=[B_dbg], dma=("dbg", 0))
            P.op("sp", lambda e: e.dma_start(out=d5, in_=wsc[:]), r=[B_wsc], w=[B_dbg], dma=("dbg", 0))
            return finish_debug()

        w_uv = IN("w_uv")
        attnT_d = dram_scr("attnT_d", [128, 16, 1024], BF16)
        B_attnT = [Buf("attnT%d" % j) for j in range(NOWN)]
        with ExitStack() as s4:
            cT_all = sb("cT_all3", [128, 2, T], BF16, s4); B_cTs = Buf("cTs")
            c_all = sb("c_all3", [128, NT, 257], BF16, s4); B_cs = Buf("cs")
            kiT_all = sb("kiT_all3", [128, T], BF16, s4); B_kis = Buf("kis")
            P.op("sp", lambda e: e.dma_start(out=cT_all[:], in_=cT_d), r=[B_cTd], w=[B_cTs], dma=("cTs", 0))
            P.op("sp", lambda e: e.dma_start(out=c_all[:], in_=c_d), r=[B_cd], w=[B_cs], dma=("cs", 0))
            P.op("sp", lambda e: e.dma_start(out=kiT_all[:], in_=ki_d), r=[B_kid], w=[B_kis], dma=("kis", 0))
            wuv = sb("wuv", [128, 2, 16, 128], BF16, s4); B_wuv = Buf("wuv")
            P.op("pool", lambda e: e.dma_start(out=wuv[:, 0], in_=w_uv[:, 0:128, :].rearrange("h p d -> p h d")), w=[B_wuv], dma=("wuv", 0))
            P.op("pool", lambda e: e.dma_start(out=wuv[:, 1], in_=w_uv[:, 128:256, :].rearrange("h p d -> p h d")), w=[B_wuv], dma=("wuv", 0))
            vrow3 = sb("vrow3", [128, 896], F32, s4); pen7 = sb("pen7", [128, 896], F32, s4); B_v3 = Buf("v3")
            P.op("sp", lambda e: e.dma_start(out=vrow3[:], in_=valid7[0].partition_broadcast(128)), w=[B_v3], dma=("v3", 0))
            P.op("pool", lambda e: e.tensor_scalar(out=pen7[:], in0=vrow3[:], scalar1=-1.0, scalar2=BIG, op0=ALU.add, op1=ALU.mult),
                 r=[B_v3], w=[B_v3])
            pow2 = sb("pow2", [128, NBIS], F32, s4); B_pow2 = Buf("pow2")
            for k in range(NBIS):
                P.op("pool", lambda e, k=k: e.memset(pow2[:, k:k + 1], 2.0 ** -(k + 1)), w=[B_pow2])
            Isc = sb("Isc", [128, T], F32, s4); B_Ikb = [Buf("Isc%d" % i) for i in range(16)]
            junk = sb("junk", [128, T], mybir.dt.uint8, s4); B_junk = Buf("junk")
            maskT = sb("maskT", [128, NT, 128], BF16, s4); B_mT = Buf("maskT")
            bis = sb("bis", [128, 8], F32, s4); B_bis = Buf("bis")
            wk = sb("wk", [128, NBIS], F32, s4)
            qi_ring = Ring("qit", [sb("qit%d" % i, [128, 8, 128], BF16, s4) for i in range(2)])
            ql_ring = Ring("qlt", [sb("qlt%d" % i, [128, 16, 2, 128], BF16, s4) for i in range(2)])
            pT_ring = Ring("pT", [sb("pT%d" % i, [128, 2, 128], BF16, s4) for i in range(6)])
            mk_ring = Ring("mk", [sb("mk%d" % i, [128, 512], BF16, s4) for i in range(2)])
            ol_ring = Ring("ol", [sb("ol%d" % i, [128, 256], BF16, s4) for i in range(2)])
            rc_ring = Ring("rc", [sb("rc%d" % i, [128, 1], F32, s4) for i in range(2)])
            olatT = sb("olatT", [128, 2, 16, 128], BF16, s4); B_olT = [Buf("olT%d" % h) for h in range(16)]
            as_ring = Ring("ast", [sb("ast%d" % i, [128, 16, 128], BF16, s4) for i in range(2)])
            HPG = 2
            L_ring = Ring("Lps", [ps("Lps%d" % i, [128, 512], F32, s4) for i in range(3)])
            _stb = [ps("stps%d" % i, [128, 512], F32, s4) for i in range(3)]
            st_ring3 = Ring("stps", [_stb[i][:, 0:256] for i in range(3)])
            acc = [ps("acc%d" % i, [128, 512], F32, s4) for i in range(HPG)]
            B_acc = [Buf("acc%d" % i) for i in range(HPG)]

            negbig = sb("negbig", [128, 1], F32, s4)
            P.op("pool", lambda e: e.memset(negbig[:], -30000.0), w=[B_v3])
            BB, W0, LO, MID, CNT, GW = 0, 1, 2, 3, 4, 5
            tiles3 = {}

            def load3(j):
                qit, B_qit, kq = qi_ring.next()
                qlt, B_qlt, kl = ql_ring.next()
                P.op("sp", lambda e, qit=qit, j=j: e.dma_start(out=qit[:], in_=qi_d[:, :, j * 128:(j + 1) * 128]), r=[B_qid], w=[B_qit], dma=kq)
                P.op("sp", lambda e, qlt=qlt, j=j: e.dma_start(out=qlt[:], in_=qlat_d[j]), r=[b for bb in B_qlat for b in bb], w=[B_qlt], dma=kl)
                tiles3[j] = (qit, B_qit, qlt, B_qlt)

            def idx_step(j, kb, h):
                qit, B_qit, qlt, B_qlt = tiles3[j]
                ksl = slice(kb * 512, (kb + 1) * 512)
                half = h % 2
                Lp, B_Lp, _ = L_ring.next()
                P.op("pe", lambda e, Lp=Lp, qit=qit, h=h, half=half, ksl=ksl: e.matmul(
                    Lp[:], lhsT=qit[64 * half:64 * half + 64, h // 2, :], rhs=kiT_all[64 * half:64 * half + 64, ksl],
                    start=True, stop=True), r=[B_qit, B_kis], w=[B_Lp])
                P.op("act", lambda e, Lp=Lp: e.activation(out=Lp[:], in_=Lp[:], func=AF.Relu), r=[B_Lp], w=[B_Lp])
                if h == 0:
                    P.op("dve", lambda e, Lp=Lp, ksl=ksl, j=j: e.tensor_scalar(
                        out=Isc[:, ksl], in0=Lp[:], scalar1=wsc[:, j, 0:1], scalar2=None, op0=ALU.mult), r=[B_Lp, B_wsc], w=[B_Ikb[kb]])
                else:
                    P.op("dve", lambda e, Lp=Lp, ksl=ksl, j=j, h=h: e.scalar_tensor_tensor(
                        out=Isc[:, ksl], in0=Lp[:], scalar=wsc[:, j, h:h + 1], in1=Isc[:, ksl], op0=ALU.mult, op1=ALU.add),
                        r=[B_Lp, B_wsc, B_Ikb[kb]], w=[B_Ikb[kb]])

            def emit_bis(j):
                ncol = 8 * (j + 1) * 128
                B_Iall = B_Ikb[0:2 * (j + 1)]
                P.op("dve", lambda e, ncol=ncol: e.tensor_reduce(out=bis[:, BB:BB + 1], in_=Isc[:, 0:ncol], axis=AX.X, op=ALU.max,
                                                               apply_absolute_value=True), r=B_Iall, w=[B_bis])
                P.op("dve", lambda e: e.tensor_scalar(out=bis[:, W0:W0 + 1], in0=bis[:, BB:BB + 1], scalar1=2.0, scalar2=2.0,
                                                      op0=ALU.mult, op1=ALU.add), r=[B_bis], w=[B_bis])
                P.op("dve", lambda e: e.tensor_scalar(out=bis[:, LO:LO + 1], in0=bis[:, BB:BB + 1], scalar1=-1.0, scalar2=-1.0,
                                                      op0=ALU.mult, op1=ALU.add), r=[B_bis], w=[B_bis])
                P.op("dve", lambda e: e.tensor_scalar(out=wk[:], in0=pow2[:], scalar1=bis[:, W0:W0 + 1], scalar2=None, op0=ALU.mult),
                     r=[B_bis, B_pow2], w=[B_bis])
                P.op("dve", lambda e: e.tensor_tensor(out=Isc[:, 0:896], in0=Isc[:, 0:896], in1=vrow3[:], op=ALU.mult), r=B_Ikb[0:2] + [B_v3], w=B_Ikb[0:2])
                P.op("dve", lambda e: e.tensor_tensor(out=Isc[:, 0:896], in0=Isc[:, 0:896], in1=pen7[:], op=ALU.add), r=B_Ikb[0:2] + [B_v3], w=B_Ikb[0:2])
                P.op("dve", lambda e, ncol=ncol: e.memset(Isc[0:64, ncol - 64:ncol], -BIG), r=[B_Iall[-1]], w=[B_Iall[-1]])
                for k in range(NBIS):
                    P.op("dve", lambda e, k=k: e.tensor_tensor(out=bis[:, MID:MID + 1], in0=bis[:, LO:LO + 1], in1=wk[:, k:k + 1], op=ALU.add),
                         r=[B_bis], w=[B_bis])
                    P.op("dve", lambda e, ncol=ncol: e.tensor_scalar(out=junk[:, 0:ncol], in0=Isc[:, 0:ncol], scalar1=bis[:, MID:MID + 1],
                                                                    scalar2=None, op0=ALU.is_ge, op1=ALU.add, accum_out=bis[:, CNT:CNT + 1]),
                         r=B_Iall + [B_bis], w=[B_bis, B_junk])
                    P.op("dve", lambda e, k=k: e.scalar_tensor_tensor(out=bis[:, GW:GW + 1], in0=bis[:, CNT:CNT + 1], scalar=NSEL - 0.5,
                                                                      in1=wk[:, k:k + 1], op0=ALU.is_ge, op1=ALU.mult), r=[B_bis], w=[B_bis])
                    P.op("dve", lambda e: e.tensor_tensor(out=bis[:, LO:LO + 1], in0=bis[:, LO:LO + 1], in1=bis[:, GW:GW + 1], op=ALU.add),
                         r=[B_bis], w=[B_bis])

            def emit_mask(j):
                for kb in range(2 * (j + 1)):
                    ksl = slice(kb * 512, (kb + 1) * 512)
                    mk, B_mk, _ = mk_ring.next()
                    P.op("dve", lambda e, mk=mk, ksl=ksl: e.tensor_scalar(out=mk[:], in0=Isc[:, ksl], scalar1=bis[:, LO:LO + 1], scalar2=None,
                                                                         op0=ALU.is_ge), r=[B_Ikb[kb], B_bis], w=[B_mk])
                    Lp, B_Lp, _ = L_ring.next()
                    Lb = Lp[:].bitcast(BF16)
                    for t4 in range(4):
                        P.op("pe", lambda e, Lb=Lb, mk=mk, t4=t4: e.transpose(out=Lb[:, t4 * 128:(t4 + 1) * 128], in_=mk[:, t4 * 128:(t4 + 1) * 128],
                                                                            identity=ident_b[:]), r=[B_mk, B_const], w=[B_Lp])
                    P.op("act", lambda e, Lb=Lb, kb=kb: e.activation(out=maskT[:, kb * 4:(kb + 1) * 4, :], in_=Lb[:, 0:512].rearrange("p (a b) -> p a b", a=4),
                                                                    func=AF.Identity, scale=30000.0, bias=negbig[:, 0:1]), r=[B_Lp, B_v3], w=[B_mT])

            load3(0)
            for h in range(16):
                for kb in range(2):
                    idx_step(0, kb, h)
            emit_bis(0)
            emit_mask(0)
            for j in range(NOWN):
                nk = 8 * (j + 1)
                qit, B_qit, qlt, B_qlt = tiles3[j]
                pending_idx = []
                if j + 1 < NOWN:
                    load3(j + 1)
                    pending_idx = [(j + 1, kb2 + b, h) for kb2 in range(0, 2 * (j + 2), 2) for h in range(16) for b in range(2)]
                bis_done = [j + 1 >= NOWN]
                SKEW = 2
                inflight = {}
                nit = (16 // HPG) * nk
                per_it = -(-len(pending_idx) // max(1, int(0.45 * nit)))

                def att_s0(it, j=j, nk=nk, qlt=qlt, B_qlt=B_qlt):
                    hg, kt = divmod(it, nk)
                    stp, B_stp, _ = st_ring3.next()
                    for ch in range(2):
                        P.op("pe", lambda e, stp=stp, ch=ch, kt=kt, qlt=qlt, hg=hg: e.matmul(
                            stp, lhsT=cT_all[:, ch, kt * 128:(kt + 1) * 128], rhs=qlt[:, HPG * hg:HPG * hg + HPG, ch, :],
                            start=(ch == 0), stop=False), r=[B_cTs, B_qlt], w=[B_stp])
                    P.op("pe", lambda e, stp=stp, kt=kt: e.matmul(
                        stp, lhsT=ident_b[:], rhs=maskT[:, kt:kt + 1, :].broadcast_to([128, HPG, 128]), start=False, stop=True),
                        r=[B_const, B_mT], w=[B_stp])
                    pT, B_pT, _ = pT_ring.next()
                    P.op("act", lambda e, stp=stp, pT=pT: e.activation(out=pT[:], in_=stp.rearrange("p (a b) -> p a b", a=HPG), func=AF.Exp),
                         r=[B_stp], w=[B_pT])
                    inflight[it] = (pT, B_pT)

                def att_s1(it, j=j, nk=nk):
                    hg, kt = divmod(it, nk)
                    pT, B_pT = inflight.pop(it)
                    for hh in range(HPG):
                        P.op("pe", lambda e, pT=pT, hh=hh, kt=kt, nk=nk: e.matmul(
                            acc[hh][:, 0:257], lhsT=pT[:, hh, :], rhs=c_all[:, kt, :], start=(kt == 0), stop=(kt == nk - 1)),
                            r=[B_pT, B_cs], w=[B_acc[hh]])
                    if kt != nk - 1:
                        return
                    for hh in range(HPG):
                        h = HPG * hg + hh
                        rc, B_rc, _ = rc_ring.next()
                        ol, B_ol, _ = ol_ring.next()
                        P.op("dve", lambda e, rc=rc, hh=hh: e.reciprocal(out=rc[:], in_=acc[hh][:, 256:257]), r=[B_acc[hh]], w=[B_rc])
                        P.op("dve", lambda e, rc=rc, ol=ol, hh=hh: e.tensor_scalar(out=ol[:], in0=acc[hh][:, 0:256], scalar1=rc[:, 0:1], scalar2=None,
                                                                              op0=ALU.mult), r=[B_acc[hh], B_rc], w=[B_ol])
                        Lp, B_Lp, _ = L_ring.next()
                        Lb = Lp[:].bitcast(BF16)
                        for ch in range(2):
                            P.op("pe", lambda e, Lb=Lb, ol=ol, ch=ch: e.transpose(out=Lb[:, ch * 128:(ch + 1) * 128], in_=ol[:, ch * 128:(ch + 1) * 128],
                                                                                identity=ident_b[:]), r=[B_ol, B_const], w=[B_Lp])
                        P.op("act", lambda e, Lb=Lb, h=h: e.copy(out=olatT[:, :, h, :], in_=Lb[:, 0:256].rearrange("p (a b) -> p a b", a=2)),
                             r=[B_Lp], w=[B_olT[h]])

                for step in range(nit + SKEW):
                    if step < nit:
                        att_s0(step)
                    if step - SKEW >= 0:
                        att_s1(step - SKEW)
                    for _ in range(per_it):
                        if pending_idx:
                            idx_step(*pending_idx.pop(0))
                    if not pending_idx and not bis_done[0]:
                        emit_bis(j + 1)
                        bis_done[0] = True
                while pending_idx:
                    idx_step(*pending_idx.pop(0))
                if not bis_done[0]:
                    emit_bis(j + 1)
                ast, B_ast, _ = as_ring.next()
                for h4 in range(4):
                    Lp, B_Lp, _ = L_ring.next()
                    for hh in range(4):
                        h = 4 * h4 + hh
                        for ch in range(2):
                            P.op("pe", lambda e, Lp=Lp, hh=hh, h=h, ch=ch: e.matmul(
                                Lp[:, hh * 128:(hh + 1) * 128], lhsT=wuv[:, ch, h, :], rhs=olatT[:, ch, h, :], start=(ch == 0), stop=(ch == 1)),
                                r=[B_wuv, B_olT[h]], w=[B_Lp])
                    P.op("act", lambda e, Lp=Lp, ast=ast, h4=h4: e.copy(out=ast[:, 4 * h4:4 * h4 + 4, :], in_=Lp[:].rearrange("p (a b) -> p a b", a=4)),
                         r=[B_Lp], w=[B_ast])
                P.op("sp", lambda e, ast=ast, j=j: e.dma_start(out=attnT_d[:, :, j * 128:(j + 1) * 128], in_=ast[:]), r=[B_ast], w=[B_attnT[j]],
                     dma=("attnT_d", j % 4))
                if j + 1 < NOWN:
                    emit_mask(j + 1)

            if debug and stop_after == "3":
                dI = dbg_out("I7", [128, T], F32); dB = dbg_out("bis7", [128, 8], F32)
                P.op("sp", lambda e: e.dma_start(out=dI, in_=Isc[:]), r=B_Ikb, w=[B_dbg], dma=("dbg", 0))
                P.op("sp", lambda e: e.dma_start(out=dB, in_=bis[:]), r=[B_bis], w=[B_dbg], dma=("dbg", 0))

        P.barrier()
        if debug and stop_after == "3":
            d1 = dbg_out("attnT", [128, 16, 1024], BF16)
            P.op("sp", lambda e: e.dma_start(out=d1, in_=attnT_d), r=B_attnT, w=[B_dbg], dma=("dbg", 0))
            return finish_debug()

        w_branch_a = IN("w_branch_a"); w_branch_b = IN("w_branch_b"); w_out = IN("w_out")
        ln1_g = IN("ln1_g"); ln1_b = IN("ln1_b"); w_router = IN("w_router")
        h2_d = dram_scr("h2_d", [NOWN, 128, D], F32)
        B_h2d = [Buf("h2d%d" % j) for j in range(NOWN)]
        h2b = sb("h2b", [128, NOWN, D], BF16); B_h2b = [Buf("h2b%d" % j) for j in range(NOWN)]
        scr = sb("scr", [128, NOWN, NEXP], F32); B_scr = [Buf("scr%d" % j) for j in range(NOWN)]
        wa_v = w_branch_a.rearrange("(k p) n -> p k n", p=128)
        wb_v = w_branch_b.rearrange("(k p) n -> p k n", p=128)
        wo_v = w_out.rearrange("(k p) n -> p k n", p=128)
        with ExitStack() as s5o:
            mergedT = sb("mergedT", [128, KC, 1024], BF16, s5o); B_mg = [[Buf("mg%d_%d" % (m, th)) for th in range(2)] for m in range(KC)]
            with ExitStack() as s5:
                attnT_sb = sb("attnT_sb", [128, KC, 1024], BF16, s5); B_at = Buf("attnT_sb")
                lruT_sb = sb("lruT_sb", [128, KC, 1024], BF16, s5); B_lt = Buf("lruT_sb")
                P.op("sp", lambda e: e.dma_start(out=attnT_sb[:], in_=attnT_d), r=B_attnT, w=[B_at], dma=("attnT_sb", 0))
                P.op("sp", lambda e: e.dma_start(out=lruT_sb[:], in_=lruT_d), r=[b for bb in B_lruT for b in bb], w=[B_lt], dma=("lruT_sb", 0))
                w_ring4 = Ring("wr4", [sb("wr4_%d" % i, [128, KC, 512], BF16, s5) for i in range(2)])
                g_ring = Ring("g4", [sb("g4_%d" % i, [128, 512], BF16, s5) for i in range(4)])
                tA_ring = Ring("tA", [sb("tA%d" % i, [128, 512], F32, s5) for i in range(2)])
                tB_ring = Ring("tB", [sb("tB%d" % i, [128, 512], F32, s5) for i in range(2)])
                pA_ring = Ring("pA", [ps("pA%d" % i, [128, 512], F32, s5) for i in range(3)])
                pB_ring = Ring("pB", [ps("pB%d" % i, [128, 512], F32, s5) for i in range(3)])
                for u in range(4):
                    wa_t, B_wa, kwa = w_ring4.next()
                    wb_t, B_wb, kwb = w_ring4.next()
                    P.op("pool", lambda e, wa_t=wa_t, u=u: e.dma_start(out=wa_t[:], in_=wa_v[:, :, u * 512:(u + 1) * 512]), w=[B_wa], dma=kwa)
                    P.op("pool", lambda e, wb_t=wb_t, u=u: e.dma_start(out=wb_t[:], in_=wb_v[:, :, u * 512:(u + 1) * 512]), w=[B_wb], dma=kwb)
                    for mm in range(4):
                        m = u * 4 + mm
                        for th in range(2):
                            tsl = slice(th * 512, (th + 1) * 512)
                            pA, B_pA, _ = pA_ring.next()
                            pB, B_pB, _ = pB_ring.next()
                            gat, B_gat, kga = g_ring.next()
                            gbt, B_gbt, kgb = g_ring.next()
                            P.op("sp", lambda e, gat=gat, m=m, tsl=tsl: e.dma_start(out=gat[:], in_=gaT_d[:, m, tsl]), r=[B_gaT[m][th]], w=[B_gat], dma=kga)
                            P.op("sp", lambda e, gbt=gbt, m=m, tsl=tsl: e.dma_start(out=gbt[:], in_=gbT_d[:, m, tsl]), r=[B_gbT[m][th]], w=[B_gbt], dma=kgb)
                            for k in range(KC):
                                P.op("pe", lambda e, pA=pA, wa_t=wa_t, k=k, mm=mm, tsl=tsl: e.matmul(
                                    pA[:], lhsT=wa_t[:, k, mm * 128:(mm + 1) * 128], rhs=attnT_sb[:, k, tsl], start=(k == 0), stop=(k == KC - 1)),
                                    r=[B_wa, B_at], w=[B_pA])
                            for k in range(KC):
                                P.op("pe", lambda e, pB=pB, wb_t=wb_t, k=k, mm=mm, tsl=tsl: e.matmul(
                                    pB[:], lhsT=wb_t[:, k, mm * 128:(mm + 1) * 128], rhs=lruT_sb[:, k, tsl], start=(k == 0), stop=(k == KC - 1)),
                                    r=[B_wb, B_lt], w=[B_pB])
                            tA, B_tA, _ = tA_ring.next()
                            tB, B_tB, _ = tB_ring.next()
                            P.op("dve", lambda e, tA=tA, pA=pA, gat=gat: e.tensor_tensor(out=tA[:], in0=pA[:], in1=gat[:], op=ALU.mult), r=[B_pA, B_gat], w=[B_tA])
                            P.op("dve", lambda e, tB=tB, pB=pB, gbt=gbt: e.tensor_tensor(out=tB[:], in0=pB[:], in1=gbt[:], op=ALU.mult), r=[B_pB, B_gbt], w=[B_tB])
                            P.op("pool", lambda e, tA=tA, tB=tB, m=m, tsl=tsl: e.tensor_tensor(out=mergedT[:, m, tsl], in0=tA[:], in1=tB[:], op=ALU.add),
                                 r=[B_tA, B_tB], w=[B_mg[m][th]])
            P.barrier()
            if debug and stop_after == "4a":
                d1 = dbg_out("mergedT", [128, KC, 1024], BF16)
                P.op("sp", lambda e: e.dma_start(out=d1, in_=mergedT[:]), r=[b for bb in B_mg for b in bb], w=[B_dbg], dma=("dbg", 0))
                return finish_debug()
            with ExitStack() as s6:
                wo_sb = sb("wo_sb", [128, KC, D], BF16, s6); B_wo = [Buf("wo%d" % n) for n in range(4)]
                for n in range(4):
                    P.op("pool", lambda e, n=n: e.dma_start(out=wo_sb[:, :, n * 512:(n + 1) * 512], in_=wo_v[:, :, n * 512:(n + 1) * 512]), w=[B_wo[n]], dma=("wo", n))
                g1row = sb("g1row", [128, D], F32, s6); b1row = sb("b1row", [128, D], F32, s6); B_g1 = Buf("g1row")
                P.op("sp", lambda e: e.dma_start(out=g1row[:], in_=ln1_g.partition_broadcast(128)), w=[B_g1], dma=("g1row", 0))
                P.op("sp", lambda e: e.dma_start(out=b1row[:], in_=ln1_b.partition_broadcast(128)), w=[B_g1], dma=("g1row", 0))
                wr_sb = sb("wr_sb", [128, KC, NEXP], F32, s6); B_wr = Buf("wr_sb")
                P.op("sp", lambda e: e.dma_start(out=wr_sb[:], in_=w_router.rearrange("(k p) n -> p k n", p=128)), w=[B_wr], dma=("wr_sb", 0))
                x1_ring = Ring("x1", [sb("x1_%d" % i, [128, D], F32, s6) for i in range(2)])
                ho_ring = Ring("ho4", [sb("ho4_%d" % i, [128, D], F32, s6) for i in range(2)])
                st_ring4 = Ring("st4", [sb("st4_%d" % i, [128, 40], F32, s6) for i in range(2)])
                h2Tf = sb("h2Tf", [128, KC, 128], F32, s6); B_h2Tf = Buf("h2Tf")
                po_ring = Ring("po", [ps("po%d" % i, [128, 512], F32, s6) for i in range(3)])
                tp_ring4 = Ring("tp4", [ps("tp4_%d" % i, [128, 512], F32, s6) for i in range(2)])
                lgps = ps("lgps", [128, NEXP], F32, s6); B_lg = Buf("lgps")
                for i in range(NOWN):
                    isl = slice(i * 128, (i + 1) * 128)
                    hot, B_hot, kho = ho_ring.next()
                    P.op("sp", lambda e, hot=hot, i=i: e.dma_start(out=hot[:], in_=h_own[i]), r=[B_hown[i]], w=[B_hot], dma=kho)
                    x1, B_x1, _ = x1_ring.next()
                    for n in range(4):
                        po, B_po, _ = po_ring.next()
                        for k in range(KC):
                            P.op("pe", lambda e, po=po, k=k, n=n, isl=isl: e.matmul(
                                po[:], lhsT=mergedT[:, k, isl], rhs=wo_sb[:, k, n * 512:(n + 1) * 512], start=(k == 0), stop=(k == KC - 1)),
                                r=[B_mg[k][i // 4], B_wo[n]], w=[B_po])
                        P.op("dve", lambda e, x1=x1, hot=hot, po=po, n=n: e.scalar_tensor_tensor(
                            out=x1[:, n * 512:(n + 1) * 512], in0=hot[:, n * 512:(n + 1) * 512], scalar=ALPHA, in1=po[:], op0=ALU.mult, op1=ALU.add),
                            r=[B_hot, B_po], w=[B_x1])
                    st, B_st, _ = st_ring4.next()
                    for q4 in range(4):
                        P.op("dve", lambda e, st=st, x1=x1, q4=q4: e.bn_stats(out=st[:, q4 * 6:(q4 + 1) * 6], in_=x1[:, q4 * 512:(q4 + 1) * 512]), r=[B_x1], w=[B_st])
                    P.op("dve", lambda e, st=st: e.bn_aggr(out=st[:, 24:26], in_=st[:, 0:24]), r=[B_st], w=[B_st])
                    P.op("act", lambda e, st=st: e.activation(out=st[:, 26:27], in_=st[:, 25:26], func=AF.Sqrt, bias=eps_ln[:, 0:1], scale=1.0), r=[B_st, B_const], w=[B_st])
                    P.op("dve", lambda e, st=st: e.reciprocal(out=st[:, 27:28], in_=st[:, 26:27]), r=[B_st], w=[B_st])
                    P.op("dve", lambda e, st=st, x1=x1: e.tensor_scalar(out=x1[:], in0=x1[:], scalar1=st[:, 24:25], scalar2=st[:, 27:28],
                                                                       op0=ALU.subtract, op1=ALU.mult), r=[B_x1, B_st], w=[B_x1])
                    P.op("pool", lambda e, x1=x1: e.tensor_tensor(out=x1[:], in0=x1[:], in1=g1row[:], op=ALU.mult), r=[B_x1, B_g1], w=[B_x1])
                    P.op("pool", lambda e, x1=x1: e.tensor_tensor(out=x1[:], in0=x1[:], in1=b1row[:], op=ALU.add), r=[B_x1, B_g1], w=[B_x1])
                    P.op("sp", lambda e, x1=x1, i=i: e.dma_start(out=h2_d[i], in_=x1[:]), r=[B_x1], w=[B_h2d[i]], dma=("h2_d", i % 4))
                    P.op("act", lambda e, x1=x1, i=i: e.copy(out=h2b[:, i, :], in_=x1[:]), r=[B_x1], w=[B_h2b[i]])
                    for k4 in range(4):
                        tp, B_tp, _ = tp_ring4.next()
                        for kk in range(4):
                            k = k4 * 4 + kk
                            P.op("pe", lambda e, tp=tp, x1=x1, k=k, kk=kk: e.transpose(out=tp[:, kk * 128:(kk + 1) * 128], in_=x1[:, k * 128:(k + 1) * 128],
                                                                                     identity=ident_f[:]), r=[B_x1, B_const], w=[B_tp])
                        P.op("dve", lambda e, tp=tp, k4=k4: e.tensor_copy(out=h2Tf[:, k4 * 4:(k4 + 1) * 4, :], in_=tp[:].rearrange("p (a b) -> p a b", a=4)),
                             r=[B_tp], w=[B_h2Tf])
                    for k in range(KC):
                        P.op("pe", lambda e, k=k: e.matmul(lgps[:], lhsT=h2Tf[:, k, :], rhs=wr_sb[:, k, :], start=(k == 0), stop=(k == KC - 1)),
                             r=[B_h2Tf, B_wr], w=[B_lg])
                    P.op("act", lambda e, i=i: e.activation(out=scr[:, i, :], in_=lgps[:], func=AF.Sigmoid), r=[B_lg], w=[B_scr[i]])
        P.barrier()
        if debug and stop_after == "4b":
            d1 = dbg_out("h2", [NOWN, 128, D], F32)
            d2 = dbg_out("scr", [128, NOWN, NEXP], F32)
            d3 = dbg_out("h2b", [128, NOWN, D], BF16)
            P.op("sp", lambda e: e.dma_start(out=d1.rearrange("j p d -> p j d"), in_=h2_d.rearrange("j p d -> p j d")), r=B_h2d, w=[B_dbg], dma=("dbg", 0))
            P.op("sp", lambda e: e.dma_start(out=d2, in_=scr[:]), r=B_scr, w=[B_dbg], dma=("dbg", 0))
            P.op("sp", lambda e: e.dma_start(out=d3, in_=h2b[:]), r=B_h2b, w=[B_dbg], dma=("dbg", 0))
            return finish_debug()

        router_bias = IN("router_bias"); w_gate_e = IN("w_gate_e"); w_up_e = IN("w_up_e"); w_down_e = IN("w_down_e")
        w_gate_s = IN("w_gate_s"); w_up_s = IN("w_up_s"); w_down_s = IN("w_down_s"); ln2_g = IN("ln2_g"); ln2_b = IN("ln2_b")
        with ExitStack() as s7:
            yacc = sb("yacc", [128, NOWN, D], F32, s7); B_y = [[Buf("y%d_%d" % (i, n)) for n in range(4)] for i in range(NOWN)]
            iota_row = sb("iota_row", [128, CAP], F32, s7)
            iota_i = sb("iota_i", [128, CAP], I32, s7)
            ones_b = sb("ones_b", [128, 128], BF16, s7); ustr_b = sb("ustr_b", [128, 128], BF16, s7)
            B_c5 = Buf("const5")
            P.op("pool", lambda e: e.iota(iota_i[:], pattern=[[1, CAP]], base=0, channel_multiplier=0), w=[B_c5])
            P.op("pool", lambda e: e.tensor_copy(out=iota_row[:], in_=iota_i[:]), r=[B_c5], w=[B_c5])
            P.op("pool", lambda e: e.tensor_copy(out=ones_b[:], in_=ones_f[:]), r=[B_const], w=[B_c5])
            P.op("pool", lambda e: e.affine_select(out=ustr_b[:], in_=ones_b[:], pattern=[[1, 128]], compare_op=ALU.is_gt, fill=0.0,
                                                   base=0, channel_multiplier=-1), r=[B_c5], w=[B_c5])
            rbias = sb("rbias", [128, NEXP], F32, s7); B_rb = Buf("rbias")
            P.op("sp", lambda e: e.dma_start(out=rbias[:], in_=router_bias.partition_broadcast(128)), w=[B_rb], dma=("rbias", 0))
            rankm = sb("rankm", [128, NOWN, NEXP], F32, s7); gates = sb("gates", [128, NOWN, NEXP], F32, s7)
            B_rt = [Buf("rt%d" % i) for i in range(NOWN)]
            B_gt = [Buf("gt%d" % i) for i in range(NOWN)]
            B_rk = Buf("rankm")
            B_rtt = Buf("rtt")
            BIA, SRT, GS, GSRT, GM, PEN, MBV, TOP, GSEL, SS = 0, 64, 128, 136, 144, 152, 160, 224, 232, 296
            s8 = ExitStack()
            s8.__enter__()
            Mf = sb("Mf", [128, NOWN, NEXP], F32, s8); Mb = sb("Mb", [128, NOWN, NEXP], BF16, s8)
            rt = sb("rt", [128, 400], F32, s8)
            for i in range(NOWN):
                P.op("dve", lambda e, i=i: e.tensor_tensor(out=rt[:, BIA:BIA + 64], in0=scr[:, i, :], in1=rbias[:], op=ALU.add), r=[B_scr[i], B_rb], w=[B_rtt])
                for g in range(8):
                    P.op("dve", lambda e, g=g: e.max(out=rt[:, SRT + 8 * g:SRT + 8 * g + 8], in_=rt[:, BIA + 8 * g:BIA + 8 * g + 8]), r=[B_rtt], w=[B_rtt])
                srt3 = rt[:, SRT:SRT + 64].rearrange("p (g k) -> p g k", g=8)
                P.op("dve", lambda e, srt3=srt3: e.tensor_tensor(out=rt[:, GS:GS + 8], in0=srt3[:, :, 0], in1=srt3[:, :, 1], op=ALU.add), r=[B_rtt], w=[B_rtt])
                P.op("dve", lambda e: e.max(out=rt[:, GSRT:GSRT + 8], in_=rt[:, GS:GS + 8]), r=[B_rtt], w=[B_rtt])
                P.op("dve", lambda e: e.tensor_scalar(out=rt[:, GM:GM + 8], in0=rt[:, GS:GS + 8], scalar1=rt[:, GSRT + 3:GSRT + 4], scalar2=None, op0=ALU.is_ge),
                     r=[B_rtt], w=[B_rtt])
                P.op("dve", lambda e: e.tensor_scalar(out=rt[:, PEN:PEN + 8], in0=rt[:, GM:GM + 8], scalar1=-1.0, scalar2=BIG, op0=ALU.add, op1=ALU.mult),
                     r=[B_rtt], w=[B_rtt])
                bia3 = rt[:, BIA:BIA + 64].rearrange("p (g k) -> p g k", g=8)
                mb3 = rt[:, MBV:MBV + 64].rearrange("p (g k) -> p g k", g=8)
                gm3 = rt[:, GM:GM + 8].unsqueeze(2).broadcast_to([128, 8, 8])
                pen3 = rt[:, PEN:PEN + 8].unsqueeze(2).broadcast_to([128, 8, 8])
                P.op("dve", lambda e, mb3=mb3, bia3=bia3, gm3=gm3: e.tensor_tensor(out=mb3, in0=bia3, in1=gm3, op=ALU.mult), r=[B_rtt], w=[B_rtt])
                P.op("dve", lambda e, mb3=mb3, pen3=pen3: e.tensor_tensor(out=mb3, in0=mb3, in1=pen3, op=ALU.add), r=[B_rtt], w=[B_rtt])
                P.op("dve", lambda e: e.max(out=rt[:, TOP:TOP + 8], in_=rt[:, MBV:MBV + 64]), r=[B_rtt], w=[B_rtt])
                P.op("dve", lambda e, i=i: e.tensor_scalar(out=Mf[:, i, :], in0=rt[:, MBV:MBV + 64], scalar1=rt[:, TOP + 7:TOP + 8], scalar2=None, op0=ALU.is_ge),
                     r=[B_rtt], w=[B_rt[i]])
                P.op("dve", lambda e, i=i: e.tensor_copy(out=Mb[:, i, :], in_=Mf[:, i, :]), r=[B_rt[i]], w=[B_rt[i]])
                P.op("dve", lambda e, i=i: e.tensor_tensor(out=rt[:, GSEL:GSEL + 64], in0=scr[:, i, :], in1=Mf[:, i, :], op=ALU.mult), r=[B_rt[i], B_scr[i]], w=[B_rtt])
                P.op("dve", lambda e: e.tensor_reduce(out=rt[:, SS:SS + 1], in_=rt[:, GSEL:GSEL + 64], axis=AX.X, op=ALU.add), r=[B_rtt], w=[B_rtt])
                P.op("dve", lambda e: e.reciprocal(out=rt[:, SS + 1:SS + 2], in_=rt[:, SS:SS + 1]), r=[B_rtt], w=[B_rtt])
                P.op("dve", lambda e, i=i: e.tensor_scalar(out=gates[:, i, :], in0=rt[:, GSEL:GSEL + 64], scalar1=rt[:, SS + 1:SS + 2], scalar2=2.5,
                                                           op0=ALU.mult, op1=ALU.mult), r=[B_rtt], w=[B_gt[i]])
            if True:
                rkps = ps("rkps", [128, NEXP], F32, s8); B_rkps = Buf("rkps")
                for i in range(NOWN):
                    if i % 2 == 1:
                        P.op("pe", lambda e, i=i: e.matmul(rkps[:], lhsT=ones_b[:], rhs=Mb[:, i - 1, :], start=True, stop=False),
                             r=[B_c5, B_rt[i - 1]], w=[B_rkps])
                    P.op("pe", lambda e, i=i: e.matmul(rkps[:], lhsT=ustr_b[:], rhs=Mb[:, i, :], start=(i % 2 == 0), stop=True), r=[B_c5, B_rt[i]], w=[B_rkps])
                    P.op("dve", lambda e, i=i: e.scalar_tensor_tensor(out=rankm[:, i, :], in0=rkps[:], scalar=1.0 + 64.0 * ((i // 2) % 2), in1=Mf[:, i, :],
                                                                      op0=ALU.add, op1=ALU.mult), r=[B_rkps, B_rt[i]], w=[B_rk])
                    P.op("dve", lambda e, i=i: e.tensor_scalar(out=rankm[:, i, :], in0=rankm[:, i, :], scalar1=-1.0, scalar2=None, op0=ALU.add), r=[B_rk], w=[B_rk])
                h2T_sb = sb("h2T_sb", [128, KC, 1024], BF16, s8); B_h2T = Buf("h2T")
                wsg = sb("wsg", [128, KC, 512], BF16, s8); wsu = sb("wsu", [128, KC, 512], BF16, s8); wsd = sb("wsd", [128, 4, D], BF16, s8)
                B_ws = Buf("ws")
                P.op("pool", lambda e: e.dma_start(out=wsg[:], in_=w_gate_s.rearrange("(k p) n -> p k n", p=128)), w=[B_ws], dma=("ws", 0))
                P.op("pool", lambda e: e.dma_start(out=wsu[:], in_=w_up_s.rearrange("(k p) n -> p k n", p=128)), w=[B_ws], dma=("ws", 0))
                P.op("pool", lambda e: e.dma_start(out=wsd[:], in_=w_down_s.rearrange("(k p) n -> p k n", p=128)), w=[B_ws], dma=("ws", 0))
                actTs = sb("actTs", [128, 4, 1024], BF16, s8); B_acts = Buf("actTs")
                sg_ring = Ring("sgs", [sb("sgs%d" % i, [128, 512], F32, s8) for i in range(2)])
                tpb = ps("tpb", [128, 512], F32, s8); B_tpb = Buf("tpb")
                pg_ring = Ring("pgs", [ps("pgs%d" % i, [128, 512], F32, s8) for i in range(2)])
                pu_ring = Ring("pus", [ps("pus%d" % i, [128, 512], F32, s8) for i in range(2)])
                pd_ring = Ring("pds", [ps("pds%d" % i, [128, 512], F32, s8) for i in range(2)])
                tpbb = tpb[:].bitcast(BF16)
                for i in range(NOWN):
                    for k4 in range(2):
                        for kk in range(8):
                            k = k4 * 8 + kk
                            P.op("pe", lambda e, i=i, k=k, kk=kk: e.transpose(out=tpbb[:, kk * 128:(kk + 1) * 128], in_=h2b[:, i, k * 128:(k + 1) * 128],
                                                                            identity=ident_b[:]), r=[B_h2b[i], B_const], w=[B_tpb])
                        P.op("act", lambda e, i=i, k4=k4: e.copy(out=h2T_sb[:, k4 * 8:(k4 + 1) * 8, i * 128:(i + 1) * 128],
                                                                in_=tpbb.rearrange("p (a b) -> p a b", a=8)), r=[B_tpb], w=[B_h2T])
                for m in range(4):
                    for th in range(2):
                        tsl = slice(th * 512, (th + 1) * 512)
                        pg, B_pg, _ = pg_ring.next(); pu, B_pu, _ = pu_ring.next()
                        for k in range(KC):
                            P.op("pe", lambda e, pg=pg, k=k, m=m, tsl=tsl: e.matmul(pg[:], lhsT=wsg[:, k, m * 128:(m + 1) * 128], rhs=h2T_sb[:, k, tsl],
                                                                                 start=(k == 0), stop=(k == KC - 1)), r=[B_ws, B_h2T], w=[B_pg])
                        for k in range(KC):
                            P.op("pe", lambda e, pu=pu, k=k, m=m, tsl=tsl: e.matmul(pu[:], lhsT=wsu[:, k, m * 128:(m + 1) * 128], rhs=h2T_sb[:, k, tsl],
                                                                                 start=(k == 0), stop=(k == KC - 1)), r=[B_ws, B_h2T], w=[B_pu])
                        sg, B_sg, _ = sg_ring.next()
                        P.op("act", lambda e, sg=sg, pg=pg: e.activation(out=sg[:], in_=pg[:], func=AF.Silu), r=[B_pg], w=[B_sg])
                        P.op("dve", lambda e, sg=sg, pu=pu, m=m, tsl=tsl: e.tensor_tensor(out=actTs[:, m, tsl], in0=sg[:], in1=pu[:], op=ALU.mult),
                             r=[B_sg, B_pu], w=[B_acts])
                for i in range(NOWN):
                    for n in range(4):
                        pd, B_pd, _ = pd_ring.next()
                        for kf in range(4):
                            P.op("pe", lambda e, pd=pd, kf=kf, i=i, n=n: e.matmul(pd[:], lhsT=actTs[:, kf, i * 128:(i + 1) * 128], rhs=wsd[:, kf, n * 512:(n + 1) * 512],
                                                                               start=(kf == 0), stop=(kf == 3)), r=[B_acts, B_ws], w=[B_pd])
                        if (i * 4 + n) % 2 == 0:
                            P.op("act", lambda e, pd=pd, i=i, n=n: e.copy(out=yacc[:, i, n * 512:(n + 1) * 512], in_=pd[:]), r=[B_pd], w=[B_y[i][n]])
                        else:
                            P.op("dve", lambda e, pd=pd, i=i, n=n: e.tensor_copy(out=yacc[:, i, n * 512:(n + 1) * 512], in_=pd[:]), r=[B_pd], w=[B_y[i][n]])
            s8.__exit__(None, None, None)
            P.barrier()
            if debug and stop_after == "5a":
                d1 = dbg_out("gates", [128, NOWN, NEXP], F32); d2 = dbg_out("rankm", [128, NOWN, NEXP], F32); d3 = dbg_out("ysh", [128, NOWN, D], F32)
                P.op("sp", lambda e: e.dma_start(out=d1, in_=gates[:]), r=B_gt, w=[B_dbg], dma=("dbg", 0))
                P.op("sp", lambda e: e.dma_start(out=d2, in_=rankm[:]), r=[B_rk], w=[B_dbg], dma=("dbg", 0))
                P.op("sp", lambda e: e.dma_start(out=d3, in_=yacc[:]), r=[b for bb in B_y for b in bb], w=[B_dbg], dma=("dbg", 0))
                return finish_debug()
            GCAP = 128
            with ExitStack() as s9:
                NE = NEXP if not (debug and stop_after == "5b") else 4
                wring = Ring("we", [sb("we%d" % i, [128, 8192], BF16, s9) for i in range(4)])
                S_ring = Ring("S", [sb("S_sb%d" % i, [128, NOWN, GCAP], BF16, s9) for i in range(2)])
                SW_ring = Ring("SW", [sb("SW_sb%d" % i, [128, NOWN, GCAP], BF16, s9) for i in range(2)])
                XgT = sb("XgT", [128, KC, CAP], BF16, s9); B_Xg = [Buf("Xg%d" % k) for k in range(8)]
                actT = sb("actT", [128, 4, CAP], BF16, s9); B_actT = Buf("actT")
                sg_r = Ring("sgt", [sb("sgt%d" % i, [128, CAP], F32, s9) for i in range(2)])
                Y_sb = sb("Y_sb", [128, 2, D], BF16, s9); B_Y = Buf("Y")
                SWT = sb("SWT", [128, 2, 512], BF16, s9); B_SWT = Buf("SWT")
                ga_ring5 = Ring("gat", [ps("gat%d" % i, [128, 512], F32, s9) for i in range(2)])
                gu_ring = Ring("gup", [ps("gup%d" % i, [128, 512], F32, s9) for i in range(2)])
                dn_ring = Ring("dnp", [ps("dnp%d" % i, [128, 512], F32, s9) for i in range(3)])
                swps = ps("swps", [128, 512], F32, s9); B_swps = Buf("swps")
                sc_ring = dn_ring
                swb = swps[:].bitcast(BF16)
                ev = [0]
                pend = {}

                def load_w(kind, e_):
                    wt, B_w, kw = wring.next()
                    if kind == "d":
                        P.op("pool", lambda e, wt=wt, e_=e_: e.dma_start(out=wt[:].rearrange("p (k n) -> p k n", k=4),
                                                                        in_=w_down_e[e_].rearrange("(k p) n -> p k n", p=128)), w=[B_w], dma=kw)
                        pend.setdefault(e_, {})["wd"] = (wt[:].rearrange("p (k n) -> p k n", k=4), B_w)
                    else:
                        src = w_gate_e if kind == "g" else w_up_e
                        P.op("pool", lambda e, wt=wt, e_=e_, src=src: e.dma_start(out=wt[:].rearrange("p (k n) -> p k n", k=KC),
                                                                                 in_=src[e_].rearrange("(k p) n -> p k n", p=128)), w=[B_w], dma=kw)
                        pend.setdefault(e_, {})["w" + kind] = (wt[:].rearrange("p (k n) -> p k n", k=KC), B_w)

                def E_prep(e_):
                    S_sb, B_S, _ = S_ring.next()
                    SW_sb, B_SW, _ = SW_ring.next()
                    for i in range(NOWN):
                        P.op("dve", lambda e, i=i, e_=e_, S_sb=S_sb: e.tensor_scalar(out=S_sb[:, i, :], in0=iota_row[:, 0:GCAP], scalar1=rankm[:, i, e_:e_ + 1],
                                                                                   scalar2=None, op0=ALU.is_equal), r=[B_c5, B_rk], w=[B_S])
                    for i in range(NOWN):
                        P.op("act", lambda e, i=i, e_=e_, S_sb=S_sb, SW_sb=SW_sb: e.activation(out=SW_sb[:, i, :], in_=S_sb[:, i, :], func=AF.Copy,
                                                                                             scale=gates[:, i, e_:e_ + 1]), r=[B_S, B_gt[i]], w=[B_SW])
                    pend.setdefault(e_, {}).update(S=S_sb, B_S=B_S, SW=SW_sb, B_SW=B_SW)

                def E_gather(e_):
                    d_ = pend[e_]
                    S_sb, B_S = d_["S"], d_["B_S"]
                    for k2 in range(8):
                        gp, B_gp, _ = ga_ring5.next()
                        for kk in range(2):
                            k = 2 * k2 + kk
                            for i in range(NOWN):
                                c0 = kk * CAP + GCAP * (i // 4)
                                P.op("pe", lambda e, gp=gp, c0=c0, k=k, i=i, S_sb=S_sb: e.matmul(gp[:, c0:c0 + GCAP], lhsT=h2b[:, i, k * 128:(k + 1) * 128],
                                                                                            rhs=S_sb[:, i, :], start=(i % 4 == 0), stop=(i % 4 == 3)),
                                     r=[B_h2b[i], B_S], w=[B_gp])
                        ev[0] += 1
                        if ev[0] % 2 == 0:
                            P.op("act", lambda e, gp=gp, k2=k2: e.copy(out=XgT[:, 2 * k2:2 * k2 + 2, :], in_=gp[:].rearrange("p (a b) -> p a b", a=2)), r=[B_gp], w=[B_Xg[k2]])
                        else:
                            P.op("dve", lambda e, gp=gp, k2=k2: e.tensor_copy(out=XgT[:, 2 * k2:2 * k2 + 2, :], in_=gp[:].rearrange("p (a b) -> p a b", a=2)), r=[B_gp], w=[B_Xg[k2]])

                def E_swt(e_):
                    d_ = pend[e_]
                    SW_sb, B_SW = d_["SW"], d_["B_SW"]
                    for cs in range(2):
                        for il in range(4):
                            i = 4 * cs + il
                            P.op("pe", lambda e, il=il, i=i, SW_sb=SW_sb: e.transpose(out=swb[:, il * 128:(il + 1) * 128], in_=SW_sb[:, i, :],
                                                                                   identity=ident_b[:]), r=[B_SW, B_const], w=[B_swps])
                        P.op("act", lambda e, cs=cs: e.copy(out=SWT[:, cs, :], in_=swb[:, 0:512]), r=[B_swps], w=[B_SWT])

                def E_gu(e_):
                    d_ = pend[e_]
                    wg3, B_wg = d_["wg"]; wu3, B_wu = d_["wu"]
                    for m in range(4):
                        gup, B_gu, _ = gu_ring.next()
                        for k in range(KC):
                            P.op("pe", lambda e, gup=gup, k=k, m=m, wg3=wg3: e.matmul(gup[:, 0:CAP], lhsT=wg3[:, k, m * 128:(m + 1) * 128], rhs=XgT[:, k, :],
                                                                                   start=(k == 0), stop=(k == KC - 1)), r=[B_wg, B_Xg[k // 2]], w=[B_gu])
                        for k in range(KC):
                            P.op("pe", lambda e, gup=gup, k=k, m=m, wu3=wu3: e.matmul(gup[:, CAP:2 * CAP], lhsT=wu3[:, k, m * 128:(m + 1) * 128], rhs=XgT[:, k, :],
                                                                                   start=(k == 0), stop=(k == KC - 1)), r=[B_wu, B_Xg[k // 2]], w=[B_gu])
                        sgt, B_sgt, _ = sg_r.next()
                        P.op("act", lambda e, gup=gup, sgt=sgt: e.activation(out=sgt[:], in_=gup[:, 0:CAP], func=AF.Silu), r=[B_gu], w=[B_sgt])
                        P.op("dve", lambda e, gup=gup, sgt=sgt, m=m: e.tensor_tensor(out=actT[:, m, :], in0=sgt[:], in1=gup[:, CAP:2 * CAP], op=ALU.mult),
                             r=[B_sgt, B_gu], w=[B_actT])

                def E_down(e_):
                    d_ = pend[e_]
                    wd3, B_wd = d_["wd"]
                    for cs in range(2):
                        for n in range(4):
                            dn, B_dn, _ = dn_ring.next()
                            for kf in range(4):
                                P.op("pe", lambda e, dn=dn, kf=kf, cs=cs, n=n, wd3=wd3: e.matmul(dn[:], lhsT=actT[:, kf, cs * 128:(cs + 1) * 128],
                                                                                              rhs=wd3[:, kf, n * 512:(n + 1) * 512], start=(kf == 0), stop=(kf == 3)),
                                     r=[B_actT, B_wd], w=[B_dn])
                            ev[0] += 1
                            if ev[0] % 2 == 0:
                                P.op("act", lambda e, dn=dn, cs=cs, n=n: e.copy(out=Y_sb[:, cs, n * 512:(n + 1) * 512], in_=dn[:]), r=[B_dn], w=[B_Y])
                            else:
                                P.op("dve", lambda e, dn=dn, cs=cs, n=n: e.tensor_copy(out=Y_sb[:, cs, n * 512:(n + 1) * 512], in_=dn[:]), r=[B_dn], w=[B_Y])

                def E_scatter(e_):
                    for i in range(NOWN):
                        cs = i // 4
                        pb = 64 * ((i // 2) % 2)
                        il = i % 4
                        for n in range(4):
                            scp, B_scp, _ = sc_ring.next()
                            P.op("pe", lambda e, scp=scp, cs=cs, pb=pb, il=il, n=n: e.matmul(
                                scp[:], lhsT=SWT[:, cs, il * 128:(il + 1) * 128], rhs=Y_sb[:, cs, n * 512:(n + 1) * 512],
                                start=True, stop=True), r=[B_SWT, B_Y], w=[B_scp])
                            P.op("dve", lambda e, scp=scp, i=i, n=n: e.tensor_tensor(out=yacc[:, i, n * 512:(n + 1) * 512], in0=scp[:],
                                                                                  in1=yacc[:, i, n * 512:(n + 1) * 512], op=ALU.add), r=[B_scp, B_y[i][n]], w=[B_y[i][n]])
                    pend.pop(e_, None)

                load_w("g", 0); load_w("u", 0); load_w("d", 0)
                E_prep(0)
                E_gather(0)
                for e_ in range(NE):
                    E_swt(e_)
                    if e_ + 1 < NE:
                        load_w("g", e_ + 1)
                        E_prep(e_ + 1)
                    E_gu(e_)
                    if e_ + 1 < NE:
                        load_w("u", e_ + 1); load_w("d", e_ + 1)
                    E_down(e_)
                    if e_ + 1 < NE:
                        E_gather(e_ + 1)
                    E_scatter(e_)
            P.barrier()
            with ExitStack() as s10:
                g2row = sb("g2row", [128, D], F32, s10); b2row = sb("b2row", [128, D], F32, s10); B_g2 = Buf("g2row")
                P.op("sp", lambda e: e.dma_start(out=g2row[:], in_=ln2_g.partition_broadcast(128)), w=[B_g2], dma=("g2row", 0))
                P.op("sp", lambda e: e.dma_start(out=b2row[:], in_=ln2_b.partition_broadcast(128)), w=[B_g2], dma=("g2row", 0))
                h2_ring = Ring("h2r", [sb("h2r%d" % i, [128, D], F32, s10) for i in range(2)])
                st_ring5 = Ring("st5", [sb("st5_%d" % i, [128, 40], F32, s10) for i in range(2)])
                B_out = Buf("out", wo=True)
                for i in range(NOWN):
                    h2t, B_h2t, kh2 = h2_ring.next()
                    P.op("sp", lambda e, h2t=h2t, i=i: e.dma_start(out=h2t[:], in_=h2_d[i]), r=[B_h2d[i]], w=[B_h2t], dma=kh2)
                    P.op("dve", lambda e, h2t=h2t, i=i: e.scalar_tensor_tensor(out=h2t[:], in0=h2t[:], scalar=ALPHA, in1=yacc[:, i, :], op0=ALU.mult, op1=ALU.add),
                         r=[B_h2t] + B_y[i], w=[B_h2t])
                    st, B_st, _ = st_ring5.next()
                    for q4 in range(4):
                        P.op("dve", lambda e, st=st, h2t=h2t, q4=q4: e.bn_stats(out=st[:, q4 * 6:(q4 + 1) * 6], in_=h2t[:, q4 * 512:(q4 + 1) * 512]), r=[B_h2t], w=[B_st])
                    P.op("dve", lambda e, st=st: e.bn_aggr(out=st[:, 24:26], in_=st[:, 0:24]), r=[B_st], w=[B_st])
                    P.op("act", lambda e, st=st: e.activation(out=st[:, 26:27], in_=st[:, 25:26], func=AF.Sqrt, bias=eps_ln[:, 0:1], scale=1.0), r=[B_st, B_const], w=[B_st])
                    P.op("dve", lambda e, st=st: e.reciprocal(out=st[:, 27:28], in_=st[:, 26:27]), r=[B_st], w=[B_st])
                    P.op("dve", lambda e, st=st, h2t=h2t: e.tensor_scalar(out=h2t[:], in0=h2t[:], scalar1=st[:, 24:25], scalar2=st[:, 27:28],
                                                                         op0=ALU.subtract, op1=ALU.mult), r=[B_h2t, B_st], w=[B_h2t])
                    P.op("pool", lambda e, h2t=h2t: e.tensor_tensor(out=h2t[:], in0=h2t[:], in1=g2row[:], op=ALU.mult), r=[B_h2t, B_g2], w=[B_h2t])
                    P.op("pool", lambda e, h2t=h2t: e.tensor_tensor(out=h2t[:], in0=h2t[:], in1=b2row[:], op=ALU.add), r=[B_h2t, B_g2], w=[B_h2t])
                    P.op("sp", lambda e, h2t=h2t, i=i: e.dma_start(out=out[i * 128:(i + 1) * 128, :], in_=h2t[:]), r=[B_h2t], w=[B_out], dma=("out", 0))
                P.op("sp", None, r=[B_out])

        P.emit()
    return nc, dbg, list(declared)


def make_in_maps(inputs, names=None, cores=None):
    x = np.ascontiguousarray(np.asarray(inputs["x"], dtype=np.float32).reshape(T, D))
    shared = {}
    for k, v in inputs.items():
        if k == "x":
            continue
        a = np.asarray(v, dtype=np.float32)
        if a.ndim >= 2 and a.shape[0] == 1 and k not in ("ln_in_g", "ln_in_b"):
            a = a[0]
        shared[k] = np.ascontiguousarray(a)
    maps = []
    for c in (range(NCORES) if cores is None else cores):
        pad = (7 - c) * 128
        xw = np.zeros((T, D), np.float32)
        xw[pad:] = x[:T - pad]
        v7 = np.zeros((1, 896), np.float32)
        v7[0, pad:] = 1.0
        m = dict(shared)
        m["xw"] = xw
        m["valid7"] = v7
        if names is not None:
            m = {k: v for k, v in m.items() if k in names}
        maps.append(m)
    return maps


def kernel(**inputs):
    nc, _, _ = build_program()
    maps = make_in_maps(inputs)
    res = run_bass_kernel_spmd(nc, maps, core_ids=list(range(NCORES)))
    full = np.zeros((T, D), np.float32)
    for c in range(NCORES):
        o = res.results[c]["out"].reshape(NOWN, 128, D)
        for j in range(NOWN):
            g = c + 8 * j
            full[g * 128:(g + 1) * 128] = o[j]
    return full.reshape(1, T, D)
```

```python
import os
from contextlib import ExitStack

import numpy as np
import concourse.bass as bass
import concourse.mybir as mybir
from concourse.bass_utils import run_bass_kernel_spmd

F32 = mybir.dt.float32
BF16 = mybir.dt.bfloat16
U32 = mybir.dt.uint32
I32 = mybir.dt.int32
ALU = mybir.AluOpType
AF = mybir.ActivationFunctionType
AX = mybir.AxisListType

NCORES = 8
D = 2048
T = 8192
NT = 64
NOWN = 8
KC = 16
PROJ = 11600
C_Q, C_C, C_QI, C_KI, C_WI, C_XR, C_YG, C_GA, C_GB = 0, 2048, 2304, 3328, 3392, 3408, 5456, 7504, 9552
LN_EPS = 1e-5
RMS_EPS = 1e-6
ALPHA = 2.0 ** 0.25
ATTN_SCALE = 128 ** -0.5
BIG = 1.0e30
NSEL = 256
NBIS = 14
CAP = 256
NEXP = 64


class Buf:
    __slots__ = ("name", "lw", "rd", "wo")

    def __init__(self, name, wo=False):
        self.name = name
        self.lw = None
        self.rd = []
        self.wo = wo


class Ins:
    __slots__ = ("eng", "fn", "deps", "dma", "sig", "ticket", "dsem", "dval", "idx")


class Prog:
    ENGS = ("pe", "act", "dve", "pool", "sp")

    def __init__(self, nc, es):
        self.nc = nc
        self.es = es
        self.lists = {e: [] for e in self.ENGS}
        self.n = 0
        self.psem = {e: es.enter_context(nc.semaphore("prog_" + e)) for e in ("pe", "act", "dve", "pool")}
        self.bar_pos = {}
        self.free_sems = []
        self.dma_sems = {}
        self.nsem = 0

    def _dsem(self, key):
        if key not in self.dma_sems:
            if self.free_sems:
                self.dma_sems[key] = self.free_sems.pop()
            else:
                self.dma_sems[key] = [self.es.enter_context(self.nc.semaphore("dma%d" % self.nsem)), 0]
                self.nsem += 1
        return self.dma_sems[key]

    def barrier(self):
        lasts = []
        for e in self.ENGS:
            comp = [i for i in self.lists[e] if i.dma is None and i.fn is not None]
            if comp:
                lasts.append(comp[-1])
        dmas = [i for e in self.ENGS for i in self.lists[e][self.bar_pos.get(e, 0):] if i.dma is not None]
        for e in self.ENGS:
            self.bar_pos[e] = len(self.lists[e])
        for e in self.ENGS:
            self.op(e, None, extra=lasts + dmas)
        self.free_sems.extend(self.dma_sems.values())
        self.dma_sems = {}

    def op(self, eng, fn, r=(), w=(), dma=None, extra=()):
        ins = Ins()
        ins.eng, ins.fn, ins.dma, ins.sig, ins.ticket = eng, fn, dma, False, 0
        ins.idx = self.n
        self.n += 1
        deps = list(extra)
        for b in r:
            if b.lw is not None:
                deps.append(b.lw)
        for b in w:
            if b.wo:
                continue
            if b.lw is not None:
                deps.append(b.lw)
            deps.extend(b.rd)
        seen = set()
        ins.deps = []
        for d in deps:
            if d.idx in seen or d is ins:
                continue
            seen.add(d.idx)
            if d.eng == "pe" and eng == "pe" and d.dma is None and dma is None:
                continue
            ins.deps.append(d)
            d.sig = True
        for b in r:
            b.rd.append(ins)
        for b in w:
            b.lw = ins
            b.rd = []
        if dma is not None:
            s = self._dsem(dma)
            s[1] += 16
            ins.dsem, ins.dval = s[0], s[1]
        self.lists[eng].append(ins)
        return ins

    def emit(self):
        nc = self.nc
        for e in ("pe", "act", "dve", "pool"):
            t = 0
            for ins in self.lists[e]:
                if ins.dma is None and ins.sig:
                    t += 1
                    ins.ticket = t

        def run(ename, eng):
            waited = {}
            for ins in self.lists[ename]:
                for d in ins.deps:
                    if d.dma is not None:
                        sem, val = d.dsem, d.dval
                    else:
                        sem, val = self.psem[d.eng], d.ticket
                    k = id(sem)
                    if waited.get(k, 0) >= val:
                        continue
                    waited[k] = val
                    eng.wait_ge(sem, val)
                if ins.fn is None:
                    continue
                res = ins.fn(eng)
                if ins.dma is not None:
                    res.then_inc(ins.dsem, 16)
                elif ins.sig:
                    res.then_inc(self.psem[ename], 1)

        with nc.Block() as block:
            @block.sync
            def _(e):
                run("sp", e)

            @block.scalar
            def _(e):
                run("act", e)

            @block.vector
            def _(e):
                run("dve", e)

            @block.gpsimd
            def _(e):
                run("pool", e)

            @block.tensor
            def _(e):
                run("pe", e)


class Ring:
    def __init__(self, name, tiles):
        self.tiles = tiles
        self.bufs = [Buf("%s%d" % (name, i)) for i in range(len(tiles))]
        self.i = 0
        self.name = name

    def next(self):
        k = self.i % len(self.tiles)
        self.i += 1
        return self.tiles[k], self.bufs[k], (self.name, k)


def build_program(stop_after=None, debug=False):
    nc = bass.Bass("TRN2", target_bir_lowering=False)
    es = ExitStack()
    with es:
        P = Prog(nc, es)

        def dram_in(name, shape, dt=F32):
            return nc.dram_tensor(name, list(shape), dt, kind="ExternalInput").ap()

        def dram_scr(name, shape, dt):
            return nc.dram_tensor(name, list(shape), dt, kind="Internal").ap()

        def sb(name, shape, dt, stack=None):
            return (stack or es).enter_context(nc.sbuf_tensor(name, list(shape), dt))

        def ps(name, shape, dt, stack=None):
            return (stack or es).enter_context(nc.psum_tensor(name, list(shape), dt))

        SHAPES = {
            "xw": [T, D], "valid7": [1, 896], "ln_in_g": [D], "ln_in_b": [D], "w_in": [D, PROJ],
            "kv_norm_g": [256], "w_uk": [16, 128, 256], "w_uv": [16, 256, 128], "conv_w": [4, D],
            "conv_b": [D], "w_rg_a": [16, 128, 128], "b_rg_a": [D], "w_rg_x": [16, 128, 128],
            "b_rg_x": [D], "rg_lambda": [D], "w_branch_a": [D, D], "w_branch_b": [D, D], "w_out": [D, D],
            "ln1_g": [D], "ln1_b": [D], "w_router": [D, NEXP], "router_bias": [NEXP],
            "w_gate_e": [NEXP, D, 512], "w_up_e": [NEXP, D, 512], "w_down_e": [NEXP, 512, D],
            "w_gate_s": [D, 512], "w_up_s": [D, 512], "w_down_s": [512, D], "ln2_g": [D], "ln2_b": [D],
        }
        declared = {}

        def IN(name):
            if name not in declared:
                declared[name] = dram_in(name, SHAPES[name])
            return declared[name]

        if not debug:
            for nm in SHAPES:
                IN(nm)
        xw = IN("xw"); ln_in_g = IN("ln_in_g"); ln_in_b = IN("ln_in_b"); w_in = IN("w_in")
        kv_norm_g = IN("kv_norm_g"); conv_w = IN("conv_w"); conv_b = IN("conv_b")
        b_rg_a = IN("b_rg_a"); b_rg_x = IN("b_rg_x"); rg_lambda = IN("rg_lambda")
        out = nc.dram_tensor("out", [NOWN * 128, D], F32, kind="ExternalOutput").ap()
        dbg = {}
        B_dbg = Buf("dbg", wo=True)

        def dbg_out(name, shape, dt=F32):
            t = nc.dram_tensor("dbg_" + name, list(shape), dt, kind="ExternalOutput").ap()
            dbg[name] = t
            return t

        hT_all = dram_scr("hT_all", [16, 128, KC, 512], BF16)
        h_own = dram_scr("h_own", [NOWN, 128, D], F32)
        lruh = dram_scr("lruh", [NOWN, 128, KC, 128], F32)
        B_hT = [Buf("hT_all%d" % b) for b in range(16)]
        B_hown = [Buf("h_own%d" % j) for j in range(NOWN)]
        B_lruh = [[Buf("lruh%d_%d" % (j, p)) for p in range(2)] for j in range(NOWN)]

        ident_f = sb("ident_f", [128, 128], F32)
        ident_b = sb("ident_b", [128, 128], BF16)
        ones_f = sb("ones_f", [128, 128], F32)
        B_const = Buf("const")
        eps_ln = sb("eps_ln", [128, 1], F32)
        eps_rms = sb("eps_rms", [128, 1], F32)
        P.op("pool", lambda e: e.memset(eps_ln[:], LN_EPS), w=[B_const])
        P.op("pool", lambda e: e.memset(eps_rms[:], RMS_EPS), w=[B_const])

        P.op("pool", lambda e: e.memset(ones_f[:], 1.0), w=[B_const])
        P.op("pool", lambda e: e.affine_select(out=ident_f[:], in_=ones_f[:], pattern=[[-1, 128]],
                                               compare_op=ALU.is_equal, fill=0.0, base=0,
                                               channel_multiplier=1), r=[B_const], w=[B_const])
        P.op("pool", lambda e: e.tensor_copy(out=ident_b[:], in_=ident_f[:]), r=[B_const], w=[B_const])

        PAR = [ln_in_g, ln_in_b, conv_w[0], conv_w[1], conv_w[2], conv_w[3], conv_b, b_rg_a, b_rg_x, rg_lambda]
        PI = {n: i for i, n in enumerate(["lng", "lnb", "cw0", "cw1", "cw2", "cw3", "cb", "ba", "bx", "lam"])}
        prow = sb("prow", [128, 2, 128], F32)
        pcol = sb("pcol", [128, 256], F32)
        B_prow = Buf("prow"); B_pcol = Buf("pcol")
        P.op("pool", lambda e: e.memset(prow[:], 0.0), w=[B_prow])
        for i, par in enumerate(PAR):
            r0 = i * 16
            g, rr = divmod(r0, 128)
            P.op("sp", (lambda e, par=par, g=g, rr=rr: e.dma_start(
                out=prow[rr:rr + 16, g, :], in_=par.rearrange("(k p) -> k p", p=128))),
                r=[], w=[B_prow], dma=("prow", i))
        with ExitStack() as s0:
            pst = ps("pst0", [128, 256], F32, s0)
            B_pst = Buf("pst0")
            for g in range(2):
                P.op("pe", lambda e, g=g: e.transpose(out=pst[:, g * 128:(g + 1) * 128], in_=prow[:, g, :],
                                                     identity=ident_f[:]), r=[B_prow, B_const], w=[B_pst])
            P.op("dve", lambda e: e.tensor_copy(out=pcol[:], in_=pst[:]), r=[B_pst], w=[B_pcol])

        def pc(name, k):
            i = PI[name] * 16 + k
            return pcol[:, i:i + 1]

        cT_d = dram_scr("cT_d", [128, 2, T], BF16)
        c_d = dram_scr("c_d", [128, NT, 257], BF16)
        ki_d = dram_scr("ki_d", [128, T], BF16)
        B_cTd = Buf("cT_d"); B_cd = Buf("c_d"); B_kid = Buf("ki_d")
        B_cT = [Buf("cT%d" % b) for b in range(16)]
        B_c = [Buf("c%d" % b) for b in range(16)]
        B_ki = [Buf("ki%d" % b) for b in range(16)]
        B_c1 = Buf("c_ones")

        with ExitStack() as s1:
            cT_all = sb("cT_all", [128, 2, T], BF16, s1)
            c_all = sb("c_all", [128, NT, 257], BF16, s1)
            kiT_all = sb("kiT_all", [128, T], BF16, s1)
            P.op("pool", lambda e: e.memset(c_all[:, :, 256:257], 1.0), w=[B_c1])
            wc = sb("wc", [128, KC, 256], BF16, s1)
            wki = sb("wki", [128, KC, 128], BF16, s1)
            gkv = sb("gkv", [128, 2], F32, s1)
            grow = sb("grow", [128, D], F32, s1)
            brow = sb("brow", [128, D], F32, s1)
            B_wc = Buf("wc"); B_wki = Buf("wki"); B_gkv = Buf("gkv"); B_grow = Buf("grow")
            w_in_v = w_in.rearrange("(k p) n -> p k n", p=128)
            P.op("pool", lambda e: e.dma_start(out=wc[:], in_=w_in_v[:, :, C_C:C_C + 256]), w=[B_wc], dma=("wc", 0))
            P.op("pool", lambda e: e.dma_start(out=wki[:, :, 0:64], in_=w_in_v[:, :, C_KI:C_KI + 64]), w=[B_wki], dma=("wki", 0))
            P.op("pool", lambda e: e.dma_start(out=wki[:, :, 64:128], in_=w_in_v[:, :, C_KI:C_KI + 64]), w=[B_wki], dma=("wki", 0))
            P.op("sp", lambda e: e.dma_start(out=grow[:], in_=ln_in_g.partition_broadcast(128)), w=[B_grow], dma=("grow", 0))
            P.op("sp", lambda e: e.dma_start(out=brow[:], in_=ln_in_b.partition_broadcast(128)), w=[B_grow], dma=("grow", 0))
            gkv_row = sb("gkv_row", [2, 128], F32, s1)
            B_gkvr = Buf("gkvr")
            P.op("sp", lambda e: e.dma_start(out=gkv_row[:], in_=kv_norm_g.rearrange("(k p) -> k p", p=128)),
                 w=[B_gkvr], dma=("gkvr", 0))
            pst1 = ps("pst1", [128, 2], F32, s1)
            B_pst1 = B_pst
            P.op("pe", lambda e: e.transpose(out=pst1[:, 0:2], in_=gkv_row[:, :], identity=ident_f[0:2, 0:2]),
                 r=[B_gkvr, B_const], w=[B_pst1])
            P.op("dve", lambda e: e.tensor_copy(out=gkv[:], in_=pst1[:]), r=[B_pst1], w=[B_gkv])

            xt_ring = Ring("xt", [sb("xt%d" % i, [128, D], F32, s1) for i in range(3)])
            nt_ring = Ring("nt", [sb("nt%d" % i, [128, D], F32, s1) for i in range(3)])
            hT_ring = Ring("hTb", [sb("hTb%d" % i, [128, KC, 512], BF16, s1) for i in range(2)])
            st_ring = Ring("stat", [sb("stat%d" % i, [128, 40], F32, s1) for i in range(4)])
            tp_ring = Ring("tp", [ps("tp%d" % i, [128, 512], F32, s1) for i in range(2)])
            cps = [ps("cps%d" % i, [128, 512], F32, s1) for i in range(2)]
            B_cps = [Buf("cps%d" % i) for i in range(2)]
            ssps = ps("ssps", [128, 512], F32, s1); B_ssps = Buf("ssps")
            kips = ps("kips", [128, 512], F32, s1); B_kips = Buf("kips")
            ctp = ps("ctp", [128, 4, 256], BF16, s1); B_ctp = Buf("ctp")
            sq_ring = Ring("sq", [sb("sq%d" % i, [128, 512], F32, s1) for i in range(2)])
            rs = sb("rs", [128, 512], F32, s1); B_rs = Buf("rs")
            hown_ring = Ring("hown", [sb("hown%d" % i, [128, D], F32, s1) for i in range(1)])

            blkbuf = {}
            tilebuf = {}
            hT_parts = [[Buf("hTp%d_%d" % (sl, i)) for i in range(64)] for sl in range(2)]

            def p1_s0(tile_i):
                xt, B_xt, kx = xt_ring.next()
                P.op("sp", lambda e, xt=xt, tile_i=tile_i: e.dma_start(
                    out=xt[:], in_=xw[tile_i * 128:(tile_i + 1) * 128, :]), w=[B_xt], dma=kx)
                st, B_st, _ = st_ring.next()
                for q4 in range(4):
                    P.op("dve", lambda e, st=st, xt=xt, q4=q4: e.bn_stats(
                        out=st[:, q4 * 6:(q4 + 1) * 6], in_=xt[:, q4 * 512:(q4 + 1) * 512]), r=[B_xt], w=[B_st])
                P.op("dve", lambda e, st=st: e.bn_aggr(out=st[:, 24:26], in_=st[:, 0:24]), r=[B_st], w=[B_st])
                P.op("act", lambda e, st=st: e.activation(out=st[:, 26:27], in_=st[:, 25:26], func=AF.Sqrt,
                                                          bias=eps_ln[:, 0:1], scale=1.0), r=[B_st, B_const], w=[B_st])
                tilebuf[tile_i] = (xt, B_xt, st, B_st)

            def p1_s0b(tile_i):
                xt, B_xt, st, B_st = tilebuf[tile_i]
                P.op("dve", lambda e, st=st: e.reciprocal(out=st[:, 27:28], in_=st[:, 26:27]), r=[B_st], w=[B_st])
                ntile, B_nt, _ = nt_ring.next()
                P.op("dve", lambda e, st=st, xt=xt, ntile=ntile: e.tensor_scalar(
                    out=ntile[:], in0=xt[:], scalar1=st[:, 24:25], scalar2=st[:, 27:28],
                    op0=ALU.subtract, op1=ALU.mult), r=[B_xt, B_st], w=[B_nt])
                tilebuf[tile_i] = (ntile, B_nt)
                if tile_i % 8 == 7:
                    j = tile_i // 8
                    ho, B_ho, kh = hown_ring.next()
                    P.op("pool", lambda e, ho=ho, ntile=ntile: e.tensor_tensor(
                        out=ho[:], in0=ntile[:], in1=grow[:], op=ALU.mult), r=[B_nt, B_grow], w=[B_ho])
                    P.op("pool", lambda e, ho=ho: e.tensor_tensor(
                        out=ho[:], in0=ho[:], in1=brow[:], op=ALU.add), r=[B_ho, B_grow], w=[B_ho])
                    P.op("sp", lambda e, ho=ho, j=j: e.dma_start(out=h_own[j], in_=ho[:]),
                         r=[B_ho], w=[B_hown[j]], dma=("h_own", j % 4))

            def p1_s1(tile_i):
                blk, tl = divmod(tile_i, 4)
                if tl == 0:
                    blkbuf[blk] = hT_ring.next()
                hTb, B_hTb, _ = blkbuf[blk]
                ntile, B_nt = tilebuf.pop(tile_i)
                for k4 in range(4):
                    tp, B_tp, _ = tp_ring.next()
                    for kk in range(4):
                        k = k4 * 4 + kk
                        P.op("pe", lambda e, tp=tp, ntile=ntile, k=k, kk=kk: e.transpose(
                            out=tp[:, kk * 128:(kk + 1) * 128], in_=ntile[:, k * 128:(k + 1) * 128],
                            identity=ident_f[:]), r=[B_nt, B_const], w=[B_tp])
                    for kk in range(4):
                        k = k4 * 4 + kk
                        P.op("act", lambda e, tp=tp, hTb=hTb, k=k, kk=kk, tl=tl: e.activation(
                            out=hTb[:, k, tl * 128:(tl + 1) * 128], in_=tp[:, kk * 128:(kk + 1) * 128],
                            func=AF.Identity, bias=pc("lnb", k), scale=pc("lng", k)),
                            r=[B_tp, B_pcol], w=[hT_parts[blkbuf[blk][2][1]][tl * 16 + k]])

            def p1_b0(blk):
                hTb, _unused, _key = blkbuf[blk]
                parts = hT_parts[_key[1]]
                P.op("sp", lambda e, hTb=hTb, blk=blk: e.dma_start(out=hT_all[blk], in_=hTb[:]),
                     r=parts, w=[B_hT[blk]], dma=("hT_all", blk % 4))
                cols = slice(blk * 512, (blk + 1) * 512)
                for ch in range(2):
                    for k in range(KC):
                        P.op("pe", lambda e, ch=ch, k=k, hTb=hTb: e.matmul(
                            cps[ch][:], lhsT=wc[:, k, ch * 128:(ch + 1) * 128], rhs=hTb[:, k, :],
                            start=(k == 0), stop=(k == KC - 1)), r=[B_wc] + [parts[t_ * 16 + k] for t_ in range(4)], w=[B_cps[ch]])
                for k in range(KC):
                    P.op("pe", lambda e, k=k, hTb=hTb: e.matmul(
                        kips[:], lhsT=wki[:, k, :], rhs=hTb[:, k, :], start=(k == 0), stop=(k == KC - 1)),
                        r=[B_wki] + [parts[t_ * 16 + k] for t_ in range(4)], w=[B_kips])
                P.op("act", lambda e, cols=cols: e.copy(out=kiT_all[:, cols], in_=kips[:]), r=[B_kips], w=[B_ki[blk]])
                for ch in range(2):
                    sq, B_sq, _ = sq_ring.next()
                    P.op("act", lambda e, sq=sq, ch=ch: e.activation(out=sq[:], in_=cps[ch][:], func=AF.Square),
                         r=[B_cps[ch]], w=[B_sq])
                    P.op("pe", lambda e, sq=sq, ch=ch: e.matmul(ssps[:], lhsT=ones_f[:], rhs=sq[:],
                                                                start=(ch == 0), stop=(ch == 1)),
                         r=[B_sq, B_const], w=[B_ssps])
                P.op("act", lambda e: e.activation(out=rs[:], in_=ssps[:], func=AF.Sqrt, bias=eps_rms[:, 0:1],
                                                   scale=1.0 / 256.0), r=[B_ssps, B_const], w=[B_rs])

            def p1_b1(blk):
                cols = slice(blk * 512, (blk + 1) * 512)
                P.op("dve", lambda e: e.reciprocal(out=rs[:], in_=rs[:]), r=[B_rs], w=[B_rs])
                for ch in range(2):
                    P.op("dve", lambda e, ch=ch, cols=cols: e.scalar_tensor_tensor(
                        out=cT_all[:, ch, cols], in0=cps[ch][:], scalar=gkv[:, ch:ch + 1], in1=rs[:],
                        op0=ALU.mult, op1=ALU.mult), r=[B_cps[ch], B_gkv, B_rs], w=[B_cT[blk]])

            def p1_b2(blk):
                for tl in range(4):
                    for ch in range(2):
                        c0 = blk * 512 + tl * 128
                        P.op("pe", lambda e, tl=tl, ch=ch, c0=c0: e.transpose(
                            out=ctp[:, tl, ch * 128:(ch + 1) * 128], in_=cT_all[:, ch, c0:c0 + 128],
                            identity=ident_b[:]), r=[B_cT[blk], B_const], w=[B_ctp])
                P.op("act", lambda e, blk=blk: e.copy(out=c_all[:, blk * 4:(blk + 1) * 4, 0:256], in_=ctp[:]),
                     r=[B_ctp], w=[B_c[blk]])

            for step in range(NT + 7):
                if step < NT:
                    p1_s0(step)
                if 0 <= step - 1 < NT:
                    p1_s0b(step - 1)
                t1_ = step - 3
                if 0 <= t1_ < NT:
                    p1_s1(t1_)
                    if t1_ % 4 == 3:
                        p1_b0(t1_ // 4)
                t2_ = step - 4
                if 0 <= t2_ < NT and t2_ % 4 == 3:
                    p1_b1(t2_ // 4)
                t3_ = step - 5
                if 0 <= t3_ < NT and t3_ % 4 == 3:
                    p1_b2(t3_ // 4)

            P.op("sp", lambda e: e.dma_start(out=cT_d, in_=cT_all[:]), r=B_cT, w=[B_cTd], dma=("cT_d", 0))
            P.op("sp", lambda e: e.dma_start(out=c_d, in_=c_all[:]), r=B_c + [B_c1], w=[B_cd], dma=("c_d", 0))
            P.op("sp", lambda e: e.dma_start(out=ki_d, in_=kiT_all[:]), r=B_ki, w=[B_kid], dma=("ki_d", 0))

        P.barrier()

        def finish_debug():
            P.op("sp", None, r=[B_dbg])
            P.emit()
            return nc, dbg, list(declared)

        if debug and stop_after == "1a":
            d1 = dbg_out("cT", [128, 2, T], BF16)
            d2 = dbg_out("c", [128, NT, 257], BF16)
            d3 = dbg_out("kiT", [128, T], BF16)
            d4 = dbg_out("h_own", [NOWN, 128, D], F32)
            d5 = dbg_out("hT_all", [16, 128, KC, 512], BF16)
            P.op("sp", lambda e: e.dma_start(out=d1, in_=cT_d), r=[B_cTd], w=[B_dbg], dma=("dbg", 0))
            P.op("sp", lambda e: e.dma_start(out=d2, in_=c_d), r=[B_cd], w=[B_dbg], dma=("dbg", 0))
            P.op("sp", lambda e: e.dma_start(out=d3, in_=ki_d), r=[B_kid], w=[B_dbg], dma=("dbg", 0))
            P.op("sp", lambda e: e.dma_start(out=d4.rearrange("j p d -> p j d"), in_=h_own.rearrange("j p d -> p j d")),
                 r=B_hown, w=[B_dbg], dma=("dbg", 0))
            P.op("sp", lambda e: e.dma_start(out=d5.rearrange("b p k t -> p b (k t)"),
                                             in_=hT_all.rearrange("b p k t -> p b (k t)")),
                 r=B_hT, w=[B_dbg], dma=("dbg", 0))
            return finish_debug()

        w_rg_a = IN("w_rg_a"); w_rg_x = IN("w_rg_x"); valid7 = IN("valid7")
        with ExitStack() as s2:
            hstate = sb("hstate", [128, KC], F32, s2)
            vrow = sb("vrow", [128, 896], F32, s2)
            lp = sb("lp", [128, 8, KC], F32, s2)
            B_hst = [Buf("hst%d" % c) for c in range(KC)]
            B_vrow = Buf("vrow"); B_lp = Buf("lp")
            P.op("pool", lambda e: e.memset(hstate[:], 0.0), w=B_hst)
            P.op("sp", lambda e: e.dma_start(out=vrow[:], in_=valid7[0].partition_broadcast(128)), w=[B_vrow], dma=("vrow", 0))
            lam = pcol[:, PI["lam"] * 16:PI["lam"] * 16 + 16]
            X, SER, LN1P, MSK, SP_, CH, HBA, HBX = range(8)
            P.op("act", lambda e: e.activation(out=lp[:, X, :], in_=lam, func=AF.Exp, scale=-1.0), r=[B_pcol], w=[B_lp])
            P.op("act", lambda e: e.activation(out=lp[:, LN1P, :], in_=lp[:, X, :], func=AF.Ln, bias=ones_f[:, 0:1], scale=1.0),
                 r=[B_lp, B_const], w=[B_lp])
            P.op("dve", lambda e: e.tensor_scalar(out=lp[:, SER, :], in0=lp[:, X, :], scalar1=-0.25, scalar2=1.0 / 3.0,
                                                  op0=ALU.mult, op1=ALU.add), r=[B_lp], w=[B_lp])
            P.op("dve", lambda e: e.tensor_tensor(out=lp[:, SER, :], in0=lp[:, SER, :], in1=lp[:, X, :], op=ALU.mult), r=[B_lp], w=[B_lp])
            P.op("dve", lambda e: e.tensor_scalar(out=lp[:, SER, :], in0=lp[:, SER, :], scalar1=-0.5, scalar2=None,
                                                  op0=ALU.add), r=[B_lp], w=[B_lp])
            P.op("dve", lambda e: e.tensor_tensor(out=lp[:, SER, :], in0=lp[:, SER, :], in1=lp[:, X, :], op=ALU.mult), r=[B_lp], w=[B_lp])
            P.op("dve", lambda e: e.tensor_scalar(out=lp[:, SER, :], in0=lp[:, SER, :], scalar1=1.0, scalar2=None,
                                                  op0=ALU.add), r=[B_lp], w=[B_lp])
            P.op("dve", lambda e: e.tensor_tensor(out=lp[:, SER, :], in0=lp[:, SER, :], in1=lp[:, X, :], op=ALU.mult), r=[B_lp], w=[B_lp])
            P.op("dve", lambda e: e.tensor_scalar(out=lp[:, MSK, :], in0=lp[:, X, :], scalar1=0.05, scalar2=None,
                                                  op0=ALU.is_lt), r=[B_lp], w=[B_lp])
            P.op("dve", lambda e: e.tensor_tensor(out=lp[:, SP_, :], in0=lp[:, SER, :], in1=lp[:, LN1P, :], op=ALU.subtract), r=[B_lp], w=[B_lp])
            P.op("dve", lambda e: e.tensor_tensor(out=lp[:, SP_, :], in0=lp[:, SP_, :], in1=lp[:, MSK, :], op=ALU.mult), r=[B_lp], w=[B_lp])
            P.op("dve", lambda e: e.tensor_tensor(out=lp[:, SP_, :], in0=lp[:, SP_, :], in1=lp[:, LN1P, :], op=ALU.add), r=[B_lp], w=[B_lp])
            P.op("dve", lambda e: e.tensor_scalar(out=lp[:, CH, :], in0=lp[:, SP_, :], scalar1=-4.0, scalar2=None, op0=ALU.mult), r=[B_lp], w=[B_lp])
            P.op("dve", lambda e: e.tensor_scalar(out=lp[:, HBA, :], in0=pcol[:, PI["ba"] * 16:PI["ba"] * 16 + 16], scalar1=0.5,
                                                  scalar2=None, op0=ALU.mult), r=[B_pcol], w=[B_lp])
            P.op("dve", lambda e: e.tensor_scalar(out=lp[:, HBX, :], in0=pcol[:, PI["bx"] * 16:PI["bx"] * 16 + 16], scalar1=0.5,
                                                  scalar2=None, op0=ALU.mult), r=[B_pcol], w=[B_lp])

            wxr = sb("wxr", [128, KC, 1024], BF16, s2); B_wxr = Buf("wxr")
            wga = sb("wga", [128, 8, 128], BF16, s2); wgx = sb("wgx", [128, 8, 128], BF16, s2); B_wg = Buf("wg")
            diag = sb("diag", [128, 32, 128], BF16, s2); B_diag = Buf("diag")
            hT_ring = Ring("hTb2", [sb("hTc%d" % i, [128, KC, 512], BF16, s2) for i in range(2)])
            xrb = sb("xrb", [128, 8, 515], BF16, s2); B_xrb = [Buf("xrb%d" % i) for i in range(8)]
            xcb = sb("xcb", [128, 8, 512], BF16, s2); B_xc = [Buf("xc%d" % i) for i in range(8)]
            trb = sb("trb", [128, 8, 512], F32, s2); B_tr = [Buf("tr%d" % i) for i in range(8)]
            tib = sb("tib", [128, 8, 512], BF16, s2); B_ti = [Buf("ti%d" % i) for i in range(8)]
            ab = sb("ab", [128, 8, 512], F32, s2); B_a = [Buf("a%d" % i) for i in range(8)]
            sbf = sb("sbf", [128, 8, 512], BF16, s2); B_s = [Buf("s%d" % i) for i in range(8)]
            hb = sb("hb", [128, 8, 512], F32, s2); B_hb = [Buf("hb%d" % i) for i in range(8)]
            xps_ring = Ring("xps", [ps("xps%d" % i, [128, 512], F32, s2) for i in range(2)])
            cv_ring = Ring("cvps", [ps("cvps%d" % i, [128, 512], F32, s2) for i in range(2)])
            ga_ring = Ring("gaps", [ps("gaps%d" % i, [128, 512], F32, s2) for i in range(2)])
            gx_ring = Ring("gxps", [ps("gxps%d" % i, [128, 512], F32, s2) for i in range(2)])
            w_rg_a_v = w_rg_a.rearrange("n d e -> d n e")
            w_rg_x_v = w_rg_x.rearrange("n d e -> d n e")

            for PS in range(2):
                P.op("pool", lambda e, PS=PS: e.dma_start(out=wxr[:], in_=w_in_v[:, :, C_XR + PS * 1024:C_XR + (PS + 1) * 1024]),
                     w=[B_wxr], dma=("wxr", 0))
                P.op("pool", lambda e, PS=PS: e.dma_start(out=wga[:], in_=w_rg_a_v[:, PS * 8:(PS + 1) * 8, :]), w=[B_wg], dma=("wg", 0))
                P.op("pool", lambda e, PS=PS: e.dma_start(out=wgx[:], in_=w_rg_x_v[:, PS * 8:(PS + 1) * 8, :]), w=[B_wg], dma=("wg", 0))
                for cl in range(8):
                    c = PS * 8 + cl
                    for k in range(4):
                        P.op("dve", lambda e, cl=cl, k=k, c=c: e.tensor_scalar(
                            out=diag[:, cl * 4 + k, :], in0=ident_b[:], scalar1=pc("cw%d" % k, c), scalar2=None, op0=ALU.mult),
                            r=[B_const, B_pcol], w=[B_diag])
                    P.op("pool", lambda e, cl=cl: e.memset(xrb[:, cl, 0:3], 0.0), w=[B_xrb[cl]])
                blkb = {}
                itb = {}

                def A0(it, PS=PS):
                    blk, cl = divmod(it, 8)
                    if cl == 0:
                        hTb, B_hTb, kh = hT_ring.next()
                        P.op("sp", lambda e, hTb=hTb, blk=blk: e.dma_start(out=hTb[:], in_=hT_all[blk]), r=[B_hT[blk]], w=[B_hTb], dma=kh)
                        blkb[blk] = (hTb, B_hTb)
                    hTb, B_hTb = blkb[blk]
                    xps, B_xps, _ = xps_ring.next()
                    for k in range(KC):
                        P.op("pe", lambda e, xps=xps, k=k, cl=cl, hTb=hTb: e.matmul(
                            xps[:], lhsT=wxr[:, k, cl * 128:(cl + 1) * 128], rhs=hTb[:, k, :], start=(k == 0), stop=(k == KC - 1)),
                            r=[B_wxr, B_hTb], w=[B_xps])
                    if blk == 0:
                        P.op("dve", lambda e, xps=xps, cl=cl: e.tensor_tensor(out=xrb[:, cl, 3:515], in0=xps[:], in1=vrow[:, 0:512], op=ALU.mult),
                             r=[B_xps, B_vrow], w=[B_xrb[cl]])
                    elif blk == 1:
                        P.op("dve", lambda e, xps=xps, cl=cl: e.tensor_tensor(out=xrb[:, cl, 3:387], in0=xps[:, 0:384], in1=vrow[:, 512:896], op=ALU.mult),
                             r=[B_xps, B_vrow], w=[B_xrb[cl]])
                        P.op("dve", lambda e, xps=xps, cl=cl: e.tensor_copy(out=xrb[:, cl, 387:515], in_=xps[:, 384:512]),
                             r=[B_xps], w=[B_xrb[cl]])
                    else:
                        P.op("dve", lambda e, xps=xps, cl=cl: e.tensor_copy(out=xrb[:, cl, 3:515], in_=xps[:]), r=[B_xps], w=[B_xrb[cl]])

                def A1(it, PS=PS):
                    blk, cl = divmod(it, 8)
                    c = PS * 8 + cl
                    cvp, B_cvp, _ = cv_ring.next()
                    for k in range(4):
                        P.op("pe", lambda e, cvp=cvp, k=k, cl=cl: e.matmul(
                            cvp[:], lhsT=diag[:, cl * 4 + k, :], rhs=xrb[:, cl, k:k + 512], start=(k == 0), stop=(k == 3)),
                            r=[B_diag, B_xrb[cl]], w=[B_cvp])
                    P.op("act", lambda e, cvp=cvp, cl=cl, c=c: e.activation(out=xcb[:, cl, :], in_=cvp[:], func=AF.Identity,
                                                                         bias=pc("cb", c), scale=1.0), r=[B_cvp, B_pcol], w=[B_xc[cl]])
                    P.op("pool", lambda e, cl=cl: e.tensor_copy(out=xrb[:, cl, 0:3], in_=xrb[:, cl, 512:515]), r=[B_xrb[cl]], w=[B_xrb[cl]])

                def A2(it, PS=PS):
                    blk, cl = divmod(it, 8)
                    c = PS * 8 + cl
                    gap, B_gap, _ = ga_ring.next()
                    gxp, B_gxp, _ = gx_ring.next()
                    P.op("pe", lambda e, gap=gap, cl=cl: e.matmul(gap[:], lhsT=wga[:, cl, :], rhs=xcb[:, cl, :], start=True, stop=True),
                         r=[B_wg, B_xc[cl]], w=[B_gap])
                    P.op("pe", lambda e, gxp=gxp, cl=cl: e.matmul(gxp[:], lhsT=wgx[:, cl, :], rhs=xcb[:, cl, :], start=True, stop=True),
                         r=[B_wg, B_xc[cl]], w=[B_gxp])
                    P.op("act", lambda e, gap=gap, cl=cl, c=c: e.activation(out=trb[:, cl, :], in_=gap[:], func=AF.Tanh,
                                                                         bias=lp[:, HBA, c:c + 1], scale=0.5), r=[B_gap, B_lp], w=[B_tr[cl]])
                    P.op("act", lambda e, gxp=gxp, cl=cl, c=c: e.activation(out=tib[:, cl, :], in_=gxp[:], func=AF.Tanh,
                                                                         bias=lp[:, HBX, c:c + 1], scale=0.5), r=[B_gxp, B_lp], w=[B_ti[cl]])
                    P.op("act", lambda e, cl=cl, c=c: e.activation(out=ab[:, cl, :], in_=trb[:, cl, :], func=AF.Exp,
                                                                 bias=lp[:, CH, c:c + 1], scale=lp[:, CH, c:c + 1]), r=[B_tr[cl], B_lp], w=[B_a[cl]])

                def BST(blk, PS=PS):
                    for cl in range(8):
                        P.op("pool", lambda e, cl=cl: e.tensor_tensor(out=trb[:, cl, :], in0=ab[:, cl, :], in1=ab[:, cl, :], op=ALU.mult),
                             r=[B_a[cl]], w=[B_tr[cl]])
                    for cl in range(8):
                        P.op("act", lambda e, cl=cl: e.activation(out=sbf[:, cl, :], in_=trb[:, cl, :], func=AF.Sqrt, bias=ones_f[:, 0:1], scale=-1.0),
                             r=[B_tr[cl], B_const], w=[B_s[cl]])
                    for cl in range(8):
                        P.op("dve", lambda e, cl=cl: e.tensor_scalar(out=tib[:, cl, :], in0=tib[:, cl, :], scalar1=1.0, scalar2=0.5,
                                                                     op0=ALU.add, op1=ALU.mult), r=[B_ti[cl]], w=[B_ti[cl]])
                    for cl in range(8):
                        c = PS * 8 + cl
                        P.op("dve", lambda e, cl=cl: e.tensor_tensor(out=sbf[:, cl, :], in0=tib[:, cl, :], in1=sbf[:, cl, :], op=ALU.mult),
                             r=[B_ti[cl], B_s[cl]], w=[B_s[cl]])
                        P.op("pool", lambda e, cl=cl: e.tensor_tensor(out=tib[:, cl, :], in0=sbf[:, cl, :], in1=xcb[:, cl, :], op=ALU.mult),
                             r=[B_s[cl], B_xc[cl]], w=[B_ti[cl]])
                        if blk == 0:
                            P.op("pool", lambda e, cl=cl: e.tensor_tensor(out=tib[:, cl, :], in0=tib[:, cl, :], in1=vrow[:, 0:512], op=ALU.mult),
                                 r=[B_ti[cl], B_vrow], w=[B_ti[cl]])
                        elif blk == 1:
                            P.op("pool", lambda e, cl=cl: e.tensor_tensor(out=tib[:, cl, 0:384], in0=tib[:, cl, 0:384], in1=vrow[:, 512:896], op=ALU.mult),
                                 r=[B_ti[cl], B_vrow], w=[B_ti[cl]])
                        P.op("dve", lambda e, cl=cl, c=c: e.tensor_tensor_scan(out=hb[:, cl, :], data0=ab[:, cl, :], data1=tib[:, cl, :],
                                                                            initial=hstate[:, c:c + 1], op0=ALU.mult, op1=ALU.add),
                             r=[B_a[cl], B_ti[cl], B_hst[c]], w=[B_hb[cl]])
                        P.op("dve", lambda e, cl=cl, c=c: e.tensor_copy(out=hstate[:, c:c + 1], in_=hb[:, cl, 511:512]), r=[B_hb[cl]], w=[B_hst[c]])
                    if blk % 2 == 1:
                        j = blk // 2
                        P.op("sp", lambda e, j=j, PS=PS: e.dma_start(out=lruh[j][:, PS * 8:(PS + 1) * 8, :], in_=hb[:, :, 384:512]),
                             r=B_hb, w=[B_lruh[j][PS]], dma=("lruh", (2 * j + PS) % 4))

                NIT = 128
                SK1, SK2 = 3, 4
                for step in range(NIT + SK2):
                    t2_ = step - SK2
                    if 0 <= t2_ < NIT:
                        A2(t2_)
                        if t2_ % 8 == 7:
                            BST(t2_ // 8)
                    if 0 <= step - SK1 < NIT:
                        A1(step - SK1)
                    if step < NIT:
                        A0(step)

        P.barrier()
        if debug and stop_after == "1b":
            d1 = dbg_out("lruh", [NOWN, 128, KC, 128], F32)
            P.op("sp", lambda e: e.dma_start(out=d1.rearrange("j p k t -> p j (k t)"), in_=lruh.rearrange("j p k t -> p j (k t)")),
                 r=[b for bb in B_lruh for b in bb], w=[B_dbg], dma=("dbg", 0))
            return finish_debug()

        w_uk = IN("w_uk")
        qlat_d = dram_scr("qlat_d", [NOWN, 128, 16, 2, 128], BF16)
        qi_d = dram_scr("qi_d", [128, 8, 1024], BF16)
        lruT_d = dram_scr("lruT_d", [128, KC, 1024], BF16)
        gaT_d = dram_scr("gaT_d", [128, KC, 1024], BF16)
        gbT_d = dram_scr("gbT_d", [128, KC, 1024], BF16)
        B_qlat = [[Buf("qlat%d_%d" % (h, th)) for th in range(2)] for h in range(32)]
        B_qid = Buf("qi_d")
        B_lruT = [[Buf("lruT%d_%d" % (m, th)) for th in range(2)] for m in range(KC)]
        B_gaT = [[Buf("gaT%d_%d" % (m, th)) for th in range(2)] for m in range(KC)]
        B_gbT = [[Buf("gbT%d_%d" % (m, th)) for th in range(2)] for m in range(KC)]
        wsc = sb("wsc", [128, NOWN, 16], F32); B_wsc = Buf("wsc")
        with ExitStack() as s3:
            hT_own = sb("hT_own", [128, KC, 1024], BF16, s3); B_hTo = Buf("hT_own")
            for j in range(NOWN):
                P.op("sp", lambda e, j=j: e.dma_start(out=hT_own[:, :, j * 128:(j + 1) * 128], in_=hT_all[2 * j + 1][:, :, 384:512]),
                     r=[B_hT[2 * j + 1]], w=[B_hTo], dma=("hT_own", 0))
            qT_sb = sb("qT_sb", [128, 16, 1024], BF16, s3); B_qT = [Buf("qT%d" % h) for h in range(16)]
            qiT_sb = sb("qiT_sb", [128, 8, 1024], BF16, s3); B_qiT = Buf("qiT")
            wuk = sb("wuk", [128, 16, 256], BF16, s3); B_wuk = Buf("wuk")
            wwi = sb("wwi", [128, KC, 16], BF16, s3); B_wwi = Buf("wwi")
            P.op("pool", lambda e: e.dma_start(out=wuk[:], in_=w_uk.rearrange("h d c -> d h c")), w=[B_wuk], dma=("wuk", 0))
            P.op("pool", lambda e: e.dma_start(out=wwi[:], in_=w_in_v[:, :, C_WI:C_WI + 16]), w=[B_wwi], dma=("wwi", 0))
            w_ring = Ring("wr2", [sb("wr2_%d" % i, [128, KC, 512], BF16, s3) for i in range(3)])
            pp_ring = Ring("pp2", [ps("pp2_%d" % i, [128, 512], F32, s3) for i in range(4)])
            stg_ring = Ring("stg2", [sb("stg2_%d" % i, [128, 512], BF16, s3) for i in range(4)])
            sq_ring2 = Ring("sq2", [sb("sq2_%d" % i, [128, 512], F32, s3) for i in range(2)])
            tt_ring = Ring("tt2", [sb("tt2_%d" % i, [128, 512], F32, s3) for i in range(2)])
            lh_ring = Ring("lh2", [sb("lh2_%d" % i, [128, 512], F32, s3) for i in range(2)])
            wips = ps("wips", [128, 16], F32, s3); B_wips = Buf("wips")
            for j in range(NOWN):
                for k in range(KC):
                    P.op("pe", lambda e, j=j, k=k: e.matmul(wips[:], lhsT=hT_own[:, k, j * 128:(j + 1) * 128], rhs=wwi[:, k, :],
                                                           start=(k == 0), stop=(k == KC - 1)), r=[B_hTo, B_wwi], w=[B_wips])
                P.op("dve", lambda e, j=j: e.tensor_scalar(out=wsc[:, j, :], in0=wips[:], scalar1=1.0 / 32.0, scalar2=None, op0=ALU.mult),
                     r=[B_wips], w=[B_wsc])
            units = ([("q", C_Q + 512 * u, u) for u in range(4)] + [("qi", C_QI + 512 * u, u) for u in range(2)] +
                     [("yg", C_YG + 512 * u, u) for u in range(4)] + [("ga", C_GA + 512 * u, u) for u in range(4)] +
                     [("gb", C_GB + 512 * u, u) for u in range(4)])
            ecount = 0
            for (kind, col0, u) in units:
                wt, B_wt, kw = w_ring.next()
                P.op("pool", lambda e, wt=wt, col0=col0: e.dma_start(out=wt[:], in_=w_in_v[:, :, col0:col0 + 512]), w=[B_wt], dma=kw)
                for mm in range(4):
                    m = u * 4 + mm
                    for th in range(2):
                        tsl = slice(th * 512, (th + 1) * 512)
                        pp, B_pp, _ = pp_ring.next()
                        for k in range(KC):
                            P.op("pe", lambda e, pp=pp, wt=wt, k=k, mm=mm, tsl=tsl: e.matmul(
                                pp[:], lhsT=wt[:, k, mm * 128:(mm + 1) * 128], rhs=hT_own[:, k, tsl], start=(k == 0), stop=(k == KC - 1)),
                                r=[B_wt, B_hTo], w=[B_pp])
                        if kind in ("q", "qi"):
                            dst = qT_sb[:, m, tsl] if kind == "q" else qiT_sb[:, m, tsl]
                            Bd = B_qT[m] if kind == "q" else B_qiT
                            ecount += 1
                            if ecount % 2 == 0:
                                P.op("act", lambda e, dst=dst, pp=pp: e.copy(out=dst, in_=pp[:]), r=[B_pp], w=[Bd])
                            else:
                                P.op("dve", lambda e, dst=dst, pp=pp: e.tensor_copy(out=dst, in_=pp[:]), r=[B_pp], w=[Bd])
                        elif kind == "yg":
                            sq, B_sq, _ = sq_ring2.next()
                            tt, B_tt, _ = tt_ring.next()
                            lh, B_lh, klh = lh_ring.next()
                            stg, B_stg, _ = stg_ring.next()
                            P.op("sp", lambda e, lh=lh, m=m, th=th: e.dma_start(
                                out=lh[:].rearrange("p (j t) -> p j t", j=4),
                                in_=lruh[4 * th:4 * th + 4, :, m, :].rearrange("j p t -> p j t")),
                                r=[B_lruh[jj][m // 8] for jj in range(4 * th, 4 * th + 4)], w=[B_lh], dma=klh)
                            P.op("act", lambda e, sq=sq, pp=pp: e.activation(out=sq[:], in_=pp[:], func=AF.Square), r=[B_pp], w=[B_sq])
                            P.op("dve", lambda e, sq=sq: e.tensor_scalar(out=sq[:], in0=sq[:], scalar1=0.044715, scalar2=1.0,
                                                                         op0=ALU.mult, op1=ALU.add), r=[B_sq], w=[B_sq])
                            P.op("dve", lambda e, sq=sq, pp=pp: e.tensor_tensor(out=sq[:], in0=sq[:], in1=pp[:], op=ALU.mult), r=[B_sq, B_pp], w=[B_sq])
                            P.op("act", lambda e, sq=sq, tt=tt: e.activation(out=tt[:], in_=sq[:], func=AF.Tanh, scale=0.7978845608028654),
                                 r=[B_sq], w=[B_tt])
                            P.op("dve", lambda e, tt=tt, pp=pp: e.scalar_tensor_tensor(out=tt[:], in0=tt[:], scalar=1.0, in1=pp[:],
                                                                                  op0=ALU.add, op1=ALU.mult), r=[B_tt, B_pp], w=[B_tt])
                            P.op("dve", lambda e, tt=tt, lh=lh, stg=stg: e.scalar_tensor_tensor(out=stg[:], in0=tt[:], scalar=0.5, in1=lh[:],
                                                                                           op0=ALU.mult, op1=ALU.mult), r=[B_tt, B_lh], w=[B_stg])
                            P.op("sp", lambda e, stg=stg, m=m, tsl=tsl: e.dma_start(out=lruT_d[:, m, tsl], in_=stg[:]),
                                 r=[B_stg], w=[B_lruT[m][th]], dma=("lruT_d", (2 * m + th) % 4))
                        else:
                            tt, B_tt, _ = tt_ring.next()
                            stg, B_stg, _ = stg_ring.next()
                            dd = gaT_d if kind == "ga" else gbT_d
                            Bd = (B_gaT if kind == "ga" else B_gbT)[m][th]
                            P.op("act", lambda e, tt=tt, pp=pp: e.activation(out=tt[:], in_=pp[:], func=AF.Tanh, scale=0.5), r=[B_pp], w=[B_tt])
                            P.op("dve", lambda e, tt=tt, stg=stg: e.tensor_scalar(out=stg[:], in0=tt[:], scalar1=0.5, scalar2=0.5,
                                                                                  op0=ALU.mult, op1=ALU.add), r=[B_tt], w=[B_stg])
                            P.op("sp", lambda e, stg=stg, dd=dd, m=m, tsl=tsl: e.dma_start(out=dd[:, m, tsl], in_=stg[:]),
                                 r=[B_stg], w=[Bd], dma=(kind + "T_d", (2 * m + th) % 4))
                if kind == "q":
                    for mm in range(4):
                        h = u * 4 + mm
                        for ch in range(2):
                            for th in range(2):
                                tsl = slice(th * 512, (th + 1) * 512)
                                pp, B_pp, _ = pp_ring.next()
                                stg, B_stg, _ = stg_ring.next()
                                P.op("pe", lambda e, pp=pp, h=h, ch=ch, tsl=tsl: e.matmul(
                                    pp[:], lhsT=wuk[:, h, ch * 128:(ch + 1) * 128], rhs=qT_sb[:, h, tsl], start=True, stop=True),
                                    r=[B_wuk, B_qT[h]], w=[B_pp])
                                P.op("act", lambda e, pp=pp, stg=stg: e.activation(out=stg[:], in_=pp[:], func=AF.Copy, scale=ATTN_SCALE),
                                     r=[B_pp], w=[B_stg])
                                P.op("sp", lambda e, stg=stg, h=h, ch=ch, th=th: e.dma_start(
                                    out=qlat_d[4 * th:4 * th + 4, :, h, ch, :].rearrange("j p q -> p j q"),
                                    in_=stg[:].rearrange("p (j q) -> p j q", j=4)),
                                    r=[B_stg], w=[B_qlat[h * 2 + ch][th]], dma=("qlat_d", (h * 4 + ch * 2 + th) % 4))
            P.op("sp", lambda e: e.dma_start(out=qi_d, in_=qiT_sb[:]), r=[B_qiT], w=[B_qid], dma=("qi_d", 0))

        P.barrier()
        if debug and stop_after == "2":
            d1 = dbg_out("qlat", [NOWN, 128, 16, 2, 128], BF16)
            d2 = dbg_out("qi", [128, 8, 1024], BF16)
            d3 = dbg_out("lruT", [128, KC, 1024], BF16)
            d4 = dbg_out("gaT", [128, KC, 1024], BF16)
            d5 = dbg_out("wsc", [128, NOWN, 16], F32)
            P.op("sp", lambda e: e.dma_start(out=d1.rearrange("j p h c q -> p j (h c q)"), in_=qlat_d.rearrange("j p h c q -> p j (h c q)")),
                 r=[b for bb in B_qlat for b in bb], w=[B_dbg], dma=("dbg", 0))
            P.op("sp", lambda e: e.dma_start(out=d2, in_=qi_d), r=[B_qid], w=[B_dbg], dma=("dbg", 0))
            P.op("sp", lambda e: e.dma_start(out=d3, in_=lruT_d), r=[b for bb in B_lruT for b in bb], w=[B_dbg], dma=("dbg", 0))
            P.op("sp", lambda e: e.dma_start(out=d4, in_=gaT_d), r=[b for bb in B_gaT for b in bb], w=[B_dbg], dma=("dbg", 0))
            P.op("sp", lambda e: e.dma_start(out=d5, in_=wsc[:]), r=[B_wsc], w=[B_dbg], dma=("dbg", 0))
            return finish_debug()

        w_uv = IN("w_uv")
        attnT_d = dram_scr("attnT_d", [128, 16, 1024], BF16)
        B_attnT = [Buf("attnT%d" % j) for j in range(NOWN)]
        with ExitStack() as s4:
            cT_all = sb("cT_all3", [128, 2, T], BF16, s4); B_cTs = Buf("cTs")
            c_all = sb("c_all3", [128, NT, 257], BF16, s4); B_cs = Buf("cs")
            kiT_all = sb("kiT_all3", [128, T], BF16, s4); B_kis = Buf("kis")
            P.op("sp", lambda e: e.dma_start(out=cT_all[:], in_=cT_d), r=[B_cTd], w=[B_cTs], dma=("cTs", 0))
            P.op("sp", lambda e: e.dma_start(out=c_all[:], in_=c_d), r=[B_cd], w=[B_cs], dma=("cs", 0))
            P.op("sp", lambda e: e.dma_start(out=kiT_all[:], in_=ki_d), r=[B_kid], w=[B_kis], dma=("kis", 0))
            wuv = sb("wuv", [128, 2, 16, 128], BF16, s4); B_wuv = Buf("wuv")
            P.op("pool", lambda e: e.dma_start(out=wuv[:, 0], in_=w_uv[:, 0:128, :].rearrange("h p d -> p h d")), w=[B_wuv], dma=("wuv", 0))
            P.op("pool", lambda e: e.dma_start(out=wuv[:, 1], in_=w_uv[:, 128:256, :].rearrange("h p d -> p h d")), w=[B_wuv], dma=("wuv", 0))
            vrow3 = sb("vrow3", [128, 896], F32, s4); pen7 = sb("pen7", [128, 896], F32, s4); B_v3 = Buf("v3")
            P.op("sp", lambda e: e.dma_start(out=vrow3[:], in_=valid7[0].partition_broadcast(128)), w=[B_v3], dma=("v3", 0))
            P.op("pool", lambda e: e.tensor_scalar(out=pen7[:], in0=vrow3[:], scalar1=-1.0, scalar2=BIG, op0=ALU.add, op1=ALU.mult),
                 r=[B_v3], w=[B_v3])
            pow2 = sb("pow2", [128, NBIS], F32, s4); B_pow2 = Buf("pow2")
            for k in range(NBIS):
                P.op("pool", lambda e, k=k: e.memset(pow2[:, k:k + 1], 2.0 ** -(k + 1)), w=[B_pow2])
            Isc = sb("Isc", [128, T], F32, s4); B_Ikb = [Buf("Isc%d" % i) for i in range(16)]
            junk = sb("junk", [128, T], mybir.dt.uint8, s4); B_junk = Buf("junk")
            maskT = sb("maskT", [128, NT, 128], BF16, s4); B_mT = Buf("maskT")
            bis = sb("bis", [128, 8], F32, s4); B_bis = Buf("bis")
            wk = sb("wk", [128, NBIS], F32, s4)
            qi_ring = Ring("qit", [sb("qit%d" % i, [128, 8, 128], BF16, s4) for i in range(2)])
            ql_ring = Ring("qlt", [sb("qlt%d" % i, [128, 16, 2, 128], BF16, s4) for i in range(2)])
            pT_ring = Ring("pT", [sb("pT%d" % i, [128, 2, 128], BF16, s4) for i in range(6)])
            mk_ring = Ring("mk", [sb("mk%d" % i, [128, 512], BF16, s4) for i in range(2)])
            ol_ring = Ring("ol", [sb("ol%d" % i, [128, 256], BF16, s4) for i in range(2)])
            rc_ring = Ring("rc", [sb("rc%d" % i, [128, 1], F32, s4) for i in range(2)])
            olatT = sb("olatT", [128, 2, 16, 128], BF16, s4); B_olT = [Buf("olT%d" % h) for h in range(16)]
            as_ring = Ring("ast", [sb("ast%d" % i, [128, 16, 128], BF16, s4) for i in range(2)])
            HPG = 2
            L_ring = Ring("Lps", [ps("Lps%d" % i, [128, 512], F32, s4) for i in range(3)])
            _stb = [ps("stps%d" % i, [128, 512], F32, s4) for i in range(3)]
            st_ring3 = Ring("stps", [_stb[i][:, 0:256] for i in range(3)])
            acc = [ps("acc%d" % i, [128, 512], F32, s4) for i in range(HPG)]
            B_acc = [Buf("acc%d" % i) for i in range(HPG)]

            negbig = sb("negbig", [128, 1], F32, s4)
            P.op("pool", lambda e: e.memset(negbig[:], -30000.0), w=[B_v3])
            BB, W0, LO, MID, CNT, GW = 0, 1, 2, 3, 4, 5
            tiles3 = {}

            def load3(j):
                qit, B_qit, kq = qi_ring.next()
                qlt, B_qlt, kl = ql_ring.next()
                P.op("sp", lambda e, qit=qit, j=j: e.dma_start(out=qit[:], in_=qi_d[:, :, j * 128:(j + 1) * 128]), r=[B_qid], w=[B_qit], dma=kq)
                P.op("sp", lambda e, qlt=qlt, j=j: e.dma_start(out=qlt[:], in_=qlat_d[j]), r=[b for bb in B_qlat for b in bb], w=[B_qlt], dma=kl)
                tiles3[j] = (qit, B_qit, qlt, B_qlt)

            def idx_step(j, kb, h):
                qit, B_qit, qlt, B_qlt = tiles3[j]
                ksl = slice(kb * 512, (kb + 1) * 512)
                half = h % 2
                Lp, B_Lp, _ = L_ring.next()
                P.op("pe", lambda e, Lp=Lp, qit=qit, h=h, half=half, ksl=ksl: e.matmul(
                    Lp[:], lhsT=qit[64 * half:64 * half + 64, h // 2, :], rhs=kiT_all[64 * half:64 * half + 64, ksl],
                    start=True, stop=True), r=[B_qit, B_kis], w=[B_Lp])
                P.op("act", lambda e, Lp=Lp: e.activation(out=Lp[:], in_=Lp[:], func=AF.Relu), r=[B_Lp], w=[B_Lp])
                if h == 0:
                    P.op("dve", lambda e, Lp=Lp, ksl=ksl, j=j: e.tensor_scalar(
                        out=Isc[:, ksl], in0=Lp[:], scalar1=wsc[:, j, 0:1], scalar2=None, op0=ALU.mult), r=[B_Lp, B_wsc], w=[B_Ikb[kb]])
                else:
                    P.op("dve", lambda e, Lp=Lp, ksl=ksl, j=j, h=h: e.scalar_tensor_tensor(
                        out=Isc[:, ksl], in0=Lp[:], scalar=wsc[:, j, h:h + 1], in1=Isc[:, ksl], op0=ALU.mult, op1=ALU.add),
                        r=[B_Lp, B_wsc, B_Ikb[kb]], w=[B_Ikb[kb]])

            def emit_bis(j):
                ncol = 8 * (j + 1) * 128
                B_Iall = B_Ikb[0:2 * (j + 1)]
                P.op("dve", lambda e, ncol=ncol: e.tensor_reduce(out=bis[:, BB:BB + 1], in_=Isc[:, 0:ncol], axis=AX.X, op=ALU.max,
                                                               apply_absolute_value=True), r=B_Iall, w=[B_bis])
                P.op("dve", lambda e: e.tensor_scalar(out=bis[:, W0:W0 + 1], in0=bis[:, BB:BB + 1], scalar1=2.0, scalar2=2.0,
                                                      op0=ALU.mult, op1=ALU.add), r=[B_bis], w=[B_bis])
                P.op("dve", lambda e: e.tensor_scalar(out=bis[:, LO:LO + 1], in0=bis[:, BB:BB + 1], scalar1=-1.0, scalar2=-1.0,
                                                      op0=ALU.mult, op1=ALU.add), r=[B_bis], w=[B_bis])
                P.op("dve", lambda e: e.tensor_scalar(out=wk[:], in0=pow2[:], scalar1=bis[:, W0:W0 + 1], scalar2=None, op0=ALU.mult),
                     r=[B_bis, B_pow2], w=[B_bis])
                P.op("dve", lambda e: e.tensor_tensor(out=Isc[:, 0:896], in0=Isc[:, 0:896], in1=vrow3[:], op=ALU.mult), r=B_Ikb[0:2] + [B_v3], w=B_Ikb[0:2])
                P.op("dve", lambda e: e.tensor_tensor(out=Isc[:, 0:896], in0=Isc[:, 0:896], in1=pen7[:], op=ALU.add), r=B_Ikb[0:2] + [B_v3], w=B_Ikb[0:2])
                P.op("dve", lambda e, ncol=ncol: e.memset(Isc[0:64, ncol - 64:ncol], -BIG), r=[B_Iall[-1]], w=[B_Iall[-1]])
                for k in range(NBIS):
                    P.op("dve", lambda e, k=k: e.tensor_tensor(out=bis[:, MID:MID + 1], in0=bis[:, LO:LO + 1], in1=wk[:, k:k + 1], op=ALU.add),
                         r=[B_bis], w=[B_bis])
                    P.op("dve", lambda e, ncol=ncol: e.tensor_scalar(out=junk[:, 0:ncol], in0=Isc[:, 0:ncol], scalar1=bis[:, MID:MID + 1],
                                                                    scalar2=None, op0=ALU.is_ge, op1=ALU.add, accum_out=bis[:, CNT:CNT + 1]),
                         r=B_Iall + [B_bis], w=[B_bis, B_junk])
                    P.op("dve", lambda e, k=k: e.scalar_tensor_tensor(out=bis[:, GW:GW + 1], in0=bis[:, CNT:CNT + 1], scalar=NSEL - 0.5,
                                                                      in1=wk[:, k:k + 1], op0=ALU.is_ge, op1=ALU.mult), r=[B_bis], w=[B_bis])
                    P.op("dve", lambda e: e.tensor_tensor(out=bis[:, LO:LO + 1], in0=bis[:, LO:LO + 1], in1=bis[:, GW:GW + 1], op=ALU.add),
                         r=[B_bis], w=[B_bis])

            def emit_mask(j):
                for kb in range(2 * (j + 1)):
                    ksl = slice(kb * 512, (kb + 1) * 512)
                    mk, B_mk, _ = mk_ring.next()
                    P.op("dve", lambda e, mk=mk, ksl=ksl: e.tensor_scalar(out=mk[:], in0=Isc[:, ksl], scalar1=bis[:, LO:LO + 1], scalar2=None,
                                                                         op0=ALU.is_ge), r=[B_Ikb[kb], B_bis], w=[B_mk])
                    Lp, B_Lp, _ = L_ring.next()
                    Lb = Lp[:].bitcast(BF16)
                    for t4 in range(4):
                        P.op("pe", lambda e, Lb=Lb, mk=mk, t4=t4: e.transpose(out=Lb[:, t4 * 128:(t4 + 1) * 128], in_=mk[:, t4 * 128:(t4 + 1) * 128],
                                                                            identity=ident_b[:]), r=[B_mk, B_const], w=[B_Lp])
                    P.op("act", lambda e, Lb=Lb, kb=kb: e.activation(out=maskT[:, kb * 4:(kb + 1) * 4, :], in_=Lb[:, 0:512].rearrange("p (a b) -> p a b", a=4),
                                                                    func=AF.Identity, scale=30000.0, bias=negbig[:, 0:1]), r=[B_Lp, B_v3], w=[B_mT])

            load3(0)
            for h in range(16):
                for kb in range(2):
                    idx_step(0, kb, h)
            emit_bis(0)
            emit_mask(0)
            for j in range(NOWN):
                nk = 8 * (j + 1)
                qit, B_qit, qlt, B_qlt = tiles3[j]
                pending_idx = []
                if j + 1 < NOWN:
                    load3(j + 1)
                    pending_idx = [(j + 1, kb2 + b, h) for kb2 in range(0, 2 * (j + 2), 2) for h in range(16) for b in range(2)]
                bis_done = [j + 1 >= NOWN]
                SKEW = 2
                inflight = {}
                nit = (16 // HPG) * nk
                per_it = -(-len(pending_idx) // max(1, int(0.45 * nit)))

                def att_s0(it, j=j, nk=nk, qlt=qlt, B_qlt=B_qlt):
                    hg, kt = divmod(it, nk)
                    stp, B_stp, _ = st_ring3.next()
                    for ch in range(2):
                        P.op("pe", lambda e, stp=stp, ch=ch, kt=kt, qlt=qlt, hg=hg: e.matmul(
                            stp, lhsT=cT_all[:, ch, kt * 128:(kt + 1) * 128], rhs=qlt[:, HPG * hg:HPG * hg + HPG, ch, :],
                            start=(ch == 0), stop=False), r=[B_cTs, B_qlt], w=[B_stp])
                    P.op("pe", lambda e, stp=stp, kt=kt: e.matmul(
                        stp, lhsT=ident_b[:], rhs=maskT[:, kt:kt + 1, :].broadcast_to([128, HPG, 128]), start=False, stop=True),
                        r=[B_const, B_mT], w=[B_stp])
                    pT, B_pT, _ = pT_ring.next()
                    P.op("act", lambda e, stp=stp, pT=pT: e.activation(out=pT[:], in_=stp.rearrange("p (a b) -> p a b", a=HPG), func=AF.Exp),
                         r=[B_stp], w=[B_pT])
                    inflight[it] = (pT, B_pT)

                def att_s1(it, j=j, nk=nk):
                    hg, kt = divmod(it, nk)
                    pT, B_pT = inflight.pop(it)
                    for hh in range(HPG):
                        P.op("pe", lambda e, pT=pT, hh=hh, kt=kt, nk=nk: e.matmul(
                            acc[hh][:, 0:257], lhsT=pT[:, hh, :], rhs=c_all[:, kt, :], start=(kt == 0), stop=(kt == nk - 1)),
                            r=[B_pT, B_cs], w=[B_acc[hh]])
                    if kt != nk - 1:
                        return
                    for hh in range(HPG):
                        h = HPG * hg + hh
                        rc, B_rc, _ = rc_ring.next()
                        ol, B_ol, _ = ol_ring.next()
                        P.op("dve", lambda e, rc=rc, hh=hh: e.reciprocal(out=rc[:], in_=acc[hh][:, 256:257]), r=[B_acc[hh]], w=[B_rc])
                        P.op("dve", lambda e, rc=rc, ol=ol, hh=hh: e.tensor_scalar(out=ol[:], in0=acc[hh][:, 0:256], scalar1=rc[:, 0:1], scalar2=None,
                                                                              op0=ALU.mult), r=[B_acc[hh], B_rc], w=[B_ol])
                        Lp, B_Lp, _ = L_ring.next()
                        Lb = Lp[:].bitcast(BF16)
                        for ch in range(2):
                            P.op("pe", lambda e, Lb=Lb, ol=ol, ch=ch: e.transpose(out=Lb[:, ch * 128:(ch + 1) * 128], in_=ol[:, ch * 128:(ch + 1) * 128],
                                                                                identity=ident_b[:]), r=[B_ol, B_const], w=[B_Lp])
                        P.op("act", lambda e, Lb=Lb, h=h: e.copy(out=olatT[:, :, h, :], in_=Lb[:, 0:256].rearrange("p (a b) -> p a b", a=2)),
                             r=[B_Lp], w=[B_olT[h]])

                for step in range(nit + SKEW):
                    if step < nit:
                        att_s0(step)
                    if step - SKEW >= 0:
                        att_s1(step - SKEW)
                    for _ in range(per_it):
                        if pending_idx:
                            idx_step(*pending_idx.pop(0))
                    if not pending_idx and not bis_done[0]:
                        emit_bis(j + 1)
                        bis_done[0] = True
                while pending_idx:
                    idx_step(*pending_idx.pop(0))
                if not bis_done[0]:
                    emit_bis(j + 1)
                ast, B_ast, _ = as_ring.next()
                for h4 in range(4):
                    Lp, B_Lp, _ = L_ring.next()
                    for hh in range(4):
                        h = 4 * h4 + hh
                        for ch in range(2):
                            P.op("pe", lambda e, Lp=Lp, hh=hh, h=h, ch=ch: e.matmul(
                                Lp[:, hh * 128:(hh + 1) * 128], lhsT=wuv[:, ch, h, :], rhs=olatT[:, ch, h, :], start=(ch == 0), stop=(ch == 1)),
                                r=[B_wuv, B_olT[h]], w=[B_Lp])
                    P.op("act", lambda e, Lp=Lp, ast=ast, h4=h4: e.copy(out=ast[:, 4 * h4:4 * h4 + 4, :], in_=Lp[:].rearrange("p (a b) -> p a b", a=4)),
                         r=[B_Lp], w=[B_ast])
                P.op("sp", lambda e, ast=ast, j=j: e.dma_start(out=attnT_d[:, :, j * 128:(j + 1) * 128], in_=ast[:]), r=[B_ast], w=[B_attnT[j]],
                     dma=("attnT_d", j % 4))
                if j + 1 < NOWN:
                    emit_mask(j + 1)

            if debug and stop_after == "3":
                dI = dbg_out("I7", [128, T], F32); dB = dbg_out("bis7", [128, 8], F32)
                P.op("sp", lambda e: e.dma_start(out=dI, in_=Isc[:]), r=B_Ikb, w=[B_dbg], dma=("dbg", 0))
                P.op("sp", lambda e: e.dma_start(out=dB, in_=bis[:]), r=[B_bis], w=[B_dbg], dma=("dbg", 0))

        P.barrier()
        if debug and stop_after == "3":
            d1 = dbg_out("attnT", [128, 16, 1024], BF16)
            P.op("sp", lambda e: e.dma_start(out=d1, in_=attnT_d), r=B_attnT, w=[B_dbg], dma=("dbg", 0))
            return finish_debug()

        w_branch_a = IN("w_branch_a"); w_branch_b = IN("w_branch_b"); w_out = IN("w_out")
        ln1_g = IN("ln1_g"); ln1_b = IN("ln1_b"); w_router = IN("w_router")
        h2_d = dram_scr("h2_d", [NOWN, 128, D], F32)
        B_h2d = [Buf("h2d%d" % j) for j in range(NOWN)]
        h2b = sb("h2b", [128, NOWN, D], BF16); B_h2b = [Buf("h2b%d" % j) for j in range(NOWN)]
        scr = sb("scr", [128, NOWN, NEXP], F32); B_scr = [Buf("scr%d" % j) for j in range(NOWN)]
        wa_v = w_branch_a.rearrange("(k p) n -> p k n", p=128)
        wb_v = w_branch_b.rearrange("(k p) n -> p k n", p=128)
        wo_v = w_out.rearrange("(k p) n -> p k n", p=128)
        with ExitStack() as s5o:
            mergedT = sb("mergedT", [128, KC, 1024], BF16, s5o); B_mg = [[Buf("mg%d_%d" % (m, th)) for th in range(2)] for m in range(KC)]
            with ExitStack() as s5:
                attnT_sb = sb("attnT_sb", [128, KC, 1024], BF16, s5); B_at = Buf("attnT_sb")
                lruT_sb = sb("lruT_sb", [128, KC, 1024], BF16, s5); B_lt = Buf("lruT_sb")
                P.op("sp", lambda e: e.dma_start(out=attnT_sb[:], in_=attnT_d), r=B_attnT, w=[B_at], dma=("attnT_sb", 0))
                P.op("sp", lambda e: e.dma_start(out=lruT_sb[:], in_=lruT_d), r=[b for bb in B_lruT for b in bb], w=[B_lt], dma=("lruT_sb", 0))
                w_ring4 = Ring("wr4", [sb("wr4_%d" % i, [128, KC, 512], BF16, s5) for i in range(2)])
                g_ring = Ring("g4", [sb("g4_%d" % i, [128, 512], BF16, s5) for i in range(4)])
                tA_ring = Ring("tA", [sb("tA%d" % i, [128, 512], F32, s5) for i in range(2)])
                tB_ring = Ring("tB", [sb("tB%d" % i, [128, 512], F32, s5) for i in range(2)])
                pA_ring = Ring("pA", [ps("pA%d" % i, [128, 512], F32, s5) for i in range(3)])
                pB_ring = Ring("pB", [ps("pB%d" % i, [128, 512], F32, s5) for i in range(3)])
                for u in range(4):
                    wa_t, B_wa, kwa = w_ring4.next()
                    wb_t, B_wb, kwb = w_ring4.next()
                    P.op("pool", lambda e, wa_t=wa_t, u=u: e.dma_start(out=wa_t[:], in_=wa_v[:, :, u * 512:(u + 1) * 512]), w=[B_wa], dma=kwa)
                    P.op("pool", lambda e, wb_t=wb_t, u=u: e.dma_start(out=wb_t[:], in_=wb_v[:, :, u * 512:(u + 1) * 512]), w=[B_wb], dma=kwb)
                    for mm in range(4):
                        m = u * 4 + mm
                        for th in range(2):
                            tsl = slice(th * 512, (th + 1) * 512)
                            pA, B_pA, _ = pA_ring.next()
                            pB, B_pB, _ = pB_ring.next()
                            gat, B_gat, kga = g_ring.next()
                            gbt, B_gbt, kgb = g_ring.next()
                            P.op("sp", lambda e, gat=gat, m=m, tsl=tsl: e.dma_start(out=gat[:], in_=gaT_d[:, m, tsl]), r=[B_gaT[m][th]], w=[B_gat], dma=kga)
                            P.op("sp", lambda e, gbt=gbt, m=m, tsl=tsl: e.dma_start(out=gbt[:], in_=gbT_d[:, m, tsl]), r=[B_gbT[m][th]], w=[B_gbt], dma=kgb)
                            for k in range(KC):
                                P.op("pe", lambda e, pA=pA, wa_t=wa_t, k=k, mm=mm, tsl=tsl: e.matmul(
                                    pA[:], lhsT=wa_t[:, k, mm * 128:(mm + 1) * 128], rhs=attnT_sb[:, k, tsl], start=(k == 0), stop=(k == KC - 1)),
                                    r=[B_wa, B_at], w=[B_pA])
                            for k in range(KC):
                                P.op("pe", lambda e, pB=pB, wb_t=wb_t, k=k, mm=mm, tsl=tsl: e.matmul(
                                    pB[:], lhsT=wb_t[:, k, mm * 128:(mm + 1) * 128], rhs=lruT_sb[:, k, tsl], start=(k == 0), stop=(k == KC - 1)),
                                    r=[B_wb, B_lt], w=[B_pB])
                            tA, B_tA, _ = tA_ring.next()
                            tB, B_tB, _ = tB_ring.next()
                            P.op("dve", lambda e, tA=tA, pA=pA, gat=gat: e.tensor_tensor(out=tA[:], in0=pA[:], in1=gat[:], op=ALU.mult), r=[B_pA, B_gat], w=[B_tA])
                            P.op("dve", lambda e, tB=tB, pB=pB, gbt=gbt: e.tensor_tensor(out=tB[:], in0=pB[:], in1=gbt[:], op=ALU.mult), r=[B_pB, B_gbt], w=[B_tB])
                            P.op("pool", lambda e, tA=tA, tB=tB, m=m, tsl=tsl: e.tensor_tensor(out=mergedT[:, m, tsl], in0=tA[:], in1=tB[:], op=ALU.add),
                                 r=[B_tA, B_tB], w=[B_mg[m][th]])
            P.barrier()
            if debug and stop_after == "4a":
                d1 = dbg_out("mergedT", [128, KC, 1024], BF16)
                P.op("sp", lambda e: e.dma_start(out=d1, in_=mergedT[:]), r=[b for bb in B_mg for b in bb], w=[B_dbg], dma=("dbg", 0))
                return finish_debug()
            with ExitStack() as s6:
                wo_sb = sb("wo_sb", [128, KC, D], BF16, s6); B_wo = [Buf("wo%d" % n) for n in range(4)]
                for n in range(4):
                    P.op("pool", lambda e, n=n: e.dma_start(out=wo_sb[:, :, n * 512:(n + 1) * 512], in_=wo_v[:, :, n * 512:(n + 1) * 512]), w=[B_wo[n]], dma=("wo", n))
                g1row = sb("g1row", [128, D], F32, s6); b1row = sb("b1row", [128, D], F32, s6); B_g1 = Buf("g1row")
                P.op("sp", lambda e: e.dma_start(out=g1row[:], in_=ln1_g.partition_broadcast(128)), w=[B_g1], dma=("g1row", 0))
                P.op("sp", lambda e: e.dma_start(out=b1row[:], in_=ln1_b.partition_broadcast(128)), w=[B_g1], dma=("g1row", 0))
                wr_sb = sb("wr_sb", [128, KC, NEXP], F32, s6); B_wr = Buf("wr_sb")
                P.op("sp", lambda e: e.dma_start(out=wr_sb[:], in_=w_router.rearrange("(k p) n -> p k n", p=128)), w=[B_wr], dma=("wr_sb", 0))
                x1_ring = Ring("x1", [sb("x1_%d" % i, [128, D], F32, s6) for i in range(2)])
                ho_ring = Ring("ho4", [sb("ho4_%d" % i, [128, D], F32, s6) for i in range(2)])
                st_ring4 = Ring("st4", [sb("st4_%d" % i, [128, 40], F32, s6) for i in range(2)])
                h2Tf = sb("h2Tf", [128, KC, 128], F32, s6); B_h2Tf = Buf("h2Tf")
                po_ring = Ring("po", [ps("po%d" % i, [128, 512], F32, s6) for i in range(3)])
                tp_ring4 = Ring("tp4", [ps("tp4_%d" % i, [128, 512], F32, s6) for i in range(2)])
                lgps = ps("lgps", [128, NEXP], F32, s6); B_lg = Buf("lgps")
                for i in range(NOWN):
                    isl = slice(i * 128, (i + 1) * 128)
                    hot, B_hot, kho = ho_ring.next()
                    P.op("sp", lambda e, hot=hot, i=i: e.dma_start(out=hot[:], in_=h_own[i]), r=[B_hown[i]], w=[B_hot], dma=kho)
                    x1, B_x1, _ = x1_ring.next()
                    for n in range(4):
                        po, B_po, _ = po_ring.next()
                        for k in range(KC):
                            P.op("pe", lambda e, po=po, k=k, n=n, isl=isl: e.matmul(
                                po[:], lhsT=mergedT[:, k, isl], rhs=wo_sb[:, k, n * 512:(n + 1) * 512], start=(k == 0), stop=(k == KC - 1)),
                                r=[B_mg[k][i // 4], B_wo[n]], w=[B_po])
                        P.op("dve", lambda e, x1=x1, hot=hot, po=po, n=n: e.scalar_tensor_tensor(
                            out=x1[:, n * 512:(n + 1) * 512], in0=hot[:, n * 512:(n + 1) * 512], scalar=ALPHA, in1=po[:], op0=ALU.mult, op1=ALU.add),
                            r=[B_hot, B_po], w=[B_x1])
                    st, B_st, _ = st_ring4.next()
                    for q4 in range(4):
                        P.op("dve", lambda e, st=st, x1=x1, q4=q4: e.bn_stats(out=st[:, q4 * 6:(q4 + 1) * 6], in_=x1[:, q4 * 512:(q4 + 1) * 512]), r=[B_x1], w=[B_st])
                    P.op("dve", lambda e, st=st: e.bn_aggr(out=st[:, 24:26], in_=st[:, 0:24]), r=[B_st], w=[B_st])
                    P.op("act", lambda e, st=st: e.activation(out=st[:, 26:27], in_=st[:, 25:26], func=AF.Sqrt, bias=eps_ln[:, 0:1], scale=1.0), r=[B_st, B_const], w=[B_st])
                    P.op("dve", lambda e, st=st: e.reciprocal(out=st[:, 27:28], in_=st[:, 26:27]), r=[B_st], w=[B_st])
                    P.op("dve", lambda e, st=st, x1=x1: e.tensor_scalar(out=x1[:], in0=x1[:], scalar1=st[:, 24:25], scalar2=st[:, 27:28],
                                                                       op0=ALU.subtract, op1=ALU.mult), r=[B_x1, B_st], w=[B_x1])
                    P.op("pool", lambda e, x1=x1: e.tensor_tensor(out=x1[:], in0=x1[:], in1=g1row[:], op=ALU.mult), r=[B_x1, B_g1], w=[B_x1])
                    P.op("pool", lambda e, x1=x1: e.tensor_tensor(out=x1[:], in0=x1[:], in1=b1row[:], op=ALU.add), r=[B_x1, B_g1], w=[B_x1])
                    P.op("sp", lambda e, x1=x1, i=i: e.dma_start(out=h2_d[i], in_=x1[:]), r=[B_x1], w=[B_h2d[i]], dma=("h2_d", i % 4))
                    P.op("act", lambda e, x1=x1, i=i: e.copy(out=h2b[:, i, :], in_=x1[:]), r=[B_x1], w=[B_h2b[i]])
                    for k4 in range(4):
                        tp, B_tp, _ = tp_ring4.next()
                        for kk in range(4):
                            k = k4 * 4 + kk
                            P.op("pe", lambda e, tp=tp, x1=x1, k=k, kk=kk: e.transpose(out=tp[:, kk * 128:(kk + 1) * 128], in_=x1[:, k * 128:(k + 1) * 128],
                                                                                     identity=ident_f[:]), r=[B_x1, B_const], w=[B_tp])
                        P.op("dve", lambda e, tp=tp, k4=k4: e.tensor_copy(out=h2Tf[:, k4 * 4:(k4 + 1) * 4, :], in_=tp[:].rearrange("p (a b) -> p a b", a=4)),
                             r=[B_tp], w=[B_h2Tf])
                    for k in range(KC):
                        P.op("pe", lambda e, k=k: e.matmul(lgps[:], lhsT=h2Tf[:, k, :], rhs=wr_sb[:, k, :], start=(k == 0), stop=(k == KC - 1)),
                             r=[B_h2Tf, B_wr], w=[B_lg])
                    P.op("act", lambda e, i=i: e.activation(out=scr[:, i, :], in_=lgps[:], func=AF.Sigmoid), r=[B_lg], w=[B_scr[i]])
        P.barrier()
        if debug and stop_after == "4b":
            d1 = dbg_out("h2", [NOWN, 128, D], F32)
            d2 = dbg_out("scr", [128, NOWN, NEXP], F32)
            d3 = dbg_out("h2b", [128, NOWN, D], BF16)
            P.op("sp", lambda e: e.dma_start(out=d1.rearrange("j p d -> p j d"), in_=h2_d.rearrange("j p d -> p j d")), r=B_h2d, w=[B_dbg], dma=("dbg", 0))
            P.op("sp", lambda e: e.dma_start(out=d2, in_=scr[:]), r=B_scr, w=[B_dbg], dma=("dbg", 0))
            P.op("sp", lambda e: e.dma_start(out=d3, in_=h2b[:]), r=B_h2b, w=[B_dbg], dma=("dbg", 0))
            return finish_debug()

        router_bias = IN("router_bias"); w_gate_e = IN("w_gate_e"); w_up_e = IN("w_up_e"); w_down_e = IN("w_down_e")
        w_gate_s = IN("w_gate_s"); w_up_s = IN("w_up_s"); w_down_s = IN("w_down_s"); ln2_g = IN("ln2_g"); ln2_b = IN("ln2_b")
        with ExitStack() as s7:
            yacc = sb("yacc", [128, NOWN, D], F32, s7); B_y = [[Buf("y%d_%d" % (i, n)) for n in range(4)] for i in range(NOWN)]
            iota_row = sb("iota_row", [128, CAP], F32, s7)
            iota_i = sb("iota_i", [128, CAP], I32, s7)
            ones_b = sb("ones_b", [128, 128], BF16, s7); ustr_b = sb("ustr_b", [128, 128], BF16, s7)
            B_c5 = Buf("const5")
            P.op("pool", lambda e: e.iota(iota_i[:], pattern=[[1, CAP]], base=0, channel_multiplier=0), w=[B_c5])
            P.op("pool", lambda e: e.tensor_copy(out=iota_row[:], in_=iota_i[:]), r=[B_c5], w=[B_c5])
            P.op("pool", lambda e: e.tensor_copy(out=ones_b[:], in_=ones_f[:]), r=[B_const], w=[B_c5])
            P.op("pool", lambda e: e.affine_select(out=ustr_b[:], in_=ones_b[:], pattern=[[1, 128]], compare_op=ALU.is_gt, fill=0.0,
                                                   base=0, channel_multiplier=-1), r=[B_c5], w=[B_c5])
            rbias = sb("rbias", [128, NEXP], F32, s7); B_rb = Buf("rbias")
            P.op("sp", lambda e: e.dma_start(out=rbias[:], in_=router_bias.partition_broadcast(128)), w=[B_rb], dma=("rbias", 0))
            rankm = sb("rankm", [128, NOWN, NEXP], F32, s7); gates = sb("gates", [128, NOWN, NEXP], F32, s7)
            B_rt = [Buf("rt%d" % i) for i in range(NOWN)]
            B_gt = [Buf("gt%d" % i) for i in range(NOWN)]
            B_rk = Buf("rankm")
            B_rtt = Buf("rtt")
            BIA, SRT, GS, GSRT, GM, PEN, MBV, TOP, GSEL, SS = 0, 64, 128, 136, 144, 152, 160, 224, 232, 296
            s8 = ExitStack()
            s8.__enter__()
            Mf = sb("Mf", [128, NOWN, NEXP], F32, s8); Mb = sb("Mb", [128, NOWN, NEXP], BF16, s8)
            rt = sb("rt", [128, 400], F32, s8)
            for i in range(NOWN):
                P.op("dve", lambda e, i=i: e.tensor_tensor(out=rt[:, BIA:BIA + 64], in0=scr[:, i, :], in1=rbias[:], op=ALU.add), r=[B_scr[i], B_rb], w=[B_rtt])
                for g in range(8):
                    P.op("dve", lambda e, g=g: e.max(out=rt[:, SRT + 8 * g:SRT + 8 * g + 8], in_=rt[:, BIA + 8 * g:BIA + 8 * g + 8]), r=[B_rtt], w=[B_rtt])
                srt3 = rt[:, SRT:SRT + 64].rearrange("p (g k) -> p g k", g=8)
                P.op("dve", lambda e, srt3=srt3: e.tensor_tensor(out=rt[:, GS:GS + 8], in0=srt3[:, :, 0], in1=srt3[:, :, 1], op=ALU.add), r=[B_rtt], w=[B_rtt])
                P.op("dve", lambda e: e.max(out=rt[:, GSRT:GSRT + 8], in_=rt[:, GS:GS + 8]), r=[B_rtt], w=[B_rtt])
                P.op("dve", lambda e: e.tensor_scalar(out=rt[:, GM:GM + 8], in0=rt[:, GS:GS + 8], scalar1=rt[:, GSRT + 3:GSRT + 4], scalar2=None, op0=ALU.is_ge),
                     r=[B_rtt], w=[B_rtt])
                P.op("dve", lambda e: e.tensor_scalar(out=rt[:, PEN:PEN + 8], in0=rt[:, GM:GM + 8], scalar1=-1.0, scalar2=BIG, op0=ALU.add, op1=ALU.mult),
                     r=[B_rtt], w=[B_rtt])
                bia3 = rt[:, BIA:BIA + 64].rearrange("p (g k) -> p g k", g=8)
                mb3 = rt[:, MBV:MBV + 64].rearrange("p (g k) -> p g k", g=8)
                gm3 = rt[:, GM:GM + 8].unsqueeze(2).broadcast_to([128, 8, 8])
                pen3 = rt[:, PEN:PEN + 8].unsqueeze(2).broadcast_to([128, 8, 8])
                P.op("dve", lambda e, mb3=mb3, bia3=bia3, gm3=gm3: e.tensor_tensor(out=mb3, in0=bia3, in1=gm3, op=ALU.mult), r=[B_rtt], w=[B_rtt])
                P.op("dve", lambda e, mb3=mb3, pen3=pen3: e.tensor_tensor(out=mb3, in0=mb3, in1=pen3, op=ALU.add), r=[B_rtt], w=[B_rtt])
                P.op("dve", lambda e: e.max(out=rt[:, TOP:TOP + 8], in_=rt[:, MBV:MBV + 64]), r=[B_rtt], w=[B_rtt])
                P.op("dve", lambda e, i=i: e.tensor_scalar(out=Mf[:, i, :], in0=rt[:, MBV:MBV + 64], scalar1=rt[:, TOP + 7:TOP + 8], scalar2=None, op0=ALU.is_ge),
                     r=[B_rtt], w=[B_rt[i]])
                P.op("dve", lambda e, i=i: e.tensor_copy(out=Mb[:, i, :], in_=Mf[:, i, :]), r=[B_rt[i]], w=[B_rt[i]])
                P.op("dve", lambda e, i=i: e.tensor_tensor(out=rt[:, GSEL:GSEL + 64], in0=scr[:, i, :], in1=Mf[:, i, :], op=ALU.mult), r=[B_rt[i], B_scr[i]], w=[B_rtt])
                P.op("dve", lambda e: e.tensor_reduce(out=rt[:, SS:SS + 1], in_=rt[:, GSEL:GSEL + 64], axis=AX.X, op=ALU.add), r=[B_rtt], w=[B_rtt])
                P.op("dve", lambda e: e.reciprocal(out=rt[:, SS + 1:SS + 2], in_=rt[:, SS:SS + 1]), r=[B_rtt], w=[B_rtt])
                P.op("dve", lambda e, i=i: e.tensor_scalar(out=gates[:, i, :], in0=rt[:, GSEL:GSEL + 64], scalar1=rt[:, SS + 1:SS + 2], scalar2=2.5,
                                                           op0=ALU.mult, op1=ALU.mult), r=[B_rtt], w=[B_gt[i]])
            if True:
                rkps = ps("rkps", [128, NEXP], F32, s8); B_rkps = Buf("rkps")
                for i in range(NOWN):
                    if i % 2 == 1:
                        P.op("pe", lambda e, i=i: e.matmul(rkps[:], lhsT=ones_b[:], rhs=Mb[:, i - 1, :], start=True, stop=False),
                             r=[B_c5, B_rt[i - 1]], w=[B_rkps])
                    P.op("pe", lambda e, i=i: e.matmul(rkps[:], lhsT=ustr_b[:], rhs=Mb[:, i, :], start=(i % 2 == 0), stop=True), r=[B_c5, B_rt[i]], w=[B_rkps])
                    P.op("dve", lambda e, i=i: e.scalar_tensor_tensor(out=rankm[:, i, :], in0=rkps[:], scalar=1.0 + 64.0 * ((i // 2) % 2), in1=Mf[:, i, :],
                                                                      op0=ALU.add, op1=ALU.mult), r=[B_rkps, B_rt[i]], w=[B_rk])
                    P.op("dve", lambda e, i=i: e.tensor_scalar(out=rankm[:, i, :], in0=rankm[:, i, :], scalar1=-1.0, scalar2=None, op0=ALU.add), r=[B_rk], w=[B_rk])
                h2T_sb = sb("h2T_sb", [128, KC, 1024], BF16, s8); B_h2T = Buf("h2T")
                wsg = sb("wsg", [128, KC, 512], BF16, s8); wsu = sb("wsu", [128, KC, 512], BF16, s8); wsd = sb("wsd", [128, 4, D], BF16, s8)
                B_ws = Buf("ws")
                P.op("pool", lambda e: e.dma_start(out=wsg[:], in_=w_gate_s.rearrange("(k p) n -> p k n", p=128)), w=[B_ws], dma=("ws", 0))
                P.op("pool", lambda e: e.dma_start(out=wsu[:], in_=w_up_s.rearrange("(k p) n -> p k n", p=128)), w=[B_ws], dma=("ws", 0))
                P.op("pool", lambda e: e.dma_start(out=wsd[:], in_=w_down_s.rearrange("(k p) n -> p k n", p=128)), w=[B_ws], dma=("ws", 0))
                actTs = sb("actTs", [128, 4, 1024], BF16, s8); B_acts = Buf("actTs")
                sg_ring = Ring("sgs", [sb("sgs%d" % i, [128, 512], F32, s8) for i in range(2)])
                tpb = ps("tpb", [128, 512], F32, s8); B_tpb = Buf("tpb")
                pg_ring = Ring("pgs", [ps("pgs%d" % i, [128, 512], F32, s8) for i in range(2)])
                pu_ring = Ring("pus", [ps("pus%d" % i, [128, 512], F32, s8) for i in range(2)])
                pd_ring = Ring("pds", [ps("pds%d" % i, [128, 512], F32, s8) for i in range(2)])
                tpbb = tpb[:].bitcast(BF16)
                for i in range(NOWN):
                    for k4 in range(2):
                        for kk in range(8):
                            k = k4 * 8 + kk
                            P.op("pe", lambda e, i=i, k=k, kk=kk: e.transpose(out=tpbb[:, kk * 128:(kk + 1) * 128], in_=h2b[:, i, k * 128:(k + 1) * 128],
                                                                            identity=ident_b[:]), r=[B_h2b[i], B_const], w=[B_tpb])
                        P.op("act", lambda e, i=i, k4=k4: e.copy(out=h2T_sb[:, k4 * 8:(k4 + 1) * 8, i * 128:(i + 1) * 128],
                                                                in_=tpbb.rearrange("p (a b) -> p a b", a=8)), r=[B_tpb], w=[B_h2T])
                for m in range(4):
                    for th in range(2):
                        tsl = slice(th * 512, (th + 1) * 512)
                        pg, B_pg, _ = pg_ring.next(); pu, B_pu, _ = pu_ring.next()
                        for k in range(KC):
                            P.op("pe", lambda e, pg=pg, k=k, m=m, tsl=tsl: e.matmul(pg[:], lhsT=wsg[:, k, m * 128:(m + 1) * 128], rhs=h2T_sb[:, k, tsl],
                                                                                 start=(k == 0), stop=(k == KC - 1)), r=[B_ws, B_h2T], w=[B_pg])
                        for k in range(KC):
                            P.op("pe", lambda e, pu=pu, k=k, m=m, tsl=tsl: e.matmul(pu[:], lhsT=wsu[:, k, m * 128:(m + 1) * 128], rhs=h2T_sb[:, k, tsl],
                                                                                 start=(k == 0), stop=(k == KC - 1)), r=[B_ws, B_h2T], w=[B_pu])
                        sg, B_sg, _ = sg_ring.next()
                        P.op("act", lambda e, sg=sg, pg=pg: e.activation(out=sg[:], in_=pg[:], func=AF.Silu), r=[B_pg], w=[B_sg])
                        P.op("dve", lambda e, sg=sg, pu=pu, m=m, tsl=tsl: e.tensor_tensor(out=actTs[:, m, tsl], in0=sg[:], in1=pu[:], op=ALU.mult),
                             r=[B_sg, B_pu], w=[B_acts])
                for i in range(NOWN):
                    for n in range(4):
                        pd, B_pd, _ = pd_ring.next()
                        for kf in range(4):
                            P.op("pe", lambda e, pd=pd, kf=kf, i=i, n=n: e.matmul(pd[:], lhsT=actTs[:, kf, i * 128:(i + 1) * 128], rhs=wsd[:, kf, n * 512:(n + 1) * 512],
                                                                               start=(kf == 0), stop=(kf == 3)), r=[B_acts, B_ws], w=[B_pd])
                        if (i * 4 + n) % 2 == 0:
                            P.op("act", lambda e, pd=pd, i=i, n=n: e.copy(out=yacc[:, i, n * 512:(n + 1) * 512], in_=pd[:]), r=[B_pd], w=[B_y[i][n]])
                        else:
                            P.op("dve", lambda e, pd=pd, i=i, n=n: e.tensor_copy(out=yacc[:, i, n * 512:(n + 1) * 512], in_=pd[:]), r=[B_pd], w=[B_y[i][n]])
            s8.__exit__(None, None, None)
            P.barrier()
            if debug and stop_after == "5a":
                d1 = dbg_out("gates", [128, NOWN, NEXP], F32); d2 = dbg_out("rankm", [128, NOWN, NEXP], F32); d3 = dbg_out("ysh", [128, NOWN, D], F32)
                P.op("sp", lambda e: e.dma_start(out=d1, in_=gates[:]), r=B_gt, w=[B_dbg], dma=("dbg", 0))
                P.op("sp", lambda e: e.dma_start(out=d2, in_=rankm[:]), r=[B_rk], w=[B_dbg], dma=("dbg", 0))
                P.op("sp", lambda e: e.dma_start(out=d3, in_=yacc[:]), r=[b for bb in B_y for b in bb], w=[B_dbg], dma=("dbg", 0))
                return finish_debug()
            GCAP = 128
            with ExitStack() as s9:
                NE = NEXP if not (debug and stop_after == "5b") else 4
                wring = Ring("we", [sb("we%d" % i, [128, 8192], BF16, s9) for i in range(4)])
                S_ring = Ring("S", [sb("S_sb%d" % i, [128, NOWN, GCAP], BF16, s9) for i in range(2)])
                SW_ring = Ring("SW", [sb("SW_sb%d" % i, [128, NOWN, GCAP], BF16, s9) for i in range(2)])
                XgT = sb("XgT", [128, KC, CAP], BF16, s9); B_Xg = [Buf("Xg%d" % k) for k in range(8)]
                actT = sb("actT", [128, 4, CAP], BF16, s9); B_actT = Buf("actT")
                sg_r = Ring("sgt", [sb("sgt%d" % i, [128, CAP], F32, s9) for i in range(2)])
                Y_sb = sb("Y_sb", [128, 2, D], BF16, s9); B_Y = Buf("Y")
                SWT = sb("SWT", [128, 2, 512], BF16, s9); B_SWT = Buf("SWT")
                ga_ring5 = Ring("gat", [ps("gat%d" % i, [128, 512], F32, s9) for i in range(2)])
                gu_ring = Ring("gup", [ps("gup%d" % i, [128, 512], F32, s9) for i in range(2)])
                dn_ring = Ring("dnp", [ps("dnp%d" % i, [128, 512], F32, s9) for i in range(3)])
                swps = ps("swps", [128, 512], F32, s9); B_swps = Buf("swps")
                sc_ring = dn_ring
                swb = swps[:].bitcast(BF16)
                ev = [0]
                pend = {}

                def load_w(kind, e_):
                    wt, B_w, kw = wring.next()
                    if kind == "d":
                        P.op("pool", lambda e, wt=wt, e_=e_: e.dma_start(out=wt[:].rearrange("p (k n) -> p k n", k=4),
                                                                        in_=w_down_e[e_].rearrange("(k p) n -> p k n", p=128)), w=[B_w], dma=kw)
                        pend.setdefault(e_, {})["wd"] = (wt[:].rearrange("p (k n) -> p k n", k=4), B_w)
                    else:
                        src = w_gate_e if kind == "g" else w_up_e
                        P.op("pool", lambda e, wt=wt, e_=e_, src=src: e.dma_start(out=wt[:].rearrange("p (k n) -> p k n", k=KC),
                                                                                 in_=src[e_].rearrange("(k p) n -> p k n", p=128)), w=[B_w], dma=kw)
                        pend.setdefault(e_, {})["w" + kind] = (wt[:].rearrange("p (k n) -> p k n", k=KC), B_w)

                def E_prep(e_):
                    S_sb, B_S, _ = S_ring.next()
                    SW_sb, B_SW, _ = SW_ring.next()
                    for i in range(NOWN):
                        P.op("dve", lambda e, i=i, e_=e_, S_sb=S_sb: e.tensor_scalar(out=S_sb[:, i, :], in0=iota_row[:, 0:GCAP], scalar1=rankm[:, i, e_:e_ + 1],
                                                                                   scalar2=None, op0=ALU.is_equal), r=[B_c5, B_rk], w=[B_S])
                    for i in range(NOWN):
                        P.op("act", lambda e, i=i, e_=e_, S_sb=S_sb, SW_sb=SW_sb: e.activation(out=SW_sb[:, i, :], in_=S_sb[:, i, :], func=AF.Copy,
                                                                                             scale=gates[:, i, e_:e_ + 1]), r=[B_S, B_gt[i]], w=[B_SW])
                    pend.setdefault(e_, {}).update(S=S_sb, B_S=B_S, SW=SW_sb, B_SW=B_SW)

                def E_gather(e_):
                    d_ = pend[e_]
                    S_sb, B_S = d_["S"], d_["B_S"]
                    for k2 in range(8):
                        gp, B_gp, _ = ga_ring5.next()
                        for kk in range(2):
                            k = 2 * k2 + kk
                            for i in range(NOWN):
                                c0 = kk * CAP + GCAP * (i // 4)
                                P.op("pe", lambda e, gp=gp, c0=c0, k=k, i=i, S_sb=S_sb: e.matmul(gp[:, c0:c0 + GCAP], lhsT=h2b[:, i, k * 128:(k + 1) * 128],
                                                                                            rhs=S_sb[:, i, :], start=(i % 4 == 0), stop=(i % 4 == 3)),
                                     r=[B_h2b[i], B_S], w=[B_gp])
                        ev[0] += 1
                        if ev[0] % 2 == 0:
                            P.op("act", lambda e, gp=gp, k2=k2: e.copy(out=XgT[:, 2 * k2:2 * k2 + 2, :], in_=gp[:].rearrange("p (a b) -> p a b", a=2)), r=[B_gp], w=[B_Xg[k2]])
                        else:
                            P.op("dve", lambda e, gp=gp, k2=k2: e.tensor_copy(out=XgT[:, 2 * k2:2 * k2 + 2, :], in_=gp[:].rearrange("p (a b) -> p a b", a=2)), r=[B_gp], w=[B_Xg[k2]])

                def E_swt(e_):
                    d_ = pend[e_]
                    SW_sb, B_SW = d_["SW"], d_["B_SW"]
                    for cs in range(2):
                        for il in range(4):
                            i = 4 * cs + il
                            P.op("pe", lambda e, il=il, i=i, SW_sb=SW_sb: e.transpose(out=swb[:, il * 128:(il + 1) * 128], in_=SW_sb[:, i, :],
                                                                                   identity=ident_b[:]), r=[B_SW, B_const], w=[B_swps])
                        P.op("act", lambda e, cs=cs: e.copy(out=SWT[:, cs, :], in_=swb[:, 0:512]), r=[B_swps], w=[B_SWT])

                def E_gu(e_):
                    d_ = pend[e_]
                    wg3, B_wg = d_["wg"]; wu3, B_wu = d_["wu"]
                    for m in range(4):
                        gup, B_gu, _ = gu_ring.next()
                        for k in range(KC):
                            P.op("pe", lambda e, gup=gup, k=k, m=m, wg3=wg3: e.matmul(gup[:, 0:CAP], lhsT=wg3[:, k, m * 128:(m + 1) * 128], rhs=XgT[:, k, :],
                                                                                   start=(k == 0), stop=(k == KC - 1)), r=[B_wg, B_Xg[k // 2]], w=[B_gu])
                        for k in range(KC):
                            P.op("pe", lambda e, gup=gup, k=k, m=m, wu3=wu3: e.matmul(gup[:, CAP:2 * CAP], lhsT=wu3[:, k, m * 128:(m + 1) * 128], rhs=XgT[:, k, :],
                                                                                   start=(k == 0), stop=(k == KC - 1)), r=[B_wu, B_Xg[k // 2]], w=[B_gu])
                        sgt, B_sgt, _ = sg_r.next()
                        P.op("act", lambda e, gup=gup, sgt=sgt: e.activation(out=sgt[:], in_=gup[:, 0:CAP], func=AF.Silu), r=[B_gu], w=[B_sgt])
                        P.op("dve", lambda e, gup=gup, sgt=sgt, m=m: e.tensor_tensor(out=actT[:, m, :], in0=sgt[:], in1=gup[:, CAP:2 * CAP], op=ALU.mult),
                             r=[B_sgt, B_gu], w=[B_actT])

                def E_down(e_):
                    d_ = pend[e_]
                    wd3, B_wd = d_["wd"]
                    for cs in range(2):
                        for n in range(4):
                            dn, B_dn, _ = dn_ring.next()
                            for kf in range(4):
                                P.op("pe", lambda e, dn=dn, kf=kf, cs=cs, n=n, wd3=wd3: e.matmul(dn[:], lhsT=actT[:, kf, cs * 128:(cs + 1) * 128],
                                                                                              rhs=wd3[:, kf, n * 512:(n + 1) * 512], start=(kf == 0), stop=(kf == 3)),
                                     r=[B_actT, B_wd], w=[B_dn])
                            ev[0] += 1
                            if ev[0] % 2 == 0:
                                P.op("act", lambda e, dn=dn, cs=cs, n=n: e.copy(out=Y_sb[:, cs, n * 512:(n + 1) * 512], in_=dn[:]), r=[B_dn], w=[B_Y])
                            else:
                                P.op("dve", lambda e, dn=dn, cs=cs, n=n: e.tensor_copy(out=Y_sb[:, cs, n * 512:(n + 1) * 512], in_=dn[:]), r=[B_dn], w=[B_Y])

                def E_scatter(e_):
                    for i in range(NOWN):
                        cs = i // 4
                        pb = 64 * ((i // 2) % 2)
                        il = i % 4
                        for n in range(4):
                            scp, B_scp, _ = sc_ring.next()
                            P.op("pe", lambda e, scp=scp, cs=cs, pb=pb, il=il, n=n: e.matmul(
                                scp[:], lhsT=SWT[:, cs, il * 128:(il + 1) * 128], rhs=Y_sb[:, cs, n * 512:(n + 1) * 512],
                                start=True, stop=True), r=[B_SWT, B_Y], w=[B_scp])
                            P.op("dve", lambda e, scp=scp, i=i, n=n: e.tensor_tensor(out=yacc[:, i, n * 512:(n + 1) * 512], in0=scp[:],
                                                                                  in1=yacc[:, i, n * 512:(n + 1) * 512], op=ALU.add), r=[B_scp, B_y[i][n]], w=[B_y[i][n]])
                    pend.pop(e_, None)

                load_w("g", 0); load_w("u", 0); load_w("d", 0)
                E_prep(0)
                E_gather(0)
                for e_ in range(NE):
                    E_swt(e_)
                    if e_ + 1 < NE:
                        load_w("g", e_ + 1)
                        E_prep(e_ + 1)
                    E_gu(e_)
                    if e_ + 1 < NE:
                        load_w("u", e_ + 1); load_w("d", e_ + 1)
                    E_down(e_)
                    if e_ + 1 < NE:
                        E_gather(e_ + 1)
                    E_scatter(e_)
            P.barrier()
            with ExitStack() as s10:
                g2row = sb("g2row", [128, D], F32, s10); b2row = sb("b2row", [128, D], F32, s10); B_g2 = Buf("g2row")
                P.op("sp", lambda e: e.dma_start(out=g2row[:], in_=ln2_g.partition_broadcast(128)), w=[B_g2], dma=("g2row", 0))
                P.op("sp", lambda e: e.dma_start(out=b2row[:], in_=ln2_b.partition_broadcast(128)), w=[B_g2], dma=("g2row", 0))
                h2_ring = Ring("h2r", [sb("h2r%d" % i, [128, D], F32, s10) for i in range(2)])
                st_ring5 = Ring("st5", [sb("st5_%d" % i, [128, 40], F32, s10) for i in range(2)])
                B_out = Buf("out", wo=True)
                for i in range(NOWN):
                    h2t, B_h2t, kh2 = h2_ring.next()
                    P.op("sp", lambda e, h2t=h2t, i=i: e.dma_start(out=h2t[:], in_=h2_d[i]), r=[B_h2d[i]], w=[B_h2t], dma=kh2)
                    P.op("dve", lambda e, h2t=h2t, i=i: e.scalar_tensor_tensor(out=h2t[:], in0=h2t[:], scalar=ALPHA, in1=yacc[:, i, :], op0=ALU.mult, op1=ALU.add),
                         r=[B_h2t] + B_y[i], w=[B_h2t])
                    st, B_st, _ = st_ring5.next()
                    for q4 in range(4):
                        P.op("dve", lambda e, st=st, h2t=h2t, q4=q4: e.bn_stats(out=st[:, q4 * 6:(q4 + 1) * 6], in_=h2t[:, q4 * 512:(q4 + 1) * 512]), r=[B_h2t], w=[B_st])
                    P.op("dve", lambda e, st=st: e.bn_aggr(out=st[:, 24:26], in_=st[:, 0:24]), r=[B_st], w=[B_st])
                    P.op("act", lambda e, st=st: e.activation(out=st[:, 26:27], in_=st[:, 25:26], func=AF.Sqrt, bias=eps_ln[:, 0:1], scale=1.0), r=[B_st, B_const], w=[B_st])
                    P.op("dve", lambda e, st=st: e.reciprocal(out=st[:, 27:28], in_=st[:, 26:27]), r=[B_st], w=[B_st])
                    P.op("dve", lambda e, st=st, h2t=h2t: e.tensor_scalar(out=h2t[:], in0=h2t[:], scalar1=st[:, 24:25], scalar2=st[:, 27:28],
                                                                         op0=ALU.subtract, op1=ALU.mult), r=[B_h2t, B_st], w=[B_h2t])
                    P.op("pool", lambda e, h2t=h2t: e.tensor_tensor(out=h2t[:], in0=h2t[:], in1=g2row[:], op=ALU.mult), r=[B_h2t, B_g2], w=[B_h2t])
                    P.op("pool", lambda e, h2t=h2t: e.tensor_tensor(out=h2t[:], in0=h2t[:], in1=b2row[:], op=ALU.add), r=[B_h2t, B_g2], w=[B_h2t])
                    P.op("sp", lambda e, h2t=h2t, i=i: e.dma_start(out=out[i * 128:(i + 1) * 128, :], in_=h2t[:]), r=[B_h2t], w=[B_out], dma=("out", 0))
                P.op("sp", None, r=[B_out])

        P.emit()
    return nc, dbg, list(declared)


def make_in_maps(inputs, names=None, cores=None):
    x = np.ascontiguousarray(np.asarray(inputs["x"], dtype=np.float32).reshape(T, D))
    shared = {}
    for k, v in inputs.items():
        if k == "x":
            continue
        a = np.asarray(v, dtype=np.float32)
        if a.ndim >= 2 and a.shape[0] == 1 and k not in ("ln_in_g", "ln_in_b"):
            a = a[0]
        shared[k] = np.ascontiguousarray(a)
    maps = []
    for c in (range(NCORES) if cores is None else cores):
        pad = (7 - c) * 128
        xw = np.zeros((T, D), np.float32)
        xw[pad:] = x[:T - pad]
        v7 = np.zeros((1, 896), np.float32)
        v7[0, pad:] = 1.0
        m = dict(shared)
        m["xw"] = xw
        m["valid7"] = v7
        if names is not None:
            m = {k: v for k, v in m.items() if k in names}
        maps.append(m)
    return maps


def kernel(**inputs):
    nc, _, _ = build_program()
    maps = make_in_maps(inputs)
    res = run_bass_kernel_spmd(nc, maps, core_ids=list(range(NCORES)))
    full = np.zeros((T, D), np.float32)
    for c in range(NCORES):
        o = res.results[c]["out"].reshape(NOWN, 128, D)
        for j in range(NOWN):
            g = c + 8 * j
            full[g * 128:(g + 1) * 128] = o[j]
    return full.reshape(1, T, D)
```
